# Optimizing a Trainium2 kernel written in Bass

```python
import jax, jax.numpy as jnp
from jax import lax
import numpy as np

D_MODEL = 1024
BATCH = 4
SEQ = 8192
DEPTH = 4

CTX_LEN = 256
GRID_W = 64
EPS = 1e-6
N_MOD = 6

MLSTM_HEADS = 4
MLSTM_HD = 128
MLSTM_W = MLSTM_HEADS * MLSTM_HD
MLSTM_CHUNK = 128
QK_CONV = 3

SGU_GROUPS = 4
SGU_CHUNK = 128
SGU_W = 512
SGU_GW = SGU_W // SGU_GROUPS

FNET_GROUPS = 4
FNET_W = 512
FNET_GW = FNET_W // FNET_GROUPS

N_BRANCH = 3
BRANCH_W = 512

PROJ_SPLITS = (MLSTM_W, MLSTM_W, MLSTM_W, MLSTM_W, 4 * MLSTM_HEADS, SGU_W, SGU_W, FNET_W, N_BRANCH * D_MODEL)
PROJ_W = 4 * MLSTM_W + 4 * MLSTM_HEADS + 2 * SGU_W + FNET_W + N_BRANCH * D_MODEL
GATE_OFF = 4 * MLSTM_W

PEER_HEADS = 8
N_KEYS = 128
N_EXPERTS = N_KEYS * N_KEYS
PEER_TOPK = 16
D_QUERY = D_MODEL // 4
D_SUB = D_QUERY // 2
PEER_BLOCK = 128

kernel_name = 'hybrid_mlstm_sgu_fnet_peer_prefix_dit'


def rmsnorm(x, g):
    xf = x.astype(jnp.float32)
    y = xf * lax.rsqrt(jnp.mean(xf * xf, axis=-1, keepdims=True) + EPS)
    return (y * g.astype(jnp.float32)).astype(x.dtype)


def modulate(h, shift, scale):
    return h * (1 + scale) + shift


def pos_embed_2d(rows, dtype):
    quarter = D_MODEL // 4
    omega = 1.0 / (10000.0 ** (jnp.arange(quarter, dtype=jnp.float32) / quarter))
    r = jnp.repeat(jnp.arange(rows, dtype=jnp.float32), GRID_W)[:, None] * omega
    cc = jnp.tile(jnp.arange(GRID_W, dtype=jnp.float32), rows)[:, None] * omega
    return jnp.concatenate([jnp.sin(r), jnp.cos(r), jnp.sin(cc), jnp.cos(cc)], axis=-1).astype(dtype)


def conv_centred(x, w):
    pad = w.shape[0] // 2
    return lax.conv_general_dilated(x, w[:, None, :].astype(x.dtype), window_strides=(1,), padding=[(pad, pad)], dimension_numbers=('NWC', 'WIO', 'NWC'), feature_group_count=x.shape[-1])


def mlstm_zero_state(batch):
    return (jnp.zeros((batch, MLSTM_HEADS, MLSTM_HD, MLSTM_HD), jnp.float32),
            jnp.zeros((batch, MLSTM_HEADS, MLSTM_HD), jnp.float32),
            jnp.zeros((batch, MLSTM_HEADS), jnp.float32))


def mlstm_scan(q, k, v, ig, lf, state):
    B, T, H, d = q.shape
    L = MLSTM_CHUNK
    nc = T // L
    to_chunks = lambda a: a.reshape(B, nc, L, H, d).transpose(1, 0, 3, 2, 4)
    gate_chunks = lambda a: a.reshape(B, nc, L, H).transpose(1, 0, 3, 2)
    earlier = jnp.tril(jnp.ones((L, L), dtype=bool))

    def step(carry, inp):
        C, n, m = carry
        qc, kc, vc, ic, fc = inp
        b = jnp.cumsum(fc, axis=-1)
        dmat = jnp.where(earlier, b[..., :, None] - b[..., None, :] + ic[..., None, :], -jnp.inf)
        inter = b + m[..., None]
        m_t = jnp.maximum(inter, jnp.max(dmat, axis=-1))
        s = jnp.einsum('bhtd,bhsd->bhts', qc, kc) * jnp.exp(dmat - m_t[..., None])
        a = jnp.exp(inter - m_t)
        num = jnp.einsum('bhts,bhse->bhte', s, vc) + a[..., None] * jnp.einsum('bhtd,bhde->bhte', qc, C)
        den = jnp.sum(s, axis=-1) + a * jnp.einsum('bhtd,bhd->bht', qc, n)
        h = num / jnp.maximum(jnp.abs(den), jnp.exp(-m_t))[..., None]
        b_last = b[..., -1]
        g = b_last[..., None] - b + ic
        m_new = jnp.maximum(b_last + m, jnp.max(g, axis=-1))
        w = jnp.exp(g - m_new[..., None])
        decay = jnp.exp(b_last + m - m_new)
        C_new = decay[..., None, None] * C + jnp.einsum('bhsd,bhse->bhde', kc * w[..., None], vc)
        n_new = decay[..., None] * n + jnp.einsum('bhs,bhsd->bhd', w, kc)
        return (C_new, n_new, m_new), h

    final, hs = lax.scan(step, state, (to_chunks(q), to_chunks(k), to_chunks(v), gate_chunks(ig), gate_chunks(lf)))
    return hs.transpose(1, 0, 3, 2, 4).reshape(B, T, H, d), final


def mlstm_branch(q_raw, k_raw, v_raw, o_raw, gate_raw, conv_w, norm_g, init_fwd, init_bwd):
    B, T, _ = q_raw.shape
    qk = jax.nn.silu(conv_centred(jnp.concatenate([q_raw, k_raw], axis=-1), conv_w))
    heads = lambda a: a.astype(jnp.float32).reshape(B, T, MLSTM_HEADS, MLSTM_HD)
    q = heads(qk[..., :MLSTM_W])
    k = heads(qk[..., MLSTM_W:]) * (MLSTM_HD ** -0.5)
    v = heads(v_raw)
    gates = gate_raw.astype(jnp.float32).reshape(B, T, 4, MLSTM_HEADS)
    i_f, lf_f = gates[:, :, 0], jax.nn.log_sigmoid(gates[:, :, 1])
    i_b, lf_b = gates[:, :, 2], jax.nn.log_sigmoid(gates[:, :, 3])
    h_f, st_f = mlstm_scan(q, k, v, i_f, lf_f, init_fwd)
    rev = lambda a: jnp.flip(a, axis=1)
    h_b, st_b = mlstm_scan(rev(q), rev(k), rev(v), rev(i_b), rev(lf_b), init_bwd)
    h = h_f + rev(h_b)
    mu = jnp.mean(h, axis=-1, keepdims=True)
    var = jnp.mean(jnp.square(h - mu), axis=-1, keepdims=True)
    h = (h - mu) * lax.rsqrt(var + EPS) * norm_g.astype(jnp.float32).reshape(MLSTM_HEADS, MLSTM_HD)
    out = jax.nn.sigmoid(o_raw.astype(jnp.float32)) * h.reshape(B, T, MLSTM_W)
    return out.astype(q_raw.dtype), st_f, st_b


def sgu_branch(u_raw, v_raw, w_s, b_s):
    B, T, _ = u_raw.shape
    u = jax.nn.gelu(u_raw)
    vf = jax.nn.gelu(v_raw).astype(jnp.float32)
    mu = jnp.mean(vf, axis=-1, keepdims=True)
    var = jnp.mean(jnp.square(vf - mu), axis=-1, keepdims=True)
    v = ((vf - mu) * lax.rsqrt(var + EPS)).astype(u_raw.dtype)
    v = v.reshape(B, T // SGU_CHUNK, SGU_CHUNK, SGU_GROUPS, SGU_GW)
    s = jnp.einsum('gts,bnsgc->bntgc', w_s, v) + b_s.T[None, None, :, :, None]
    return u * s.reshape(B, T, SGU_W)


def fnet_branch(z):
    B, T, _ = z.shape
    zf = z.astype(jnp.float32).reshape(B, T, FNET_GROUPS, FNET_GW)
    y = jnp.fft.fft2(zf, axes=(1, 3), norm='ortho').real
    return y.reshape(B, T, FNET_W).astype(z.dtype)


def token_mixers(h, w_in, b_in, conv_w, mnorm_g, sgu_w, sgu_b, w_br, w_out, init_fwd, init_bwd, with_output):
    B, T, _ = h.shape
    split_at = np.cumsum(PROJ_SPLITS)[:-1].tolist()
    q, k, v, o, g, su, sv, fz, mg = jnp.split(h @ w_in + b_in, split_at, axis=-1)
    y_m, st_f, st_b = mlstm_branch(q, k, v, o, g, conv_w, mnorm_g, init_fwd, init_bwd)
    if not with_output:
        return None, st_f, st_b
    branches = (y_m, sgu_branch(su, sv, sgu_w, sgu_b), fnet_branch(fz))
    gate = jax.nn.sigmoid(mg).reshape(B, T, N_BRANCH, D_MODEL)
    y = gate[:, :, 0] * (branches[0] @ w_br[0])
    for r in range(1, N_BRANCH):
        y = y + gate[:, :, r] * (branches[r] @ w_br[r])
    return y @ w_out, st_f, st_b


def peer_ffn(h, wq, keys, u_tab, v_tab):
    B, T, D = h.shape
    tokens = h.reshape(-1, PEER_BLOCK, D)

    def block(xb):
        nb = xb.shape[0]
        q = (xb @ wq).reshape(nb, PEER_HEADS, 2, D_SUB)
        s = jnp.einsum('thpc,pkc->thpk', q, keys)
        s_top, i_top = lax.top_k(s, PEER_TOPK)
        n_cand = PEER_TOPK * PEER_TOPK
        cand_s = (s_top[:, :, 0, :, None] + s_top[:, :, 1, None, :]).reshape(nb, PEER_HEADS, n_cand)
        cand_i = (i_top[:, :, 0, :, None] * N_KEYS + i_top[:, :, 1, None, :]).reshape(nb, PEER_HEADS, n_cand)
        best_s, best_j = lax.top_k(cand_s, PEER_TOPK)
        idx = jnp.take_along_axis(cand_i, best_j, axis=-1)
        w = jax.nn.softmax(best_s.astype(jnp.float32), axis=-1)
        u = jnp.take(u_tab, idx, axis=0)
        act = jax.nn.gelu(jnp.einsum('thkd,td->thk', u, xb).astype(jnp.float32)) * w
        v = jnp.take(v_tab, idx, axis=0)
        return jnp.einsum('thk,thkd->td', act.astype(xb.dtype), v)

    return lax.map(block, tokens).reshape(B, T, D)


def setup_inputs(seed: int = 0) -> dict:
    key = jax.random.key(seed)
    ks = jax.random.split(key, 24)
    nrm = lambda k, shape, s: jax.random.normal(k, shape, jnp.float32) * s
    D = D_MODEL
    x = nrm(ks[0], (BATCH, SEQ, D), 1.0)
    c = nrm(ks[1], (BATCH, D), 1.0)
    ctx = nrm(ks[2], (BATCH, CTX_LEN, D), 1.0)
    c_ctx = nrm(ks[3], (D,), 1.0)
    w_mod = nrm(ks[4], (DEPTH, D, N_MOD * D), 0.5 * D ** -0.5)
    b_mod = nrm(ks[5], (DEPTH, N_MOD * D), 0.02)
    norm1_g = 1.0 + nrm(ks[6], (DEPTH, D), 0.05)
    norm2_g = 1.0 + nrm(ks[7], (DEPTH, D), 0.05)
    w_in = nrm(ks[8], (DEPTH, D, PROJ_W), D ** -0.5)
    fbias = jnp.linspace(3.0, 6.0, MLSTM_HEADS, dtype=jnp.float32)
    b_in = nrm(ks[9], (DEPTH, PROJ_W), 0.02)
    b_in = b_in.at[:, GATE_OFF + MLSTM_HEADS:GATE_OFF + 2 * MLSTM_HEADS].add(fbias)
    b_in = b_in.at[:, GATE_OFF + 3 * MLSTM_HEADS:GATE_OFF + 4 * MLSTM_HEADS].add(fbias)
    conv_qk = nrm(ks[10], (DEPTH, QK_CONV, 2 * MLSTM_W), QK_CONV ** -0.5)
    mlstm_norm_g = 1.0 + nrm(ks[11], (DEPTH, MLSTM_W), 0.05)
    sgu_w = nrm(ks[12], (DEPTH, SGU_GROUPS, SGU_CHUNK, SGU_CHUNK), SGU_CHUNK ** -0.5)
    sgu_b = 1.0 + nrm(ks[13], (DEPTH, SGU_GROUPS, SGU_CHUNK), 0.05)
    w_br = nrm(ks[14], (DEPTH, N_BRANCH, BRANCH_W, D), BRANCH_W ** -0.5)
    w_out = nrm(ks[15], (DEPTH, D, D), D ** -0.5)
    peer_wq = nrm(ks[16], (DEPTH, D, PEER_HEADS * D_QUERY), D ** -0.5)
    peer_keys = nrm(ks[17], (DEPTH, 2, N_KEYS, D_SUB), D_SUB ** -0.5)
    peer_u = nrm(ks[18], (DEPTH, N_EXPERTS, D), D ** -0.5)
    peer_v = nrm(ks[19], (DEPTH, N_EXPERTS, D), PEER_HEADS ** -0.5)
    final_g = 1.0 + nrm(ks[20], (D,), 0.05)
    return {'x': x, 'c': c, 'ctx': ctx, 'c_ctx': c_ctx, 'w_mod': w_mod, 'b_mod': b_mod,
            'norm1_g': norm1_g, 'norm2_g': norm2_g, 'w_in': w_in, 'b_in': b_in, 'conv_qk': conv_qk,
            'mlstm_norm_g': mlstm_norm_g, 'sgu_w': sgu_w, 'sgu_b': sgu_b, 'w_br': w_br, 'w_out': w_out,
            'peer_wq': peer_wq, 'peer_keys': peer_keys, 'peer_u': peer_u, 'peer_v': peer_v, 'final_g': final_g}


def reference(x, c, ctx, c_ctx, w_mod, b_mod, norm1_g, norm2_g, w_in, b_in, conv_qk, mlstm_norm_g, sgu_w, sgu_b, w_br, w_out, peer_wq, peer_keys, peer_u, peer_v, final_g):
    B, T, D = x.shape
    rows = T // GRID_W
    x = x + pos_embed_2d(rows, x.dtype)[None]
    silu_c = jax.nn.silu(c)
    silu_cc = jax.nn.silu(c_ctx)
    zero = mlstm_zero_state(B)
    for l in range(DEPTH):
        last = l == DEPTH - 1
        mod_x = jnp.split((silu_c @ w_mod[l] + b_mod[l])[:, None, :], N_MOD, axis=-1)
        mod_c = jnp.split(silu_cc @ w_mod[l] + b_mod[l], N_MOD, axis=-1)
        mixer_w = (w_in[l], b_in[l], conv_qk[l], mlstm_norm_g[l], sgu_w[l], sgu_b[l], w_br[l], w_out[l])
        peer_w = (peer_wq[l], peer_keys[l], peer_u[l], peer_v[l])
        hc = modulate(rmsnorm(ctx, norm1_g[l]), mod_c[0], mod_c[1])
        yc, ctx_fwd, ctx_bwd = token_mixers(hc, *mixer_w, zero, zero, not last)
        hx = modulate(rmsnorm(x, norm1_g[l]), mod_x[0], mod_x[1])
        yx, _, _ = token_mixers(hx, *mixer_w, ctx_fwd, ctx_bwd, True)
        x = x + mod_x[2] * yx
        x = x + mod_x[5] * peer_ffn(modulate(rmsnorm(x, norm2_g[l]), mod_x[3], mod_x[4]), *peer_w)
        if not last:
            ctx = ctx + mod_c[2] * yc
            ctx = ctx + mod_c[5] * peer_ffn(modulate(rmsnorm(ctx, norm2_g[l]), mod_c[3], mod_c[4]), *peer_w)
    return rmsnorm(x, final_g)
```

```python
import math
import numpy as np
from contextlib import ExitStack
import concourse.bass as bass
import concourse.mybir as mybir
from concourse.bass_utils import run_bass_kernel_spmd

F32 = mybir.dt.float32
BF16 = mybir.dt.bfloat16
I32 = mybir.dt.int32
U32 = mybir.dt.uint32
AF = mybir.ActivationFunctionType
ALU = mybir.AluOpType
AX = mybir.AxisListType
ENGS = ("sp", "act", "dve", "pool", "pe")

D = 1024
PW = 6672
OQ, OK_, OV, OO, OG, OSU, OSV, OFZ, OMG = 0, 512, 1024, 1536, 2048, 2064, 2576, 3088, 3600
EPS = 1e-6
NEXP = 16384
NSLOT = 24


class Tile:
    def __init__(self, t, key):
        self.t = t
        self.key = key

    def __getitem__(self, idx):
        return self.t[idx]


class Prog:
    def __init__(self, nc, csem, dsem):
        self.nc = nc
        self.csem, self.dsem = csem, dsem
        self.streams = {e: [] for e in ENGS}
        self.ccount = {e: 0 for e in ENGS}
        self.dcount = {e: 0 for e in ENGS}
        self.waited = {e: {} for e in ENGS}
        self.lastw = {}
        self.reads = {}

    def _issue(self, eng, fn, reads, writes, is_dma):
        reads = [getattr(r, "key", r) for r in reads]
        writes = [getattr(w, "key", w) for w in writes]
        deps = {}
        def add(k, n):
            if deps.get(k, 0) < n:
                deps[k] = n
        for r in reads:
            w = self.lastw.get(r)
            if w: add((w[0], w[1]), w[2])
        for wkey in writes:
            w = self.lastw.get(wkey)
            if w: add((w[0], w[1]), w[2])
            for k, n in self.reads.get(wkey, {}).items():
                add(k, n)
        waits = []
        wd = self.waited[eng]
        for k, n in deps.items():
            if k == ("c", "pe") and eng == "pe" and not is_dma:
                continue
            if wd.get(k, 0) >= n:
                continue
            wd[k] = n
            waits.append((k[0], k[1], n))
        slot = None
        if is_dma:
            self.dcount[eng] += 1
            n = self.dcount[eng]
            slot = (n - 1) % NSLOT
            me = ("d", (eng, slot), (n - 1) // NSLOT + 1)
        else:
            self.ccount[eng] += 1
            me = ("c", eng, self.ccount[eng])
        self.streams[eng].append((waits, fn, is_dma, slot))
        for r in reads:
            self.reads.setdefault(r, {})[(me[0], me[1])] = me[2]
        for w in writes:
            self.lastw[w] = me
            self.reads[w] = {}

    def op(self, eng, fn, R=(), W=()):
        self._issue(eng, fn, R, W, False)

    def dma(self, eng, out, in_, R=(), W=(), **kw):
        self._issue(eng, lambda e: e.dma_start(out=out, in_=in_, **kw), R, W, True)

    def gather(self, out, table, idx_ap, R=(), W=()):
        self._issue("pool", lambda e: e.indirect_dma_start(
            out=out, out_offset=None, in_=table,
            in_offset=bass.IndirectOffsetOnAxis(ap=idx_ap, axis=0)), R, W, True)

    def act(self, out, in_, func, R, W, eng="act", **kw):
        self.op(eng, lambda e: e.activation(out=out, in_=in_, func=func, **kw), R, W)

    def tt(self, out, in0, in1, op, R, W, eng="dve"):
        self.op(eng, lambda e: e.tensor_tensor(out=out, in0=in0, in1=in1, op=op), R, W)

    def ts(self, out, in0, s1, s2, op0, op1, R, W, eng="dve"):
        if s2 is None:
            self.op(eng, lambda e: e.tensor_scalar(out=out, in0=in0, scalar1=s1, scalar2=None, op0=op0), R, W)
        else:
            self.op(eng, lambda e: e.tensor_scalar(out=out, in0=in0, scalar1=s1, scalar2=s2, op0=op0, op1=op1), R, W)

    def stt(self, out, in0, scalar, in1, op0, op1, R, W, eng="dve"):
        self.op(eng, lambda e: e.scalar_tensor_tensor(out=out, in0=in0, scalar=scalar, in1=in1, op0=op0, op1=op1), R, W)

    def cp(self, out, in_, R, W, eng="dve"):
        self.op(eng, lambda e: e.tensor_copy(out=out, in_=in_), R, W)

    def red(self, out, in_, op, R, W, eng="dve", axis=AX.X):
        self.op(eng, lambda e: e.tensor_reduce(out=out, in_=in_, axis=axis, op=op), R, W)

    def mm(self, out, lhsT, rhs, start, stop, R, W):
        self.op("pe", lambda e: e.matmul(out, lhsT=lhsT, rhs=rhs, start=start, stop=stop), R, W)

    def tr(self, out, in_, ident, R, W):
        self.op("pe", lambda e: e.transpose(out, in_, ident), R, W)

    def memset(self, ap, val, W, eng="pool"):
        self.op(eng, lambda e: e.memset(ap, val), (), W)

    def barrier(self):
        for e in ENGS:
            waits = []
            wd = self.waited[e]
            for E in ENGS:
                if self.ccount[E] and not (E == e) and wd.get(("c", E), 0) < self.ccount[E]:
                    wd[("c", E)] = self.ccount[E]
                    waits.append(("c", E, self.ccount[E]))
                n = self.dcount[E]
                for slot in range(min(n, NSLOT)):
                    cnt = (n - 1 - slot) // NSLOT + 1
                    if wd.get(("d", (E, slot)), 0) < cnt:
                        wd[("d", (E, slot))] = cnt
                        waits.append(("d", (E, slot), cnt))
            if waits:
                self.streams[e].append((waits, None, False, None))

    def emit(self, final=False):
        nc = self.nc
        csem, dsem = self.csem, self.dsem
        if final:
            self.barrier()
        with nc.Block() as block:
            engobj = {"sp": block.sync, "act": block.scalar, "dve": block.vector,
                      "pool": block.gpsimd, "pe": block.tensor}

            def mk(ename):
                stream = self.streams[ename]
                def body(eng):
                    for waits, fn, is_dma, slot in stream:
                        for kind, E, n in waits:
                            if kind == "c":
                                eng.wait_ge(csem[E], n)
                            else:
                                eng.wait_ge(dsem[E[0]][E[1]], 16 * n)
                        if fn is None:
                            continue
                        inst = fn(eng)
                        if is_dma:
                            inst.then_inc(dsem[ename][slot], 16)
                        else:
                            inst.then_inc(csem[ename], 1)
                return body
            for e in ENGS:
                if self.streams[e]:
                    engobj[e](mk(e))
        self.streams = {e: [] for e in ENGS}


def _consts(T):
    N1 = T // 128
    n2 = np.arange(128)[:, None].astype(np.float64)
    k2 = np.arange(128)[None, :].astype(np.float64)
    gx = np.zeros((N1, 128, 3, 128), np.float32)
    for n1 in range(N1):
        ang = 2 * np.pi * (((N1 * n2 + n1) * k2) % T) / T
        gx[n1, :, 0, :] = np.cos(ang)
        gx[n1, :, 1, :] = np.sin(ang)
        gx[n1, :, 2, :] = -np.sin(ang)
    a = np.arange(N1)[:, None].astype(np.float64)
    b = np.arange(N1)[None, :].astype(np.float64)
    ang = 2 * np.pi * ((a * b) % N1) / N1
    fs = np.concatenate([np.cos(ang), np.sin(ang)], axis=0) / np.sqrt(T * 128.0)
    return gx, fs.astype(np.float32)


def _pos_embed(T):
    quarter = D // 4
    omega = (1.0 / (10000.0 ** (np.arange(quarter, dtype=np.float32) / np.float32(quarter)))).astype(np.float32)
    rows = T // 64
    r = np.repeat(np.arange(rows, dtype=np.float32), 64)[:, None] * omega
    cc = np.tile(np.arange(64, dtype=np.float32), rows)[:, None] * omega
    return np.concatenate([np.sin(r), np.cos(r), np.sin(cc), np.cos(cc)], axis=-1).astype(np.float32)


def build(T, CT, L, dbg=()):
    TT = T + CT
    NT = TT // 128
    NCT = CT // 128
    nc = bass.Bass("TRN2", target_bir_lowering=False)

    def din(name, shape, dt=F32):
        return nc.dram_tensor(name, list(shape), dt, kind="ExternalInput").ap()

    def dscr(name, shape, dt=F32):
        kind = "ExternalOutput" if name in dbg else "Internal"
        return nc.dram_tensor(name, list(shape), dt, kind=kind).ap()

    x_in = din("x", [T, D]); pe = din("pe", [T, D]); ctx_in = din("ctx", [CT, D]); cc = din("cc", [2, D])
    w_mod = din("w_mod", [L, D, 6 * D]); b_mod = din("b_mod", [L, 6 * D])
    norm1_g = din("norm1_g", [L, D]); norm2_g = din("norm2_g", [L, D])
    w_in = din("w_in", [L, D, PW]); b_in = din("b_in", [L, PW])
    conv_qk = din("conv_qk", [L, 3, 1024]); mng = din("mlstm_norm_g", [L, 512])
    sgu_w = din("sgu_w", [L, 4, 128, 128]); sgu_b = din("sgu_b", [L, 512])
    w_br = din("w_br", [L, 3, 512, D]); w_out = din("w_out", [L, D, D])
    peer_wq = din("peer_wq", [L, D, 2048]); peer_keys = din("peer_keys", [L, 2, 128, 128])
    peer_u = din("peer_u", [L, NEXP, D]); peer_v = din("peer_v", [L, NEXP, D])
    final_g = din("final_g", [1, D])
    cdft = din("cdft", [128, 256])
    gx_x = din("gx_x", [T // 128, 128, 3, 128]); fs_x = din("fs_x", [2 * (T // 128), T // 128])
    gx_c = din("gx_c", [CT // 128, 128, 3, 128]); fs_c = din("fs_c", [2 * (CT // 128), CT // 128])
    out = nc.dram_tensor("out", [T, D], F32, kind="ExternalOutput").ap()

    XS = dscr("XS", [TT, D])
    QT = dscr("QT", [512, TT]); KT = dscr("KT", [512, TT]); SUT = dscr("SUT", [512, TT]); FZT = dscr("FZT", [512, TT])
    QC = dscr("QC", [512, TT]); KC = dscr("KC", [512, TT]); KTOK = dscr("KTOK", [TT, 512])
    V = dscr("V", [TT, 512]); O = dscr("O", [TT, 512]); SV = dscr("SV", [TT, 512]); G = dscr("G", [TT, 16])
    MG = dscr("MG", [TT, 3072])
    HF = dscr("HF", [TT, 512]); HB = dscr("HB", [TT, 512])
    B0 = dscr("B0", [TT, 512]); B1T = dscr("B1T", [512, TT]); B2 = dscr("B2", [TT, 512])
    Z = dscr("Z", [2, TT, 512]); APDX = dscr("APD", [2, T // 128, 128, 512]); APDC = dscr("APDC", [2, CT // 128, 128, 512])
    MOD = dscr("MOD", [L, 2, 6 * D])
    UV = dscr("UV", [NEXP, 2, D], BF16)
    IDX = dscr("IDX", [TT, 128], I32); PWT = dscr("PWT", [TT, 128]); HN = dscr("HN", [TT, D])

    es = ExitStack()
    with es:
        es.enter_context(nc.allow_non_contiguous_dma(reason="small strided parameter loads"))
        es.enter_context(nc.allow_low_precision(reason="bf16 matmul operands, fp32 accumulate"))
        csem = {e: es.enter_context(nc.semaphore("c_" + e)) for e in ENGS}
        dsem = {e: [es.enter_context(nc.semaphore(f"d_{e}{i}")) for i in range(NSLOT)] for e in ("sp", "pool")}
        P = Prog(nc, csem, dsem)

        uniq = [0]

        def sbt(st, name, shape, dt=F32):
            uniq[0] += 1
            name = f"{name}_{uniq[0]}"
            return Tile(st.enter_context(nc.sbuf_tensor(name, list(shape), dt)), name)

        class Ring(list):
            def __getitem__(self, i):
                return list.__getitem__(self, i % len(self))

        def ring(st, name, n, shape, dt=F32):
            return Ring([sbt(st, f"{name}{i}", shape, dt) for i in range(n)])

        PS = [Tile(es.enter_context(nc.psum_tensor(f"ps{i}", [128, 512], F32)), f"ps{i}") for i in range(8)]
        ident = sbt(es, "ident", [128, 128]); maskU = sbt(es, "maskU", [128, 128]); maskL = sbt(es, "maskL", [128, 128])
        ones = sbt(es, "ones", [128, 128]); iota16 = sbt(es, "iota16", [128, 16])
        Cf = [sbt(es, f"Cf{d}", [128, 4, 129]) for d in range(2)]
        Cb = [sbt(es, f"Cb{d}", [128, 4, 130], BF16) for d in range(2)]
        cdb = sbt(es, "cdb", [128, 256], BF16)
        identb = sbt(es, "identb", [128, 128], BF16)

        def ktiles(name, t0, t1):
            return [(name, i) for i in range(t0 // 128, (t1 + 127) // 128)]

        with ExitStack() as st:
            P.memset(ident[:], 0.0, [ident]); P.memset(maskU[:], 1.0, [maskU]); P.memset(maskL[:], 1.0, [maskL])
            P.memset(ones[:], 1.0, [ones])
            P.op("pool", lambda e: e.affine_select(out=ident[:], in_=ident[:], pattern=[[-1, 128]], compare_op=ALU.not_equal, fill=1.0, base=0, channel_multiplier=1), [ident], [ident])
            P.op("pool", lambda e: e.affine_select(out=maskU[:], in_=maskU[:], pattern=[[1, 128]], compare_op=ALU.is_ge, fill=0.0, base=0, channel_multiplier=-1), [maskU], [maskU])
            P.op("pool", lambda e: e.affine_select(out=maskL[:], in_=maskL[:], pattern=[[-1, 128]], compare_op=ALU.is_ge, fill=0.0, base=0, channel_multiplier=1), [maskL], [maskL])
            P.op("pool", lambda e: e.iota(iota16[:], pattern=[[1, 16]], base=0, channel_multiplier=0, allow_small_or_imprecise_dtypes=True), (), [iota16])
            P.cp(identb[:], ident[:], [ident], [identb], eng="pool")
            cdf = sbt(st, "cdf", [128, 256])
            P.dma("sp", cdf[:], cdft[:, :], W=[cdf])
            P.cp(cdb[:], cdf[:], [cdf], [cdb])
            xa = ring(st, "xa", 2, [128, D]); xb = ring(st, "xb", 2, [128, D])
            for i in range(NT):
                a = xa[i % 2]; b = xb[i % 2]
                if i < NCT:
                    P.dma("sp", a[:], ctx_in[i * 128:(i + 1) * 128, :], W=[a])
                    P.dma("sp", XS[i * 128:(i + 1) * 128, :], a[:], R=[a], W=[("XS", i)])
                else:
                    j = i - NCT
                    P.dma("sp", a[:], x_in[j * 128:(j + 1) * 128, :], W=[a])
                    P.dma("pool", b[:], pe[j * 128:(j + 1) * 128, :], W=[b])
                    P.tt(a[:], a[:], b[:], ALU.add, [a, b], [a])
                    P.dma("sp", XS[i * 128:(i + 1) * 128, :], a[:], R=[a], W=[("XS", i)])
            P.emit()

        def load_cols(st, name, src_row, eng="sp"):
            t = sbt(st, name, [128, 8])
            P.dma(eng, t[:], src_row.rearrange("(j p) -> p j", p=128), W=[t])
            return t

        def load_bc(st, name, src_row, n, eng="sp"):
            t = sbt(st, name, [128, n])
            P.dma(eng, t[:], src_row.partition_broadcast(128), W=[t])
            return t

        def rsqrt_(ap, keys):
            P.act(ap, ap, AF.Sqrt, keys, keys)
            P.op("dve", lambda e: e.reciprocal(out=ap, in_=ap), keys, keys)

        def rms_rstd(xt, junk, ss, rstd):
            P.act(junk[:], xt[:], AF.Square, [xt], [junk, ss], accum_out=ss[:])
            P.ts(rstd[:], ss[:], 1.0 / D, EPS, ALU.mult, ALU.add, [ss], [rstd])
            rsqrt_(rstd[:], [rstd])

        for l in range(L):
            P.barrier()
            with ExitStack() as st:
                scT = sbt(st, "scT", [128, 2, 8])
                for r in range(2):
                    P.dma("sp", scT[:, r, :], cc[r, :].rearrange("(j p) -> p j", p=128), W=[scT])
                P.act(scT[:], scT[:], AF.Silu, [scT], [scT])
                wm = ring(st, "wm", 2, [128, 8, 512]); bm = ring(st, "bm", 2, [2, 512]); mo = ring(st, "mo", 2, [2, 512])
                for blk in range(12):
                    w = wm[blk % 2]; bb = bm[blk % 2]; m = mo[blk % 2]; ps = PS[blk % 2]
                    P.dma("sp" if blk % 2 == 0 else "pool", w[:], w_mod[l].rearrange("(j p) n -> p j n", p=128)[:, :, blk * 512:(blk + 1) * 512], W=[w])
                    P.dma("sp", bb[:], b_mod[l, blk * 512:(blk + 1) * 512].partition_broadcast(2), W=[bb])
                    for j in range(8):
                        P.mm(ps[0:2, :], scT[:, :, j], w[:, j, :], j == 0, j == 7, [scT, w], [ps])
                    P.tt(m[:], ps[0:2, :], bb[:], ALU.add, [ps, bb], [m])
                    P.dma("sp", MOD[l, :, blk * 512:(blk + 1) * 512], m[:], R=[m], W=["MOD"])
                P.emit()

            def modcols(st, name, m, r):
                return load_cols(st, name, MOD[l, r, m * D:(m + 1) * D])

            def modbc(st, name, m, r):
                return load_bc(st, name, MOD[l, r, m * D:(m + 1) * D], D)

            P.barrier()
            with ExitStack() as st:
                winb = sbt(st, "winb", [128, 8, PW], BF16)
                wst = ring(st, "wst", 2, [128, 8, 512])
                nblk = (PW + 511) // 512
                for blk in range(nblk):
                    c0 = blk * 512; wd = min(512, PW - c0); w = wst[blk % 2]
                    P.dma("sp" if blk % 2 == 0 else "pool", w[:, :, 0:wd], w_in[l].rearrange("(j p) n -> p j n", p=128)[:, :, c0:c0 + wd], W=[w])
                    if blk % 2 == 0:
                        P.cp(winb[:, :, c0:c0 + wd], w[:, :, 0:wd], [w], [("winb", blk)])
                    else:
                        P.act(winb[:, :, c0:c0 + wd], w[:, :, 0:wd], AF.Copy, [w], [("winb", blk)])
                winb_keys = [("winb", b) for b in range(nblk)]
                bbc = load_bc(st, "bbc", b_in[l, :], PW, eng="pool")
                bcol = sbt(st, "bcol", [128, 16])
                for fi, off in enumerate((OQ, OK_, OSU, OFZ)):
                    P.dma("sp", bcol[:, fi * 4:(fi + 1) * 4], b_in[l, off:off + 512].rearrange("(i p) -> p i", p=128), W=[("bcol", fi)])
                bcol_keys = [("bcol", fi) for fi in range(4)]
                g1 = load_cols(st, "g1", norm1_g[l, :])
                Acol = []; Bcol = []
                for r in range(2):
                    sc = modcols(st, f"sc{r}", 1, r); sh = modcols(st, f"sh{r}", 0, r)
                    P.stt(sc[:], sc[:], 1.0, g1[:], ALU.add, ALU.mult, [sc, g1], [sc])
                    Acol.append(sc); Bcol.append(sh)
                xt = ring(st, "xt", 2, [128, D]); xn = ring(st, "xn", 2, [128, D]); ssr = ring(st, "ss", 2, [128, 1]); rsr = ring(st, "rs", 2, [128, 1])
                hT = ring(st, "hT", 2, [128, 8, 128], BF16)
                fo = ring(st, "fo", 4, [128, 4, 128]); to = ring(st, "to", 4, [128, 512]); go = ring(st, "go", 2, [128, 16])
                pcnt = [0]
                def nextps():
                    pcnt[0] += 1
                    return PS[2 + pcnt[0] % 6]
                fcnt = [0]; tcnt = [0]
                for i in range(NT):
                    r = 1 if i < NCT else 0
                    x_ = xt[i % 2]; n_ = xn[i % 2]; ss = ssr[i % 2]; rs = rsr[i % 2]; h_ = hT[i % 2]
                    P.dma("sp", x_[:], XS[i * 128:(i + 1) * 128, :], R=[("XS", i)], W=[x_])
                    rms_rstd(x_, n_, ss, rs)
                    P.ts(n_[:], x_[:], rs[:], None, ALU.mult, None, [x_, rs], [n_])
                    for half in range(2):
                        ps = PS[half]
                        for jj in range(4):
                            j = half * 4 + jj
                            P.tr(ps[:, jj * 128:(jj + 1) * 128], n_[:, j * 128:(j + 1) * 128], ident[:], [n_, ident], [ps])
                        for jj in range(4):
                            j = half * 4 + jj
                            P.act(h_[:, j, :], ps[:, jj * 128:(jj + 1) * 128], AF.Identity, [ps, Acol[r], Bcol[r]], [h_],
                                  scale=Acol[r][:, j:j + 1], bias=Bcol[r][:, j:j + 1])
                    for fi, (off, dst, dname) in enumerate(((OQ, QT, "QT"), (OK_, KT, "KT"), (OSU, SUT, "SUT"), (OFZ, FZT, "FZT"))):
                        ps = nextps(); f_ = fo[fcnt[0] % 4]; fcnt[0] += 1
                        for cb in range(4):
                            for j in range(8):
                                P.mm(ps[:, cb * 128:(cb + 1) * 128], winb[:, j, off + cb * 128: off + (cb + 1) * 128], h_[:, j, :], j == 0, j == 7, [h_] + winb_keys, [ps])
                        for cb in range(4):
                            P.act(f_[:, cb, :], ps[:, cb * 128:(cb + 1) * 128], AF.Identity, [ps] + bcol_keys, [f_], bias=bcol[:, fi * 4 + cb: fi * 4 + cb + 1])
                        P.dma("sp", dst.rearrange("(j c) t -> c j t", c=128)[:, :, i * 128:(i + 1) * 128], f_[:], R=[f_], W=[(dname, i)])
                    tm = [(OV, V, "V", 0), (OO, O, "O", 0), (OSV, SV, "SV", 0)] + [(OMG + 512 * m, MG, "MG", 512 * m) for m in range(6)]
                    for (off, dst, dname, dcol) in tm:
                        ps = nextps(); t_ = to[tcnt[0] % 4]; tcnt[0] += 1
                        for j in range(8):
                            P.mm(ps[:, :], h_[:, j, :], winb[:, j, off:off + 512], j == 0, j == 7, [h_] + winb_keys, [ps])
                        P.tt(t_[:], ps[:, :], bbc[:, off:off + 512], ALU.add, [ps, bbc], [t_])
                        P.dma("pool", dst[i * 128:(i + 1) * 128, dcol:dcol + 512], t_[:], R=[t_], W=[(dname, i, dcol)])
                    ps = nextps(); g_ = go[i % 2]
                    for j in range(8):
                        P.mm(ps[:, 0:16], h_[:, j, :], winb[:, j, OG:OG + 16], j == 0, j == 7, [h_] + winb_keys, [ps])
                    P.tt(g_[:], ps[:, 0:16], bbc[:, OG:OG + 16], ALU.add, [ps, bbc], [g_])
                    P.dma("pool", G[i * 128:(i + 1) * 128, :], g_[:], R=[g_], W=[("G", i)])
                P.emit()

            segs = [(0, CT), (CT, T)]

            P.barrier()
            with ExitStack() as st:
                WP = 1024
                xin = ring(st, "cin", 2, [128, WP + 2]); t1 = ring(st, "ct1", 2, [128, WP]); kt = ring(st, "ckt", 2, [128, WP // 128, 128])
                cw = ring(st, "cw", 2, [128, 3])
                it = 0
                for (src, dst, sname, dname, coff, isk) in ((QT, QC, "QT", "QC", 0, False), (KT, KC, "KT", "KC", 512, True)):
                    for cb in range(4):
                        w_ = cw[(it // 1) % 2]
                        for (s0, sl) in segs:
                            for p0 in range(0, sl, WP):
                                wlen = min(WP, sl - p0)
                                xi = xin[it % 2]; t_ = t1[it % 2]; k_ = kt[it % 2]
                                if p0 == 0 and s0 == 0:
                                    pass
                                a0 = s0 + p0
                                lo = 1 if p0 == 0 else 0
                                hi = 1 if p0 + wlen >= sl else 0
                                if lo:
                                    P.memset(xi[:, 0:1], 0.0, [xi], eng="dve")
                                if hi:
                                    P.memset(xi[:, wlen + 1:wlen + 2], 0.0, [xi], eng="dve")
                                P.dma("sp", xi[:, lo:wlen + 2 - hi], src[cb * 128:(cb + 1) * 128, a0 - 1 + lo:a0 + wlen + 1 - hi],
                                      R=ktiles(sname, a0 - 1 + lo, a0 + wlen + 1 - hi), W=[xi])
                                if True:
                                    P.dma("pool", w_[:], conv_qk[l, :, coff + cb * 128: coff + (cb + 1) * 128].rearrange("k c -> c k"), W=[w_])
                                P.ts(t_[:, 0:wlen], xi[:, 1:wlen + 1], w_[:, 1:2], None, ALU.mult, None, [xi, w_], [t_])
                                P.stt(t_[:, 0:wlen], xi[:, 0:wlen], w_[:, 0:1], t_[:, 0:wlen], ALU.mult, ALU.add, [xi, w_, t_], [t_])
                                P.stt(t_[:, 0:wlen], xi[:, 2:wlen + 2], w_[:, 2:3], t_[:, 0:wlen], ALU.mult, ALU.add, [xi, w_, t_], [t_])
                                P.act(t_[:, 0:wlen], t_[:, 0:wlen], AF.Silu, [t_], [t_])
                                P.dma("sp", dst[cb * 128:(cb + 1) * 128, a0:a0 + wlen], t_[:, 0:wlen], R=[t_], W=ktiles(dname + str(cb), a0, a0 + wlen))
                                if isk:
                                    nch = wlen // 128
                                    for c4 in range(0, nch, 4):
                                        ps = PS[(c4 // 4) % 4]
                                        for q in range(min(4, nch - c4)):
                                            P.tr(ps[:, q * 128:(q + 1) * 128], t_[:, (c4 + q) * 128:(c4 + q + 1) * 128], ident[:], [t_, ident], [ps])
                                        nq = min(4, nch - c4)
                                        P.act(k_[:, c4:c4 + nq, :], ps[:, 0:nq * 128].rearrange("p (q c) -> p q c", c=128), AF.Copy, [ps], [k_])
                                    P.dma("pool", KTOK[a0:a0 + wlen, cb * 128:(cb + 1) * 128].rearrange("(n t) c -> t n c", t=128), k_[:, 0:nch, :], R=[k_], W=ktiles("KTOK" + str(cb), a0, a0 + wlen))
                                it += 1
                P.emit()

            P.barrier()
            with ExitStack() as st:
                from itertools import zip_longest
                gt = ring(st, "gt", 2, [128, 16]); e1 = ring(st, "e1", 2, [128, 4]); sp_ = ring(st, "spl", 2, [128, 4])
                al = ring(st, "al", 2, [128, 4]); be = ring(st, "be", 2, [128, 4]); et = ring(st, "et", 2, [128, 4]); tmp4 = ring(st, "tmp4", 2, [128, 4])
                qf = ring(st, "qf", 2, [128, 4, 128]); kf = ring(st, "kf", 2, [128, 4, 128]); ktk = ring(st, "ktk", 2, [128, 512]); vf = ring(st, "vf", 2, [128, 512])
                qb = ring(st, "qb", 2, [128, 4, 128], BF16); kb = ring(st, "kb", 2, [128, 4, 128], BF16); ktb = ring(st, "ktb", 2, [128, 512], BF16)
                vb = ring(st, "vb", 2, [128, 4, 130], BF16); pt = ring(st, "pt", 2, [128, 4, 128], BF16)
                hh = ring(st, "hh", 2, [128, 4, 128]); d1 = ring(st, "d1", 2, [128, 4])
                lnb = sbt(st, "lnb", [128, 1])
                P.memset(lnb[:], -0.5 * math.log(128.0), [lnb])

                def scan_gen(dr):
                    mask = maskU if dr == 0 else maskL
                    Hd, hname = (HF, "HF") if dr == 0 else (HB, "HB")
                    fcol = 4 if dr == 0 else 12
                    icol = 0 if dr == 0 else 8
                    k = dr
                    order = []
                    for (s0, sl) in segs:
                        ch = list(range(s0 // 128, (s0 + sl) // 128))
                        order.append(ch if dr == 0 else ch[::-1])
                    for si, chs in enumerate(order):
                        if si == 0:
                            P.memset(Cf[dr][:], 0.0, [Cf[dr]], eng="dve")
                            P.memset(Cb[dr][:], 0.0, [Cb[dr]], eng="dve")
                        for c in chs:
                            g_ = gt[k]; e_ = e1[k]; s_ = sp_[k]; a_ = al[k]; b_ = be[k]; t_ = et[k]; m4 = tmp4[k]
                            r0 = c * 128
                            P.dma("sp", g_[:], G[r0:r0 + 128, :], R=[("G", c)], W=[g_])
                            P.act(e_[:], g_[:, fcol:fcol + 4], AF.Exp, [g_], [e_], scale=-1.0)
                            P.act(s_[:], e_[:], AF.Ln, [e_], [s_], bias=1.0)
                            psg = PS[7]
                            P.mm(psg[:, 0:4], mask[:], s_[:], True, True, [mask, s_], [psg])
                            P.mm(psg[:, 4:8], ones[:], s_[:], True, True, [ones, s_], [psg])
                            P.act(a_[:], psg[:, 0:4], AF.Exp, [psg, lnb], [a_], scale=-1.0, bias=lnb[:])
                            P.tt(m4[:], psg[:, 0:4], g_[:, icol:icol + 4], ALU.add, [psg, g_], [m4])
                            P.act(b_[:], m4[:], AF.Exp, [m4], [b_])
                            P.act(t_[:], psg[:, 4:8], AF.Exp, [psg], [t_], scale=-1.0)
                            q_ = qf[k]; k_ = kf[k]; kt_ = ktk[k]; v_ = vf[k]
                            P.dma("sp", q_[:], QC.rearrange("(h d) t -> d h t", d=128)[:, :, r0:r0 + 128], R=[("QC%d" % h, c) for h in range(4)], W=[q_])
                            P.dma("pool", k_[:], KC.rearrange("(h d) t -> d h t", d=128)[:, :, r0:r0 + 128], R=[("KC%d" % h, c) for h in range(4)], W=[k_])
                            P.dma("sp", kt_[:], KTOK[r0:r0 + 128, :], R=[("KTOK%d" % h, c) for h in range(4)], W=[kt_])
                            P.dma("pool", v_[:], V[r0:r0 + 128, :], R=[("V", c, 0)], W=[v_])
                            qb_ = qb[k]; kb_ = kb[k]; ktb_ = ktb[k]; vb_ = vb[k]; pt_ = pt[k]
                            P.cp(qb_[:], q_[:], [q_], [qb_], eng="pool")
                            P.cp(kb_[:], k_[:], [k_], [kb_], eng="pool")
                            P.act(ktb_[:], kt_[:], AF.Copy, [kt_], [ktb_])
                            P.tt(vb_[:, :, 0:128], v_[:].rearrange("p (h e) -> p h e", e=128), b_[:].unsqueeze(2).to_broadcast([128, 4, 128]), ALU.mult, [v_, b_], [vb_])
                            P.cp(vb_[:, :, 128:129], b_[:].unsqueeze(2), [b_], [vb_])
                            pss = PS[0]
                            for h in range(4):
                                P.mm(pss[:, h * 128:(h + 1) * 128], kb_[:, h, :], qb_[:, h, :], True, True, [kb_, qb_], [pss])
                            P.tt(pt_[:], pss[:].rearrange("p (h t) -> p h t", t=128), mask[:].unsqueeze(1).to_broadcast([128, 4, 128]), ALU.mult, [pss, mask], [pt_])
                            acc = [PS[1], PS[2]]; dcp = [PS[3], PS[4]]
                            for h in range(4):
                                a = acc[h // 2]; o_ = (h % 2) * 129
                                P.mm(a[:, o_:o_ + 129], pt_[:, h, :], vb_[:, h, 0:129], True, False, [pt_, vb_], [a])
                                P.mm(a[:, o_:o_ + 129], qb_[:, h, :], Cb[dr][:, h, 0:129], False, True, [qb_, Cb[dr]], [a])
                            for h in range(4):
                                dc = dcp[h // 2]; o_ = (h % 2) * 129
                                P.mm(dc[:, o_:o_ + 129], ktb_[:, h * 128:(h + 1) * 128], vb_[:, h, 0:129], True, True, [ktb_, vb_], [dc])
                            h_ = hh[k]; d_ = d1[k]
                            for hp in range(2):
                                a = acc[hp]; av = a[:, 0:258].rearrange("p (h e) -> p h e", e=129)
                                hs = slice(hp * 2, hp * 2 + 2)
                                P.tt(d_[:, hs], av[:, :, 128], a_[:, hs], ALU.mult, [a, a_], [("d1", k, hp)])
                                P.act(d_[:, hs], d_[:, hs], AF.Abs, [("d1", k, hp)], [("d1", k, hp)])
                                P.ts(d_[:, hs], d_[:, hs], 1.0, None, ALU.max, None, [("d1", k, hp)], [("d1", k, hp)])
                                P.op("dve", (lambda dd: (lambda e: e.reciprocal(out=dd, in_=dd)))(d_[:, hs]), [("d1", k, hp)], [("d1", k, hp)])
                                P.tt(d_[:, hs], d_[:, hs], a_[:, hs], ALU.mult, [("d1", k, hp), a_], [("d1", k, hp)])
                                P.tt(h_[:, hs, :], av[:, :, 0:128], d_[:, hs].unsqueeze(2).to_broadcast([128, 2, 128]), ALU.mult, [a, ("d1", k, hp)], [(h_.key, hp)])
                                dc = dcp[hp]; dv = dc[:, 0:258].rearrange("p (h e) -> p h e", e=129)
                                P.tt(Cf[dr][:, hs, :], dv, Cf[dr][:, hs, :], ALU.add, [dc, Cf[dr]], [Cf[dr]])
                                P.tt(Cf[dr][:, hs, :], Cf[dr][:, hs, :], t_[:, hs].unsqueeze(2).to_broadcast([128, 2, 129]), ALU.mult, [Cf[dr], t_], [Cf[dr]])
                                P.cp(Cb[dr][:, hs, 0:129], Cf[dr][:, hs, :], [Cf[dr]], [Cb[dr]])
                            P.dma("sp", Hd[r0:r0 + 128, :], h_[:].rearrange("p h e -> p (h e)"), R=[(h_.key, 0), (h_.key, 1)], W=[(hname, c)])
                            yield

                ngb = load_bc(st, "ngb", mng[l, :], 512)
                hf = ring(st, "hf", 2, [128, 4, 128]); hb_ = ring(st, "hb", 2, [128, 4, 128]); ot = ring(st, "ot", 2, [128, 512])
                sq = ring(st, "sq", 2, [128, 4, 128]); mu = ring(st, "mu", 2, [128, 4]); vr = ring(st, "vr", 2, [128, 4])

                def l2_gen():
                    for i in range(NT):
                        k = i % 2; a = hf[k]; b = hb_[k]; o_ = ot[k]; s_ = sq[k]; m_ = mu[k]; v_ = vr[k]
                        P.dma("sp", a[:].rearrange("p h e -> p (h e)"), HF[i * 128:(i + 1) * 128, :], R=[("HF", i)], W=[a])
                        P.dma("pool", b[:].rearrange("p h e -> p (h e)"), HB[i * 128:(i + 1) * 128, :], R=[("HB", i)], W=[b])
                        P.dma("sp", o_[:], O[i * 128:(i + 1) * 128, :], R=[("O", i, 0)], W=[o_])
                        P.tt(a[:], a[:], b[:], ALU.add, [a, b], [a])
                        P.red(m_[:], a[:], ALU.add, [a], [m_])
                        P.ts(m_[:], m_[:], 1.0 / 128, None, ALU.mult, None, [m_], [m_])
                        P.tt(a[:], a[:], m_[:].unsqueeze(2).to_broadcast([128, 4, 128]), ALU.subtract, [a, m_], [a])
                        P.tt(s_[:], a[:], a[:], ALU.mult, [a], [s_])
                        P.red(v_[:], s_[:], ALU.add, [s_], [v_])
                        P.ts(v_[:], v_[:], 1.0 / 128, EPS, ALU.mult, ALU.add, [v_], [v_])
                        rsqrt_(v_[:], [v_])
                        P.tt(a[:], a[:], v_[:].unsqueeze(2).to_broadcast([128, 4, 128]), ALU.mult, [a, v_], [a])
                        P.act(o_[:], o_[:], AF.Sigmoid, [o_], [o_])
                        P.tt(o_[:], o_[:], ngb[:], ALU.mult, [o_, ngb], [o_])
                        P.tt(o_[:], o_[:], a[:].rearrange("p h e -> p (h e)"), ALU.mult, [o_, a], [o_])
                        P.dma("sp", B0[i * 128:(i + 1) * 128, :], o_[:], R=[o_], W=[("B0", i)])
                        yield

                wsn = sbt(st, "wsn", [128, 4, 128]); wsT = sbt(st, "wsT", [128, 4, 128], BF16)
                P.dma("sp", wsn[:], sgu_w[l].rearrange("g t s -> t g s"), W=[wsn])
                for g in range(4):
                    P.tr(PS[5][:, g * 128:(g + 1) * 128], wsn[:, g, :], ident[:], [wsn, ident], [PS[5]])
                P.cp(wsT[:].rearrange("p g t -> p (g t)"), PS[5][:, :], [PS[5]], [wsT])
                bsT = load_bc(st, "bsT", sgu_b[l, :], 512)
                sv = ring(st, "sv", 2, [128, 512]); su = ring(st, "su", 2, [128, 4, 128]); vn = ring(st, "vn", 2, [128, 512], BF16)
                sq2 = ring(st, "sq2", 2, [128, 512]); mu2 = ring(st, "mu2", 2, [128, 1]); vr2 = ring(st, "vr2", 2, [128, 1]); b1 = ring(st, "b1", 2, [128, 4, 128])

                def sgu_gen():
                    for i in range(NT):
                        k = i % 2; v_ = sv[k]; u_ = su[k]; n_ = vn[k]; s_ = sq2[k]; m_ = mu2[k]; r_ = vr2[k]; o_ = b1[k]
                        P.dma("sp", v_[:], SV[i * 128:(i + 1) * 128, :], R=[("SV", i, 0)], W=[v_])
                        P.dma("pool", u_[:], SUT.rearrange("(g c) t -> c g t", c=128)[:, :, i * 128:(i + 1) * 128], R=[("SUT", i)], W=[u_])
                        P.act(v_[:], v_[:], AF.Gelu, [v_], [v_])
                        P.act(u_[:], u_[:], AF.Gelu, [u_], [u_])
                        P.red(m_[:], v_[:], ALU.add, [v_], [m_])
                        P.ts(m_[:], m_[:], 1.0 / 512, None, ALU.mult, None, [m_], [m_])
                        P.ts(v_[:], v_[:], m_[:], None, ALU.subtract, None, [v_, m_], [v_])
                        P.tt(s_[:], v_[:], v_[:], ALU.mult, [v_], [s_])
                        P.red(r_[:], s_[:], ALU.add, [s_], [r_])
                        P.ts(r_[:], r_[:], 1.0 / 512, EPS, ALU.mult, ALU.add, [r_], [r_])
                        rsqrt_(r_[:], [r_])
                        P.ts(n_[:], v_[:], r_[:], None, ALU.mult, None, [v_, r_], [n_])
                        ps = PS[5]
                        for g in range(4):
                            P.mm(ps[:, g * 128:(g + 1) * 128], n_[:, g * 128:(g + 1) * 128], wsT[:, g, :], True, True, [n_, wsT], [ps])
                        P.tt(s_[:], ps[:, :], bsT[:], ALU.add, [ps, bsT], [s_])
                        P.tt(o_[:].rearrange("p g t -> p (g t)"), s_[:], u_[:].rearrange("p g t -> p (g t)"), ALU.mult, [s_, u_], [o_])
                        P.dma("sp", B1T.rearrange("(g c) t -> c g t", c=128)[:, :, i * 128:(i + 1) * 128], o_[:], R=[o_], W=[("B1T", i)])
                        yield

                fz = ring(st, "fz", 2, [128, 4, 128]); fzb = ring(st, "fzb", 2, [128, 4, 128], BF16); zt = ring(st, "zt", 2, [128, 2, 4, 128])
                gf = ring(st, "gf", 2, [128, 3, 128]); gb = ring(st, "gb", 2, [128, 3, 128], BF16)
                zr = ring(st, "zr", 2, [128, 2, 512]); zb = ring(st, "zb", 2, [128, 2, 512], BF16); ao = ring(st, "ao", 2, [128, 2, 512])
                KB = 4
                rt = ring(st, "rt", 2, [128, KB, 512]); rb = ring(st, "rb", 2, [128, KB, 512], BF16); ob = ring(st, "ob", 2, [128, KB, 512])
                fstiles = {}
                for (s0, sl), fsd in ((segs[0], fs_c), (segs[1], fs_x)):
                    N1 = sl // 128
                    fsf = sbt(st, "fsf", [128, N1]); fsb = sbt(st, "fsb", [128, N1], BF16)
                    P.memset(fsf[:], 0.0, [fsf], eng="dve")
                    P.dma("sp", fsf[0:2 * N1, :], fsd[:, :], R=[fsf], W=[fsf])
                    P.cp(fsb[:], fsf[:], [fsf], [fsb])
                    fstiles[s0] = fsb

                def fnet_gen():
                    for i in range(NT):
                        k = i % 2; f_ = fz[k]; fb = fzb[k]; z_ = zt[k]
                        P.dma("sp", f_[:], FZT.rearrange("(g c) t -> c g t", c=128)[:, :, i * 128:(i + 1) * 128], R=[("FZT", i)], W=[f_])
                        P.cp(fb[:], f_[:], [f_], [fb])
                        for half in range(2):
                            ps = PS[6]
                            for gg in range(2):
                                g = half * 2 + gg
                                P.mm(ps[:, gg * 256:(gg + 1) * 256], fb[:, g, :], cdb[:], True, True, [fb, cdb], [ps])
                            P.act(z_[:, :, half * 2:half * 2 + 2, :], ps[:, :].rearrange("p (g r c) -> p r g c", r=2, c=128), AF.Copy, [ps], [z_])
                        for r in range(2):
                            P.dma("sp" if r == 0 else "pool", Z[r, i * 128:(i + 1) * 128, :], z_[:, r, :, :].rearrange("p g c -> p (g c)"), R=[z_], W=[("Z", r, i)])
                        yield
                    for (s0, sl), gxd, APD in ((segs[0], gx_c, APDC), (segs[1], gx_x, APDX)):
                        N1 = sl // 128
                        zkeys = [("Z", r, i) for r in range(2) for i in range(s0 // 128, (s0 + sl) // 128)]
                        for n1 in range(N1):
                            k = n1 % 2; g_ = gf[k]; gb_ = gb[k]; z_ = zr[k]; zb_ = zb[k]; a_ = ao[k]
                            P.dma("sp", g_[:], gxd[n1], W=[g_])
                            P.cp(gb_[:], g_[:], [g_], [gb_], eng="pool")
                            for r in range(2):
                                src = Z[r, s0:s0 + sl, :].rearrange("(n2 n1) c -> n1 n2 c", n1=N1)[n1]
                                P.dma("sp" if r == 0 else "pool", z_[:, r, :], src, R=zkeys, W=[z_])
                            P.cp(zb_[:], z_[:], [z_], [zb_])
                            pr = PS[6]; pi = PS[5]
                            P.mm(pr[:, :], gb_[:, 0, :], zb_[:, 0, :], True, False, [gb_, zb_], [pr])
                            P.mm(pr[:, :], gb_[:, 1, :], zb_[:, 1, :], False, True, [gb_, zb_], [pr])
                            P.mm(pi[:, :], gb_[:, 0, :], zb_[:, 1, :], True, False, [gb_, zb_], [pi])
                            P.mm(pi[:, :], gb_[:, 2, :], zb_[:, 0, :], False, True, [gb_, zb_], [pi])
                            P.act(a_[:, 0, :], pr[:, :], AF.Copy, [pr], [a_])
                            P.cp(a_[:, 1, :], pi[:, :], [pi], [a_])
                            for r in range(2):
                                P.dma("sp" if r == 0 else "pool", APD[r, n1, :, :], a_[:, r, :], R=[a_], W=[("APD", s0, r, n1)])
                            yield
                        fsb = fstiles[s0]
                        for r_ in rt:
                            P.memset(r_[:], 0.0, [r_], eng="dve")
                        akeys = [("APD", s0, r, n1) for r in range(2) for n1 in range(N1)]
                        for kb0 in range(0, 128, KB):
                            k = (kb0 // KB) % 2; r_ = rt[k]; rb_ = rb[k]; o_ = ob[k]
                            for r in range(2):
                                P.dma("sp" if r == 0 else "pool", r_[r * N1:(r + 1) * N1, :, :], APD[r, 0:N1, kb0:kb0 + KB, :], R=akeys + [r_], W=[r_])
                            P.cp(rb_[:], r_[:], [r_], [rb_], eng="pool")
                            for kk in range(KB):
                                ps = PS[6] if kk % 2 == 0 else PS[5]
                                P.mm(ps[0:N1, :], fsb[:], rb_[:, kk, :], True, True, [fsb, rb_], [ps])
                                P.act(o_[0:N1, kk, :], ps[0:N1, :], AF.Copy, [ps], [o_])
                            dst = B2[s0:s0 + sl, :].rearrange("(k1 k2) c -> k1 k2 c", k2=128)[:, kb0:kb0 + KB, :]
                            P.dma("sp", dst, o_[0:N1, :, :], R=[o_], W=[("B2", i) for i in range(s0 // 128, (s0 + sl) // 128)])
                            yield

                KR = 2
                tf = ring(st, "tf", 3, [128, KR, D]); tb = ring(st, "tb", 3, [128, KR, D], BF16)

                def conv_gen():
                    it = 0
                    for (src, dst, dn) in ((peer_u, UV[:, 0, :], "UB"), (peer_v, UV[:, 1, :], "VB")):
                        for ch in range(NEXP // (128 * KR)):
                            f_ = tf[it]; b_ = tb[it]
                            r0 = ch * 128 * KR
                            P.dma("sp", f_[:], src[l, r0:r0 + 128 * KR, :].rearrange("(p k) d -> p k d", k=KR), W=[f_])
                            if it % 2 == 0:
                                P.act(b_[:], f_[:], AF.Copy, [f_], [b_])
                            else:
                                P.cp(b_[:], f_[:], [f_], [b_], eng="pool")
                            P.dma("pool", dst[r0:r0 + 128 * KR, :].rearrange("(p k) d -> p k d", k=KR), b_[:], R=[b_], W=[(dn, ch)])
                            it += 1
                            yield

                def scan_both():
                    for _ in zip_longest(scan_gen(0), scan_gen(1)):
                        yield

                def seq(*gens):
                    for g in gens:
                        yield from g

                chains = [seq(scan_both(), l2_gen()), sgu_gen(), fnet_gen(), conv_gen()]
                while chains:
                    alive = []
                    for ch in chains:
                        try:
                            next(ch)
                            alive.append(ch)
                        except StopIteration:
                            pass
                    chains = alive
                P.emit()

            P.barrier()
            with ExitStack() as st:
                wbr = sbt(st, "wbr", [128, 12, D], BF16); wob = sbt(st, "wob", [128, 8, D], BF16)
                wst = ring(st, "wst2", 2, [128, 4, D])
                for r in range(3):
                    w = wst[r % 2]
                    P.dma("sp", w[:], w_br[l, r].rearrange("(j p) n -> p j n", p=128), W=[w])
                    P.cp(wbr[:, r * 4:(r + 1) * 4, :], w[:], [w], [("wbr", r)])
                for hh_ in range(2):
                    w = wst[(3 + hh_) % 2]
                    P.dma("pool", w[:], w_out[l].rearrange("(j p) n -> p j n", p=128)[:, hh_ * 4:(hh_ + 1) * 4, :], W=[w])
                    P.act(wob[:, hh_ * 4:(hh_ + 1) * 4, :], w[:], AF.Copy, [w], [("wob", hh_)])
                wkeys = [("wbr", r) for r in range(3)] + [("wob", h) for h in range(2)]
                g1bc = [modbc(st, f"g1bc{r}", 2, r) for r in range(2)]
                b0 = ring(st, "b0", 2, [128, 512]); b2 = ring(st, "b2", 2, [128, 512]); b1f = ring(st, "b1f", 2, [128, 4, 128])
                bT = ring(st, "bT", 2, [128, 12, 128], BF16); mg = ring(st, "mg", 2, [128, 3072]); y = ring(st, "y", 2, [128, D]); tm = ring(st, "tm", 2, [128, D])
                yT = ring(st, "yT", 2, [128, 8, 128], BF16); xt = ring(st, "xt2", 2, [128, D])
                for i in range(NT):
                    k = i % 2; r = 1 if i < NCT else 0
                    a = b0[k]; c_ = b2[k]; f1 = b1f[k]; bt = bT[k]; m_ = mg[k]; y_ = y[k]; t_ = tm[k]; yt = yT[k]; x_ = xt[k]
                    P.dma("sp", a[:], B0[i * 128:(i + 1) * 128, :], R=[("B0", i)], W=[a])
                    P.dma("pool", c_[:], B2[i * 128:(i + 1) * 128, :], R=[("B2", i)], W=[c_])
                    P.dma("sp", f1[:], B1T.rearrange("(g c) t -> c g t", c=128)[:, :, i * 128:(i + 1) * 128], R=[("B1T", i)], W=[f1])
                    P.dma("pool", m_[:], MG[i * 128:(i + 1) * 128, :], R=[("MG", i, 512 * m) for m in range(6)], W=[m_])
                    P.dma("sp", x_[:], XS[i * 128:(i + 1) * 128, :], R=[("XS", i)], W=[x_])
                    for bi, src in ((0, a), (2, c_)):
                        ps = PS[0 if bi == 0 else 1]
                        for j in range(4):
                            P.tr(ps[:, j * 128:(j + 1) * 128], src[:, j * 128:(j + 1) * 128], ident[:], [src, ident], [ps])
                        P.act(bt[:, bi * 4:(bi + 1) * 4, :], ps[:, :].rearrange("p (j t) -> p j t", t=128), AF.Copy, [ps], [(bt.key, bi)])
                    P.cp(bt[:, 4:8, :], f1[:], [f1], [(bt.key, 1)], eng="pool")
                    P.act(m_[:], m_[:], AF.Sigmoid, [m_], [m_])
                    for rr in range(3):
                        pa, pb = PS[2 + 2 * (rr % 2)], PS[3 + 2 * (rr % 2)]
                        for half, ps in enumerate((pa, pb)):
                            for j in range(4):
                                P.mm(ps[:, :], bt[:, rr * 4 + j, :], wbr[:, rr * 4 + j, half * 512:(half + 1) * 512], j == 0, j == 3, [(bt.key, rr)] + wkeys, [ps])
                        dstt = y_ if rr == 0 else t_
                        for half, ps in enumerate((pa, pb)):
                            P.tt(dstt[:, half * 512:(half + 1) * 512], ps[:, :], m_[:, rr * D + half * 512: rr * D + (half + 1) * 512], ALU.mult, [ps, m_], [(dstt.key, half)])
                        if rr > 0:
                            P.tt(y_[:], y_[:], t_[:], ALU.add, [(y_.key, 0), (y_.key, 1), (t_.key, 0), (t_.key, 1)], [(y_.key, 0), (y_.key, 1)], eng="pool")
                    for half in range(2):
                        ps = PS[6 + half]
                        for jj in range(4):
                            j = half * 4 + jj
                            P.tr(ps[:, jj * 128:(jj + 1) * 128], y_[:, j * 128:(j + 1) * 128], ident[:], [(y_.key, 0), (y_.key, 1), ident], [ps])
                        P.act(yt[:, half * 4:(half + 1) * 4, :], ps[:, :].rearrange("p (j t) -> p j t", t=128), AF.Copy, [ps], [(yt.key, half)])
                    for half in range(2):
                        ps = PS[0 + half]
                        for j in range(8):
                            P.mm(ps[:, :], yt[:, j, :], wob[:, j, half * 512:(half + 1) * 512], j == 0, j == 7, [(yt.key, 0), (yt.key, 1)] + wkeys, [ps])
                        P.tt(t_[:, half * 512:(half + 1) * 512], ps[:, :], g1bc[r][:, half * 512:(half + 1) * 512], ALU.mult, [ps, g1bc[r]], [(t_.key, half)])
                    P.tt(x_[:], x_[:], t_[:], ALU.add, [x_, (t_.key, 0), (t_.key, 1)], [x_], eng="pool")
                    P.dma("sp", XS[i * 128:(i + 1) * 128, :], x_[:], R=[x_], W=[("XS", i)])
                P.emit()

            P.barrier()
            with ExitStack() as st:
                wqb = sbt(st, "wqb", [128, 8, 2048], BF16)
                with ExitStack() as st2:
                    wst = ring(st2, "wst3", 2, [128, 8, 256])
                    for blk in range(8):
                        w = wst[blk % 2]
                        P.dma("sp" if blk % 2 == 0 else "pool", w[:], peer_wq[l].rearrange("(j p) n -> p j n", p=128)[:, :, blk * 256:(blk + 1) * 256], W=[w])
                        if blk % 2 == 0:
                            P.cp(wqb[:, :, blk * 256:(blk + 1) * 256], w[:], [w], [("wqb", blk)])
                        else:
                            P.act(wqb[:, :, blk * 256:(blk + 1) * 256], w[:], AF.Copy, [w], [("wqb", blk)])
                    P.emit()
                P.barrier()
                wqkeys = [("wqb", b) for b in range(8)]
                kn = sbt(st, "kn", [128, 2, 128]); kTb = sbt(st, "kTb", [128, 2, 128], BF16)
                P.dma("sp", kn[:], peer_keys[l].rearrange("p k c -> k p c"), W=[kn])
                for p in range(2):
                    P.tr(PS[0][:, p * 128:(p + 1) * 128], kn[:, p, :], ident[:], [kn, ident], [PS[0]])
                P.cp(kTb[:].rearrange("c p k -> c (p k)"), PS[0][:, 0:256], [PS[0]], [kTb])
                g2 = load_bc(st, "g2", norm2_g[l, :], D)
                A2t = sbt(st, "A2t", [128, D]); B2t = sbt(st, "B2t", [128, D]); g2bct = sbt(st, "g2bct", [128, D])

                def load_mods(r):
                    P.dma("sp", A2t[:], MOD[l, r, 4 * D:5 * D].partition_broadcast(128), W=[A2t])
                    P.dma("sp", B2t[:], MOD[l, r, 3 * D:4 * D].partition_broadcast(128), W=[B2t])
                    P.dma("sp", g2bct[:], MOD[l, r, 5 * D:6 * D].partition_broadcast(128), W=[g2bct])
                    P.stt(A2t[:], A2t[:], 1.0, g2[:], ALU.add, ALU.mult, [A2t, g2], [A2t])

                xt = ring(st, "xt3", 2, [128, D]); hn = ring(st, "hn", 2, [128, D]); ssr = ring(st, "ss3", 2, [128, 1]); rsr = ring(st, "rs3", 2, [128, 1])
                hT = ring(st, "hT3", 1, [128, 8, 128], BF16); qTb = ring(st, "qTb", 1, [128, 16, 128], BF16)
                sc_ = ring(st, "scr", 1, [128, 16, 128]); wk = ring(st, "wk", 1, [128, 16, 128])
                tv = ring(st, "tv", 2, [128, 16, 16]); ti = ring(st, "ti", 2, [128, 16, 16], U32); tif = ring(st, "tif", 1, [128, 16, 16])
                cs = ring(st, "cs", 1, [128, 8, 256])
                bv = ring(st, "bv", 2, [128, 8, 16]); bj = ring(st, "bj", 2, [128, 8, 16], U32); ba = ring(st, "ba", 2, [128, 8, 16], U32); bbq = ring(st, "bbq", 2, [128, 8, 16], U32)
                baf = ring(st, "baf", 2, [128, 8, 16]); bbf = ring(st, "bbf", 2, [128, 8, 16])
                oh = ring(st, "oh", 1, [128, 8, 16, 16]); i1s = ring(st, "i1s", 2, [128, 8, 16]); i2s = ring(st, "i2s", 2, [128, 8, 16])
                ei = ring(st, "ei", 2, [128, 128], I32); mx = ring(st, "mx", 2, [128, 8]); pw = ring(st, "pw", 2, [128, 8, 16]); sm = ring(st, "sm", 2, [128, 8])
                RC = 8
                ug = ring(st, "ug", 2, [128, RC, 2 * D], BF16)
                ux = ring(st, "ux", 4, [128, RC]); acc = ring(st, "acc", 1, [128, D]); junk = sbt(st, "junk", [128, D], BF16)
                dg = ring(st, "dg", 2, [128, RC, 128], BF16)
                UVf = UV.rearrange("e two d -> e (two d)")
                cnt = {"uc": 0, "jc": 0}

                def top16(vals, work, outv, outi, rk, wk_, ok):
                    P.op("dve", lambda e: e.max(out=outv[:, 0:8], in_=vals), rk, ok)
                    P.op("dve", lambda e: e.match_replace(out=work, in_to_replace=outv[:, 0:8], in_values=vals, imm_value=-1e30), rk + ok, wk_)
                    P.op("dve", lambda e: e.max(out=outv[:, 8:16], in_=work), wk_, ok)
                    P.op("dve", lambda e: e.max_index(out=outi[:, 0:8], in_max=outv[:, 0:8], in_values=vals), rk + ok, ok)
                    P.op("dve", lambda e: e.max_index(out=outi[:, 8:16], in_max=outv[:, 8:16], in_values=vals), rk + ok, ok)

                def p1_tile(i):
                    k = i % 2
                    x_ = xt[k]; n_ = hn[k]; ss = ssr[k]; rs = rsr[k]; h_ = hT[k]; q_ = qTb[k]; s_ = sc_[k]; w_ = wk[k]
                    tv_ = tv[k]; ti_ = ti[k]; tf_ = tif[k]; cs_ = cs[k]; bv_ = bv[k]; bj_ = bj[k]; ba_ = ba[k]; bb_ = bbq[k]
                    P.dma("sp", x_[:], XS[i * 128:(i + 1) * 128, :], R=[("XS", i)], W=[x_])
                    rms_rstd(x_, n_, ss, rs)
                    P.ts(n_[:], x_[:], rs[:], None, ALU.mult, None, [x_, rs], [n_])
                    P.tt(n_[:], n_[:], A2t[:], ALU.mult, [n_, A2t], [n_])
                    P.tt(n_[:], n_[:], B2t[:], ALU.add, [n_, B2t], [n_])
                    for half in range(2):
                        ps = PS[half]
                        for jj in range(4):
                            j = half * 4 + jj
                            P.tr(ps[:, jj * 128:(jj + 1) * 128], n_[:, j * 128:(j + 1) * 128], ident[:], [n_, ident], [ps])
                        P.act(h_[:, half * 4:(half + 1) * 4, :], ps[:, :].rearrange("p (j t) -> p j t", t=128), AF.Copy, [ps], [(h_.key, half)])
                    yield
                    for g4 in range(4):
                        ps = PS[2 + g4]
                        for gg in range(4):
                            grp = g4 * 4 + gg
                            for j in range(8):
                                P.mm(ps[:, gg * 128:(gg + 1) * 128], wqb[:, j, grp * 128:(grp + 1) * 128], h_[:, j, :], j == 0, j == 7, [(h_.key, 0), (h_.key, 1)] + wqkeys, [ps])
                        P.act(q_[:, g4 * 4:(g4 + 1) * 4, :], ps[:, :].rearrange("p (g t) -> p g t", t=128), AF.Copy, [ps], [(q_.key, g4)])
                        if g4 % 2 == 1:
                            yield
                    for g4 in range(4):
                        ps = PS[g4]
                        for gg in range(4):
                            grp = g4 * 4 + gg
                            P.mm(ps[:, gg * 128:(gg + 1) * 128], q_[:, grp, :], kTb[:, grp % 2, :], True, True, [(q_.key, g4), kTb], [ps])
                        P.act(s_[:, g4 * 4:(g4 + 1) * 4, :], ps[:, :].rearrange("p (g t) -> p g t", t=128), AF.Copy, [ps], [(s_.key, g4)])
                    yield
                    for grp in range(16):
                        top16(s_[:, grp, :], w_[:, grp, :], tv_[:, grp, :], ti_[:, grp, :], [(s_.key, grp // 4)], [(w_.key, grp)], [(tv_.key, grp)])
                        if grp % 2 == 1:
                            yield
                    tvk = [(tv_.key, g) for g in range(16)]
                    P.cp(tf_[:], ti_[:], tvk, [tf_])
                    tv4 = tv_[:].rearrange("p (h q) a -> p h q a", q=2)
                    P.tt(cs_[:].rearrange("p h (a b) -> p h a b", b=16), tv4[:, :, 0, :].unsqueeze(3).to_broadcast([128, 8, 16, 16]),
                         tv4[:, :, 1, :].unsqueeze(2).to_broadcast([128, 8, 16, 16]), ALU.add, tvk, [cs_])
                    cwv = w_[:].rearrange("p (h a) b -> p h (a b)", a=2)
                    wall = [(w_.key, g) for g in range(16)]
                    for h in range(8):
                        top16(cs_[:, h, :], cwv[:, h, :], bv_[:, h, :], bj_[:, h, :], [cs_], wall, [(bv_.key, h)])
                        if h % 2 == 1:
                            yield
                    bvk = [(bv_.key, h) for h in range(8)]
                    P.ts(ba_[:], bj_[:], 4, None, ALU.logical_shift_right, None, bvk, [ba_])
                    P.ts(bb_[:], bj_[:], 15, None, ALU.bitwise_and, None, bvk, [bb_])
                    af = baf[k]; bf = bbf[k]; oh_ = oh[k]; s1 = i1s[k]; s2 = i2s[k]
                    P.cp(af[:], ba_[:], [ba_], [af]); P.cp(bf[:], bb_[:], [bb_], [bf])
                    tf4 = tf_[:].rearrange("p (h q) a -> p h q a", q=2)
                    io4 = iota16[:].unsqueeze(1).unsqueeze(1).to_broadcast([128, 8, 16, 16])
                    for (sel, src_q, dsts) in ((af, 0, s1), (bf, 1, s2)):
                        P.tt(oh_[:], sel[:].unsqueeze(3).to_broadcast([128, 8, 16, 16]), io4, ALU.is_equal, [sel, iota16], [oh_])
                        P.tt(oh_[:], oh_[:], tf4[:, :, src_q, :].unsqueeze(2).to_broadcast([128, 8, 16, 16]), ALU.mult, [oh_, tf_], [oh_])
                        P.red(dsts[:], oh_[:], ALU.add, [oh_], [dsts])
                    P.stt(s1[:], s1[:], 128.0, s2[:], ALU.mult, ALU.add, [s1, s2], [s1])
                    e_ = ei[k]
                    P.cp(e_[:], s1[:].rearrange("p h k -> p (h k)"), [s1], [e_])
                    m_ = mx[k]; p_ = pw[k]; z_ = sm[k]
                    P.red(m_[:], bv_[:], ALU.max, bvk, [m_])
                    P.tt(p_[:], bv_[:], m_[:].unsqueeze(2).to_broadcast([128, 8, 16]), ALU.subtract, bvk + [m_], [p_])
                    P.act(p_[:], p_[:], AF.Exp, [p_], [p_])
                    P.red(z_[:], p_[:], ALU.add, [p_], [z_])
                    P.op("dve", (lambda zz: (lambda e: e.reciprocal(out=zz, in_=zz)))(z_[:]), [z_], [z_])
                    P.tt(p_[:], p_[:], z_[:].unsqueeze(2).to_broadcast([128, 8, 16]), ALU.mult, [p_, z_], [p_])
                    yield

                def p2_tile(i):
                    k = i % 2
                    ix = ei[k]; p_ = pw[k]; n_ = hn[k]; x_ = xt[k]; a_ = acc[0]
                    pwv = p_[:].rearrange("p h k -> p (h k)")
                    pacc = (PS[6], PS[7])
                    nch = 128 // RC
                    for c in range(nch):
                        uc = cnt["uc"]; cnt["uc"] += 1
                        g_ = ug[uc]; u_ = ux[uc]; d_ = dg[uc]
                        for rr in range(RC):
                            P.gather(g_[:, rr, :], UVf, ix[:, c * RC + rr: c * RC + rr + 1], R=[ix], W=[(g_.key, rr)])
                        P.memset(u_[:], 0.0, [u_], eng="dve")
                        for rr in range(RC):
                            cnt["jc"] += 1
                            P.op("dve", (lambda o, a0, b0, ac: (lambda e: e.scalar_tensor_tensor(out=o, in0=a0, scalar=1.0, in1=b0, op0=ALU.mult, op1=ALU.mult, accum_out=ac)))(junk[:], g_[:, rr, 0:D], n_[:], u_[:, rr:rr + 1]),
                                 [(g_.key, rr), n_, u_], [("junk", cnt["jc"]), (u_.key, rr)])
                        uk = [(u_.key, rr) for rr in range(RC)]
                        P.act(u_[:], u_[:], AF.Gelu, uk + [u_], [u_])
                        P.tt(u_[:], u_[:], pwv[:, c * RC:(c + 1) * RC], ALU.mult, [u_, p_], [u_])
                        for rr in range(RC):
                            P.act(d_[:, rr, :], identb[:], AF.Copy, [identb, u_], [(d_.key, rr)], scale=u_[:, rr:rr + 1])
                        for rr in range(RC):
                            first = (c == 0 and rr == 0); last = (c == nch - 1 and rr == RC - 1)
                            for half in range(2):
                                P.mm(pacc[half][:, :], d_[:, rr, :], g_[:, rr, D + half * 512: D + (half + 1) * 512], first, last, [(d_.key, rr), (g_.key, rr)], [pacc[half]])
                        yield
                    for half in range(2):
                        P.tt(a_[:, half * 512:(half + 1) * 512], pacc[half][:, :], g2bct[:, half * 512:(half + 1) * 512], ALU.mult, [pacc[half], g2bct], [(a_.key, half)])
                    P.tt(x_[:], x_[:], a_[:], ALU.add, [x_, (a_.key, 0), (a_.key, 1)], [x_])
                    P.dma("sp", XS[i * 128:(i + 1) * 128, :], x_[:], R=[x_], W=[("XS", i)])

                from itertools import zip_longest

                def run_pair(g1, g2_):
                    for _ in zip_longest(g1 if g1 is not None else (), g2_ if g2_ is not None else ()):
                        pass

                load_mods(1)
                for i in range(NCT):
                    run_pair(p1_tile(i), p2_tile(i - 1) if i >= 1 else None)
                run_pair(None, p2_tile(NCT - 1))
                load_mods(0)
                for i in range(NCT, NT):
                    run_pair(p1_tile(i), p2_tile(i - 1) if i > NCT else None)
                run_pair(None, p2_tile(NT - 1))
                P.emit()

        P.barrier()
        with ExitStack() as st:
            fg = load_bc(st, "fg", final_g[0, :], D)
            xt = ring(st, "xt5", 2, [128, D]); jn = ring(st, "jn", 2, [128, D]); ssr = ring(st, "ss5", 2, [128, 1]); rsr = ring(st, "rs5", 2, [128, 1])
            for i in range(NCT, NT):
                k = i % 2; x_ = xt[k]; j_ = jn[k]; ss = ssr[k]; rs = rsr[k]
                P.dma("sp", x_[:], XS[i * 128:(i + 1) * 128, :], R=[("XS", i)], W=[x_])
                rms_rstd(x_, j_, ss, rs)
                P.ts(j_[:], x_[:], rs[:], None, ALU.mult, None, [x_, rs], [j_])
                P.tt(j_[:], j_[:], fg[:], ALU.mult, [j_, fg], [j_])
                P.dma("sp", out[(i - NCT) * 128:(i - NCT + 1) * 128, :], j_[:], R=[j_], W=[("out", i)])
            P.emit(final=True)
    return nc


_CACHE = {}


def make_in_maps(inputs, T, CT, L, nb):
    pe = _pos_embed(T)
    gxx, fsx = _consts(T)
    gxc, fsc = _consts(CT)
    cc_ = np.arange(128, dtype=np.float64)
    ang = 2 * np.pi * ((cc_[:, None] * cc_[None, :]) % 128) / 128
    cdft = np.concatenate([np.cos(ang), -np.sin(ang)], axis=1).astype(np.float32)
    f = lambda a: np.ascontiguousarray(np.asarray(a, dtype=np.float32))
    shared = {
        "pe": pe, "w_mod": f(inputs["w_mod"])[:L], "b_mod": f(inputs["b_mod"])[:L],
        "norm1_g": f(inputs["norm1_g"])[:L], "norm2_g": f(inputs["norm2_g"])[:L],
        "w_in": f(inputs["w_in"])[:L], "b_in": f(inputs["b_in"])[:L], "conv_qk": f(inputs["conv_qk"])[:L],
        "mlstm_norm_g": f(inputs["mlstm_norm_g"])[:L], "sgu_w": f(inputs["sgu_w"])[:L],
        "sgu_b": f(inputs["sgu_b"])[:L].reshape(L, 512), "w_br": f(inputs["w_br"])[:L], "w_out": f(inputs["w_out"])[:L],
        "peer_wq": f(inputs["peer_wq"])[:L], "peer_keys": f(inputs["peer_keys"])[:L],
        "peer_u": f(inputs["peer_u"])[:L], "peer_v": f(inputs["peer_v"])[:L],
        "final_g": f(inputs["final_g"]).reshape(1, D), "cdft": cdft,
        "gx_x": gxx, "fs_x": fsx, "gx_c": gxc, "fs_c": fsc,
    }
    x = f(inputs["x"]); c = f(inputs["c"]); ctx = f(inputs["ctx"]); c_ctx = f(inputs["c_ctx"])
    maps = []
    for b in range(nb):
        m = dict(shared)
        m["x"] = x[b]; m["ctx"] = ctx[b]
        m["cc"] = np.ascontiguousarray(np.stack([c[b], c_ctx], axis=0))
        maps.append(m)
    return maps


def kernel(**inputs):
    x = np.asarray(inputs["x"])
    B, T, _ = x.shape
    CT = np.asarray(inputs["ctx"]).shape[1]
    L = np.asarray(inputs["w_mod"]).shape[0]
    key = (T, CT, L)
    if key not in _CACHE:
        _CACHE[key] = build(T, CT, L)
    nc = _CACHE[key]
    maps = make_in_maps(inputs, T, CT, L, B)
    res = run_bass_kernel_spmd(nc, maps, core_ids=list(range(B)))
    return np.stack([res.results[b]["out"] for b in range(B)], axis=0).astype(np.float32)
```

```python
import math
import numpy as np
from contextlib import ExitStack
import concourse.bass as bass
import concourse.mybir as mybir
from concourse.bass_utils import run_bass_kernel_spmd

F32 = mybir.dt.float32
BF16 = mybir.dt.bfloat16
I32 = mybir.dt.int32
U32 = mybir.dt.uint32
AF = mybir.ActivationFunctionType
ALU = mybir.AluOpType
AX = mybir.AxisListType
ENGS = ("sp", "act", "dve", "pool", "pe")

D = 1024
PW = 6672
OQ, OK_, OV, OO, OG, OSU, OSV, OFZ, OMG = 0, 512, 1024, 1536, 2048, 2064, 2576, 3088, 3600
EPS = 1e-6
NEXP = 16384
NSLOT = 24


class Tile:
    def __init__(self, t, key):
        self.t = t
        self.key = key

    def __getitem__(self, idx):
        return self.t[idx]


class Prog:
    def __init__(self, nc, csem, dsem):
        self.nc = nc
        self.csem, self.dsem = csem, dsem
        self.streams = {e: [] for e in ENGS}
        self.ccount = {e: 0 for e in ENGS}
        self.dcount = {e: 0 for e in ENGS}
        self.waited = {e: {} for e in ENGS}
        self.lastw = {}
        self.reads = {}

    def _issue(self, eng, fn, reads, writes, is_dma):
        reads = [getattr(r, "key", r) for r in reads]
        writes = [getattr(w, "key", w) for w in writes]
        deps = {}
        def add(k, n):
            if deps.get(k, 0) < n:
                deps[k] = n
        for r in reads:
            w = self.lastw.get(r)
            if w: add((w[0], w[1]), w[2])
        for wkey in writes:
            w = self.lastw.get(wkey)
            if w: add((w[0], w[1]), w[2])
            for k, n in self.reads.get(wkey, {}).items():
                add(k, n)
        waits = []
        wd = self.waited[eng]
        for k, n in deps.items():
            if k == ("c", "pe") and eng == "pe" and not is_dma:
                continue
            if wd.get(k, 0) >= n:
                continue
            wd[k] = n
            waits.append((k[0], k[1], n))
        slot = None
        if is_dma:
            self.dcount[eng] += 1
            n = self.dcount[eng]
            slot = (n - 1) % NSLOT
            me = ("d", (eng, slot), (n - 1) // NSLOT + 1)
        else:
            self.ccount[eng] += 1
            me = ("c", eng, self.ccount[eng])
        self.streams[eng].append((waits, fn, is_dma, slot))
        for r in reads:
            self.reads.setdefault(r, {})[(me[0], me[1])] = me[2]
        for w in writes:
            self.lastw[w] = me
            self.reads[w] = {}

    def op(self, eng, fn, R=(), W=()):
        self._issue(eng, fn, R, W, False)

    def dma(self, eng, out, in_, R=(), W=(), **kw):
        self._issue(eng, lambda e: e.dma_start(out=out, in_=in_, **kw), R, W, True)

    def gather(self, out, table, idx_ap, R=(), W=()):
        self._issue("pool", lambda e: e.indirect_dma_start(
            out=out, out_offset=None, in_=table,
            in_offset=bass.IndirectOffsetOnAxis(ap=idx_ap, axis=0)), R, W, True)

    def act(self, out, in_, func, R, W, eng="act", **kw):
        self.op(eng, lambda e: e.activation(out=out, in_=in_, func=func, **kw), R, W)

    def tt(self, out, in0, in1, op, R, W, eng="dve"):
        self.op(eng, lambda e: e.tensor_tensor(out=out, in0=in0, in1=in1, op=op), R, W)

    def ts(self, out, in0, s1, s2, op0, op1, R, W, eng="dve"):
        if s2 is None:
            self.op(eng, lambda e: e.tensor_scalar(out=out, in0=in0, scalar1=s1, scalar2=None, op0=op0), R, W)
        else:
            self.op(eng, lambda e: e.tensor_scalar(out=out, in0=in0, scalar1=s1, scalar2=s2, op0=op0, op1=op1), R, W)

    def stt(self, out, in0, scalar, in1, op0, op1, R, W, eng="dve"):
        self.op(eng, lambda e: e.scalar_tensor_tensor(out=out, in0=in0, scalar=scalar, in1=in1, op0=op0, op1=op1), R, W)

    def cp(self, out, in_, R, W, eng="dve"):
        self.op(eng, lambda e: e.tensor_copy(out=out, in_=in_), R, W)

    def red(self, out, in_, op, R, W, eng="dve", axis=AX.X):
        self.op(eng, lambda e: e.tensor_reduce(out=out, in_=in_, axis=axis, op=op), R, W)

    def mm(self, out, lhsT, rhs, start, stop, R, W):
        self.op("pe", lambda e: e.matmul(out, lhsT=lhsT, rhs=rhs, start=start, stop=stop), R, W)

    def tr(self, out, in_, ident, R, W):
        self.op("pe", lambda e: e.transpose(out, in_, ident), R, W)

    def memset(self, ap, val, W, eng="pool"):
        self.op(eng, lambda e: e.memset(ap, val), (), W)

    def barrier(self):
        for e in ENGS:
            waits = []
            wd = self.waited[e]
            for E in ENGS:
                if self.ccount[E] and not (E == e) and wd.get(("c", E), 0) < self.ccount[E]:
                    wd[("c", E)] = self.ccount[E]
                    waits.append(("c", E, self.ccount[E]))
                n = self.dcount[E]
                for slot in range(min(n, NSLOT)):
                    cnt = (n - 1 - slot) // NSLOT + 1
                    if wd.get(("d", (E, slot)), 0) < cnt:
                        wd[("d", (E, slot))] = cnt
                        waits.append(("d", (E, slot), cnt))
            if waits:
                self.streams[e].append((waits, None, False, None))

    def emit(self, final=False):
        nc = self.nc
        csem, dsem = self.csem, self.dsem
        if final:
            self.barrier()
        with nc.Block() as block:
            engobj = {"sp": block.sync, "act": block.scalar, "dve": block.vector,
                      "pool": block.gpsimd, "pe": block.tensor}

            def mk(ename):
                stream = self.streams[ename]
                def body(eng):
                    for waits, fn, is_dma, slot in stream:
                        for kind, E, n in waits:
                            if kind == "c":
                                eng.wait_ge(csem[E], n)
                            else:
                                eng.wait_ge(dsem[E[0]][E[1]], 16 * n)
                        if fn is None:
                            continue
                        inst = fn(eng)
                        if is_dma:
                            inst.then_inc(dsem[ename][slot], 16)
                        else:
                            inst.then_inc(csem[ename], 1)
                return body
            for e in ENGS:
                if self.streams[e]:
                    engobj[e](mk(e))
        self.streams = {e: [] for e in ENGS}


def _consts(T):
    N1 = T // 128
    n2 = np.arange(128)[:, None].astype(np.float64)
    k2 = np.arange(128)[None, :].astype(np.float64)
    gx = np.zeros((N1, 128, 3, 128), np.float32)
    for n1 in range(N1):
        ang = 2 * np.pi * (((N1 * n2 + n1) * k2) % T) / T
        gx[n1, :, 0, :] = np.cos(ang)
        gx[n1, :, 1, :] = np.sin(ang)
        gx[n1, :, 2, :] = -np.sin(ang)
    a = np.arange(N1)[:, None].astype(np.float64)
    b = np.arange(N1)[None, :].astype(np.float64)
    ang = 2 * np.pi * ((a * b) % N1) / N1
    fs = np.concatenate([np.cos(ang), np.sin(ang)], axis=0) / np.sqrt(T * 128.0)
    return gx, fs.astype(np.float32)


def _pos_embed(T):
    quarter = D // 4
    omega = (1.0 / (10000.0 ** (np.arange(quarter, dtype=np.float32) / np.float32(quarter)))).astype(np.float32)
    rows = T // 64
    r = np.repeat(np.arange(rows, dtype=np.float32), 64)[:, None] * omega
    cc = np.tile(np.arange(64, dtype=np.float32), rows)[:, None] * omega
    return np.concatenate([np.sin(r), np.cos(r), np.sin(cc), np.cos(cc)], axis=-1).astype(np.float32)


def build(T, CT, L, dbg=()):
    TT = T + CT
    NT = TT // 128
    NCT = CT // 128
    nc = bass.Bass("TRN2", target_bir_lowering=False)

    def din(name, shape, dt=F32):
        return nc.dram_tensor(name, list(shape), dt, kind="ExternalInput").ap()

    def dscr(name, shape, dt=F32):
        kind = "ExternalOutput" if name in dbg else "Internal"
        return nc.dram_tensor(name, list(shape), dt, kind=kind).ap()

    x_in = din("x", [T, D]); pe = din("pe", [T, D]); ctx_in = din("ctx", [CT, D]); cc = din("cc", [2, D])
    w_mod = din("w_mod", [L, D, 6 * D]); b_mod = din("b_mod", [L, 6 * D])
    norm1_g = din("norm1_g", [L, D]); norm2_g = din("norm2_g", [L, D])
    w_in = din("w_in", [L, D, PW]); b_in = din("b_in", [L, PW])
    conv_qk = din("conv_qk", [L, 3, 1024]); mng = din("mlstm_norm_g", [L, 512])
    sgu_w = din("sgu_w", [L, 4, 128, 128]); sgu_b = din("sgu_b", [L, 512])
    w_br = din("w_br", [L, 3, 512, D]); w_out = din("w_out", [L, D, D])
    peer_wq = din("peer_wq", [L, D, 2048]); peer_keys = din("peer_keys", [L, 2, 128, 128])
    peer_u = din("peer_u", [L, NEXP, D]); peer_v = din("peer_v", [L, NEXP, D])
    final_g = din("final_g", [1, D])
    cdft = din("cdft", [128, 256])
    gx_x = din("gx_x", [T // 128, 128, 3, 128]); fs_x = din("fs_x", [2 * (T // 128), T // 128])
    gx_c = din("gx_c", [CT // 128, 128, 3, 128]); fs_c = din("fs_c", [2 * (CT // 128), CT // 128])
    out = nc.dram_tensor("out", [T, D], F32, kind="ExternalOutput").ap()

    XS = dscr("XS", [TT, D])
    QT = dscr("QT", [512, TT]); KT = dscr("KT", [512, TT]); SUT = dscr("SUT", [512, TT]); FZT = dscr("FZT", [512, TT])
    QC = dscr("QC", [512, TT]); KC = dscr("KC", [512, TT]); KTOK = dscr("KTOK", [TT, 512])
    V = dscr("V", [TT, 512]); O = dscr("O", [TT, 512]); SV = dscr("SV", [TT, 512]); G = dscr("G", [TT, 16])
    MG = dscr("MG", [TT, 3072])
    HF = dscr("HF", [TT, 512]); HB = dscr("HB", [TT, 512])
    B0 = dscr("B0", [TT, 512]); B1T = dscr("B1T", [512, TT]); B2 = dscr("B2", [TT, 512])
    Z = dscr("Z", [2, TT, 512]); APDX = dscr("APD", [2, T // 128, 128, 512]); APDC = dscr("APDC", [2, CT // 128, 128, 512])
    MOD = dscr("MOD", [L, 2, 6 * D])
    UV = dscr("UV", [NEXP, 2, D], BF16)
    IDX = dscr("IDX", [TT, 128], I32); PWT = dscr("PWT", [TT, 128]); HN = dscr("HN", [TT, D])

    es = ExitStack()
    with es:
        es.enter_context(nc.allow_non_contiguous_dma(reason="small strided parameter loads"))
        es.enter_context(nc.allow_low_precision(reason="bf16 matmul operands, fp32 accumulate"))
        csem = {e: es.enter_context(nc.semaphore("c_" + e)) for e in ENGS}
        dsem = {e: [es.enter_context(nc.semaphore(f"d_{e}{i}")) for i in range(NSLOT)] for e in ("sp", "pool")}
        P = Prog(nc, csem, dsem)

        uniq = [0]

        def sbt(st, name, shape, dt=F32):
            uniq[0] += 1
            name = f"{name}_{uniq[0]}"
            return Tile(st.enter_context(nc.sbuf_tensor(name, list(shape), dt)), name)

        class Ring(list):
            def __getitem__(self, i):
                return list.__getitem__(self, i % len(self))

        def ring(st, name, n, shape, dt=F32):
            return Ring([sbt(st, f"{name}{i}", shape, dt) for i in range(n)])

        PS = [Tile(es.enter_context(nc.psum_tensor(f"ps{i}", [128, 512], F32)), f"ps{i}") for i in range(8)]
        ident = sbt(es, "ident", [128, 128]); maskU = sbt(es, "maskU", [128, 128]); maskL = sbt(es, "maskL", [128, 128])
        ones = sbt(es, "ones", [128, 128]); iota16 = sbt(es, "iota16", [128, 16])
        Cf = [sbt(es, f"Cf{d}", [128, 4, 129]) for d in range(2)]
        Cb = [sbt(es, f"Cb{d}", [128, 4, 130], BF16) for d in range(2)]
        cdb = sbt(es, "cdb", [128, 256], BF16)
        identb = sbt(es, "identb", [128, 128], BF16)

        def ktiles(name, t0, t1):
            return [(name, i) for i in range(t0 // 128, (t1 + 127) // 128)]

        with ExitStack() as st:
            P.memset(ident[:], 0.0, [ident]); P.memset(maskU[:], 1.0, [maskU]); P.memset(maskL[:], 1.0, [maskL])
            P.memset(ones[:], 1.0, [ones])
            P.op("pool", lambda e: e.affine_select(out=ident[:], in_=ident[:], pattern=[[-1, 128]], compare_op=ALU.not_equal, fill=1.0, base=0, channel_multiplier=1), [ident], [ident])
            P.op("pool", lambda e: e.affine_select(out=maskU[:], in_=maskU[:], pattern=[[1, 128]], compare_op=ALU.is_ge, fill=0.0, base=0, channel_multiplier=-1), [maskU], [maskU])
            P.op("pool", lambda e: e.affine_select(out=maskL[:], in_=maskL[:], pattern=[[-1, 128]], compare_op=ALU.is_ge, fill=0.0, base=0, channel_multiplier=1), [maskL], [maskL])
            P.op("pool", lambda e: e.iota(iota16[:], pattern=[[1, 16]], base=0, channel_multiplier=0, allow_small_or_imprecise_dtypes=True), (), [iota16])
            P.cp(identb[:], ident[:], [ident], [identb], eng="pool")
            cdf = sbt(st, "cdf", [128, 256])
            P.dma("sp", cdf[:], cdft[:, :], W=[cdf])
            P.cp(cdb[:], cdf[:], [cdf], [cdb])
            xa = ring(st, "xa", 2, [128, D]); xb = ring(st, "xb", 2, [128, D])
            for i in range(NT):
                a = xa[i % 2]; b = xb[i % 2]
                if i < NCT:
                    P.dma("sp", a[:], ctx_in[i * 128:(i + 1) * 128, :], W=[a])
                    P.dma("sp", XS[i * 128:(i + 1) * 128, :], a[:], R=[a], W=[("XS", i)])
                else:
                    j = i - NCT
                    P.dma("sp", a[:], x_in[j * 128:(j + 1) * 128, :], W=[a])
                    P.dma("pool", b[:], pe[j * 128:(j + 1) * 128, :], W=[b])
                    P.tt(a[:], a[:], b[:], ALU.add, [a, b], [a])
                    P.dma("sp", XS[i * 128:(i + 1) * 128, :], a[:], R=[a], W=[("XS", i)])
            P.emit()

        def load_cols(st, name, src_row, eng="sp"):
            t = sbt(st, name, [128, 8])
            P.dma(eng, t[:], src_row.rearrange("(j p) -> p j", p=128), W=[t])
            return t

        def load_bc(st, name, src_row, n, eng="sp"):
            t = sbt(st, name, [128, n])
            P.dma(eng, t[:], src_row.partition_broadcast(128), W=[t])
            return t

        def rsqrt_(ap, keys):
            P.act(ap, ap, AF.Sqrt, keys, keys)
            P.op("dve", lambda e: e.reciprocal(out=ap, in_=ap), keys, keys)

        def rms_rstd(xt, junk, ss, rstd):
            P.act(junk[:], xt[:], AF.Square, [xt], [junk, ss], accum_out=ss[:])
            P.ts(rstd[:], ss[:], 1.0 / D, EPS, ALU.mult, ALU.add, [ss], [rstd])
            rsqrt_(rstd[:], [rstd])

        for l in range(L):
            P.barrier()
            with ExitStack() as st:
                scT = sbt(st, "scT", [128, 2, 8])
                for r in range(2):
                    P.dma("sp", scT[:, r, :], cc[r, :].rearrange("(j p) -> p j", p=128), W=[scT])
                P.act(scT[:], scT[:], AF.Silu, [scT], [scT])
                wm = ring(st, "wm", 2, [128, 8, 512]); bm = ring(st, "bm", 2, [2, 512]); mo = ring(st, "mo", 2, [2, 512])
                for blk in range(12):
                    w = wm[blk % 2]; bb = bm[blk % 2]; m = mo[blk % 2]; ps = PS[blk % 2]
                    P.dma("sp" if blk % 2 == 0 else "pool", w[:], w_mod[l].rearrange("(j p) n -> p j n", p=128)[:, :, blk * 512:(blk + 1) * 512], W=[w])
                    P.dma("sp", bb[:], b_mod[l, blk * 512:(blk + 1) * 512].partition_broadcast(2), W=[bb])
                    for j in range(8):
                        P.mm(ps[0:2, :], scT[:, :, j], w[:, j, :], j == 0, j == 7, [scT, w], [ps])
                    P.tt(m[:], ps[0:2, :], bb[:], ALU.add, [ps, bb], [m])
                    P.dma("sp", MOD[l, :, blk * 512:(blk + 1) * 512], m[:], R=[m], W=["MOD"])
                P.emit()

            def modcols(st, name, m, r):
                return load_cols(st, name, MOD[l, r, m * D:(m + 1) * D])

            def modbc(st, name, m, r):
                return load_bc(st, name, MOD[l, r, m * D:(m + 1) * D], D)

            P.barrier()
            with ExitStack() as st:
                winb = sbt(st, "winb", [128, 8, PW], BF16)
                wst = ring(st, "wst", 2, [128, 8, 512])
                nblk = (PW + 511) // 512
                for blk in range(nblk):
                    c0 = blk * 512; wd = min(512, PW - c0); w = wst[blk % 2]
                    P.dma("sp" if blk % 2 == 0 else "pool", w[:, :, 0:wd], w_in[l].rearrange("(j p) n -> p j n", p=128)[:, :, c0:c0 + wd], W=[w])
                    if blk % 2 == 0:
                        P.cp(winb[:, :, c0:c0 + wd], w[:, :, 0:wd], [w], [("winb", blk)])
                    else:
                        P.act(winb[:, :, c0:c0 + wd], w[:, :, 0:wd], AF.Copy, [w], [("winb", blk)])
                winb_keys = [("winb", b) for b in range(nblk)]
                bbc = load_bc(st, "bbc", b_in[l, :], PW, eng="pool")
                bcol = sbt(st, "bcol", [128, 16])
                for fi, off in enumerate((OQ, OK_, OSU, OFZ)):
                    P.dma("sp", bcol[:, fi * 4:(fi + 1) * 4], b_in[l, off:off + 512].rearrange("(i p) -> p i", p=128), W=[("bcol", fi)])
                bcol_keys = [("bcol", fi) for fi in range(4)]
                g1 = load_cols(st, "g1", norm1_g[l, :])
                Acol = []; Bcol = []
                for r in range(2):
                    sc = modcols(st, f"sc{r}", 1, r); sh = modcols(st, f"sh{r}", 0, r)
                    P.stt(sc[:], sc[:], 1.0, g1[:], ALU.add, ALU.mult, [sc, g1], [sc])
                    Acol.append(sc); Bcol.append(sh)
                xt = ring(st, "xt", 2, [128, D]); xn = ring(st, "xn", 2, [128, D]); ssr = ring(st, "ss", 2, [128, 1]); rsr = ring(st, "rs", 2, [128, 1])
                hT = ring(st, "hT", 2, [128, 8, 128], BF16)
                fo = ring(st, "fo", 4, [128, 4, 128]); to = ring(st, "to", 4, [128, 512]); go = ring(st, "go", 2, [128, 16])
                pcnt = [0]
                def nextps():
                    pcnt[0] += 1
                    return PS[2 + pcnt[0] % 6]
                fcnt = [0]; tcnt = [0]
                for i in range(NT):
                    r = 1 if i < NCT else 0
                    x_ = xt[i % 2]; n_ = xn[i % 2]; ss = ssr[i % 2]; rs = rsr[i % 2]; h_ = hT[i % 2]
                    P.dma("sp", x_[:], XS[i * 128:(i + 1) * 128, :], R=[("XS", i)], W=[x_])
                    rms_rstd(x_, n_, ss, rs)
                    P.ts(n_[:], x_[:], rs[:], None, ALU.mult, None, [x_, rs], [n_])
                    for half in range(2):
                        ps = PS[half]
                        for jj in range(4):
                            j = half * 4 + jj
                            P.tr(ps[:, jj * 128:(jj + 1) * 128], n_[:, j * 128:(j + 1) * 128], ident[:], [n_, ident], [ps])
                        for jj in range(4):
                            j = half * 4 + jj
                            P.act(h_[:, j, :], ps[:, jj * 128:(jj + 1) * 128], AF.Identity, [ps, Acol[r], Bcol[r]], [h_],
                                  scale=Acol[r][:, j:j + 1], bias=Bcol[r][:, j:j + 1])
                    for fi, (off, dst, dname) in enumerate(((OQ, QT, "QT"), (OK_, KT, "KT"), (OSU, SUT, "SUT"), (OFZ, FZT, "FZT"))):
                        ps = nextps(); f_ = fo[fcnt[0] % 4]; fcnt[0] += 1
                        for cb in range(4):
                            for j in range(8):
                                P.mm(ps[:, cb * 128:(cb + 1) * 128], winb[:, j, off + cb * 128: off + (cb + 1) * 128], h_[:, j, :], j == 0, j == 7, [h_] + winb_keys, [ps])
                        for cb in range(4):
                            P.act(f_[:, cb, :], ps[:, cb * 128:(cb + 1) * 128], AF.Identity, [ps] + bcol_keys, [f_], bias=bcol[:, fi * 4 + cb: fi * 4 + cb + 1])
                        P.dma("sp", dst.rearrange("(j c) t -> c j t", c=128)[:, :, i * 128:(i + 1) * 128], f_[:], R=[f_], W=[(dname, i)])
                    tm = [(OV, V, "V", 0), (OO, O, "O", 0), (OSV, SV, "SV", 0)] + [(OMG + 512 * m, MG, "MG", 512 * m) for m in range(6)]
                    for (off, dst, dname, dcol) in tm:
                        ps = nextps(); t_ = to[tcnt[0] % 4]; tcnt[0] += 1
                        for j in range(8):
                            P.mm(ps[:, :], h_[:, j, :], winb[:, j, off:off + 512], j == 0, j == 7, [h_] + winb_keys, [ps])
                        P.tt(t_[:], ps[:, :], bbc[:, off:off + 512], ALU.add, [ps, bbc], [t_])
                        P.dma("pool", dst[i * 128:(i + 1) * 128, dcol:dcol + 512], t_[:], R=[t_], W=[(dname, i, dcol)])
                    ps = nextps(); g_ = go[i % 2]
                    for j in range(8):
                        P.mm(ps[:, 0:16], h_[:, j, :], winb[:, j, OG:OG + 16], j == 0, j == 7, [h_] + winb_keys, [ps])
                    P.tt(g_[:], ps[:, 0:16], bbc[:, OG:OG + 16], ALU.add, [ps, bbc], [g_])
                    P.dma("pool", G[i * 128:(i + 1) * 128, :], g_[:], R=[g_], W=[("G", i)])
                P.emit()

            segs = [(0, CT), (CT, T)]

            P.barrier()
            with ExitStack() as st:
                WP = 1024
                xin = ring(st, "cin", 2, [128, WP + 2]); t1 = ring(st, "ct1", 2, [128, WP]); kt = ring(st, "ckt", 2, [128, WP // 128, 128])
                cw = ring(st, "cw", 2, [128, 3])
                it = 0
                for (src, dst, sname, dname, coff, isk) in ((QT, QC, "QT", "QC", 0, False), (KT, KC, "KT", "KC", 512, True)):
                    for cb in range(4):
                        w_ = cw[(it // 1) % 2]
                        for (s0, sl) in segs:
                            for p0 in range(0, sl, WP):
                                wlen = min(WP, sl - p0)
                                xi = xin[it % 2]; t_ = t1[it % 2]; k_ = kt[it % 2]
                                if p0 == 0 and s0 == 0:
                                    pass
                                a0 = s0 + p0
                                lo = 1 if p0 == 0 else 0
                                hi = 1 if p0 + wlen >= sl else 0
                                if lo:
                                    P.memset(xi[:, 0:1], 0.0, [xi], eng="dve")
                                if hi:
                                    P.memset(xi[:, wlen + 1:wlen + 2], 0.0, [xi], eng="dve")
                                P.dma("sp", xi[:, lo:wlen + 2 - hi], src[cb * 128:(cb + 1) * 128, a0 - 1 + lo:a0 + wlen + 1 - hi],
                                      R=ktiles(sname, a0 - 1 + lo, a0 + wlen + 1 - hi), W=[xi])
                                if True:
                                    P.dma("pool", w_[:], conv_qk[l, :, coff + cb * 128: coff + (cb + 1) * 128].rearrange("k c -> c k"), W=[w_])
                                P.ts(t_[:, 0:wlen], xi[:, 1:wlen + 1], w_[:, 1:2], None, ALU.mult, None, [xi, w_], [t_])
                                P.stt(t_[:, 0:wlen], xi[:, 0:wlen], w_[:, 0:1], t_[:, 0:wlen], ALU.mult, ALU.add, [xi, w_, t_], [t_])
                                P.stt(t_[:, 0:wlen], xi[:, 2:wlen + 2], w_[:, 2:3], t_[:, 0:wlen], ALU.mult, ALU.add, [xi, w_, t_], [t_])
                                P.act(t_[:, 0:wlen], t_[:, 0:wlen], AF.Silu, [t_], [t_])
                                P.dma("sp", dst[cb * 128:(cb + 1) * 128, a0:a0 + wlen], t_[:, 0:wlen], R=[t_], W=ktiles(dname + str(cb), a0, a0 + wlen))
                                if isk:
                                    nch = wlen // 128
                                    for c4 in range(0, nch, 4):
                                        ps = PS[(c4 // 4) % 4]
                                        for q in range(min(4, nch - c4)):
                                            P.tr(ps[:, q * 128:(q + 1) * 128], t_[:, (c4 + q) * 128:(c4 + q + 1) * 128], ident[:], [t_, ident], [ps])
                                        nq = min(4, nch - c4)
                                        P.act(k_[:, c4:c4 + nq, :], ps[:, 0:nq * 128].rearrange("p (q c) -> p q c", c=128), AF.Copy, [ps], [k_])
                                    P.dma("pool", KTOK[a0:a0 + wlen, cb * 128:(cb + 1) * 128].rearrange("(n t) c -> t n c", t=128), k_[:, 0:nch, :], R=[k_], W=ktiles("KTOK" + str(cb), a0, a0 + wlen))
                                it += 1
                P.emit()

            P.barrier()
            with ExitStack() as st:
                gt = ring(st, "gt", 2, [128, 16]); e1 = ring(st, "e1", 2, [128, 4]); sp_ = ring(st, "spl", 2, [128, 4])
                al = ring(st, "al", 2, [128, 4]); be = ring(st, "be", 2, [128, 4]); et = ring(st, "et", 2, [128, 4]); tmp4 = ring(st, "tmp4", 2, [128, 4])
                qf = ring(st, "qf", 2, [128, 4, 128]); kf = ring(st, "kf", 2, [128, 4, 128]); ktk = ring(st, "ktk", 2, [128, 512]); vf = ring(st, "vf", 2, [128, 512])
                qb = ring(st, "qb", 2, [128, 4, 128], BF16); kb = ring(st, "kb", 2, [128, 4, 128], BF16); ktb = ring(st, "ktb", 2, [128, 512], BF16)
                vb = ring(st, "vb", 2, [128, 4, 130], BF16); pt = ring(st, "pt", 2, [128, 4, 128], BF16)
                hh = ring(st, "hh", 2, [128, 4, 128]); d1 = ring(st, "d1", 2, [128, 4])
                lnb = sbt(st, "lnb", [128, 1])
                P.memset(lnb[:], -0.5 * math.log(128.0), [lnb])
                def scan_gen(dr):
                    mask = maskU if dr == 0 else maskL
                    Hd, hname = (HF, "HF") if dr == 0 else (HB, "HB")
                    fcol = 4 if dr == 0 else 12
                    icol = 0 if dr == 0 else 8
                    order = []
                    for (s0, sl) in segs:
                        ch = list(range(s0 // 128, (s0 + sl) // 128))
                        order.append(ch if dr == 0 else ch[::-1])
                    for si, chs in enumerate(order):
                        if si == 0:
                            P.memset(Cf[dr][:], 0.0, [Cf[dr]], eng="dve")
                            P.memset(Cb[dr][:], 0.0, [Cb[dr]], eng="dve")
                        for c in chs:
                            k = dr
                            g_ = gt[k]; e_ = e1[k]; s_ = sp_[k]; a_ = al[k]; b_ = be[k]; t_ = et[k]; m4 = tmp4[k]
                            r0 = c * 128
                            P.dma("sp", g_[:], G[r0:r0 + 128, :], R=[("G", c)], W=[g_])
                            P.act(e_[:], g_[:, fcol:fcol + 4], AF.Exp, [g_], [e_], scale=-1.0)
                            P.act(s_[:], e_[:], AF.Ln, [e_], [s_], bias=1.0)
                            psg = PS[7]
                            P.mm(psg[:, 0:4], mask[:], s_[:], True, True, [mask, s_], [psg])
                            P.mm(psg[:, 4:8], ones[:], s_[:], True, True, [ones, s_], [psg])
                            P.act(a_[:], psg[:, 0:4], AF.Exp, [psg, lnb], [a_], scale=-1.0, bias=lnb[:])
                            P.tt(m4[:], psg[:, 0:4], g_[:, icol:icol + 4], ALU.add, [psg, g_], [m4])
                            P.act(b_[:], m4[:], AF.Exp, [m4], [b_])
                            P.act(t_[:], psg[:, 4:8], AF.Exp, [psg], [t_], scale=-1.0)
                            q_ = qf[k]; k_ = kf[k]; kt_ = ktk[k]; v_ = vf[k]
                            P.dma("sp", q_[:], QC.rearrange("(h d) t -> d h t", d=128)[:, :, r0:r0 + 128], R=[("QC%d" % h, c) for h in range(4)], W=[q_])
                            P.dma("pool", k_[:], KC.rearrange("(h d) t -> d h t", d=128)[:, :, r0:r0 + 128], R=[("KC%d" % h, c) for h in range(4)], W=[k_])
                            P.dma("sp", kt_[:], KTOK[r0:r0 + 128, :], R=[("KTOK%d" % h, c) for h in range(4)], W=[kt_])
                            P.dma("pool", v_[:], V[r0:r0 + 128, :], R=[("V", c, 0)], W=[v_])
                            qb_ = qb[k]; kb_ = kb[k]; ktb_ = ktb[k]; vb_ = vb[k]; pt_ = pt[k]
                            P.act(qb_[:], q_[:], AF.Copy, [q_], [qb_])
                            P.act(kb_[:], k_[:], AF.Copy, [k_], [kb_])
                            P.act(ktb_[:], kt_[:], AF.Copy, [kt_], [ktb_])
                            P.tt(vb_[:, :, 0:128], v_[:].rearrange("p (h e) -> p h e", e=128), b_[:].unsqueeze(2).to_broadcast([128, 4, 128]), ALU.mult, [v_, b_], [vb_])
                            P.cp(vb_[:, :, 128:129], b_[:].unsqueeze(2), [b_], [vb_])
                            pss = PS[0]
                            for h in range(4):
                                P.mm(pss[:, h * 128:(h + 1) * 128], kb_[:, h, :], qb_[:, h, :], True, True, [kb_, qb_], [pss])
                            P.tt(pt_[:], pss[:].rearrange("p (h t) -> p h t", t=128), mask[:].unsqueeze(1).to_broadcast([128, 4, 128]), ALU.mult, [pss, mask], [pt_])
                            acc = [PS[1], PS[2]]; dcp = [PS[3], PS[4]]
                            for h in range(4):
                                a = acc[h // 2]; o_ = (h % 2) * 129
                                P.mm(a[:, o_:o_ + 129], pt_[:, h, :], vb_[:, h, 0:129], True, False, [pt_, vb_], [a])
                                P.mm(a[:, o_:o_ + 129], qb_[:, h, :], Cb[dr][:, h, 0:129], False, True, [qb_, Cb[dr]], [a])
                            for h in range(4):
                                dc = dcp[h // 2]; o_ = (h % 2) * 129
                                P.mm(dc[:, o_:o_ + 129], ktb_[:, h * 128:(h + 1) * 128], vb_[:, h, 0:129], True, True, [ktb_, vb_], [dc])
                            h_ = hh[k]; d_ = d1[k]
                            for hp in range(2):
                                a = acc[hp]; av = a[:, 0:258].rearrange("p (h e) -> p h e", e=129)
                                hs = slice(hp * 2, hp * 2 + 2)
                                P.tt(d_[:, hs], av[:, :, 128], a_[:, hs], ALU.mult, [a, a_], [("d1", k, hp)])
                                P.act(d_[:, hs], d_[:, hs], AF.Abs, [("d1", k, hp)], [("d1", k, hp)])
                                P.ts(d_[:, hs], d_[:, hs], 1.0, None, ALU.max, None, [("d1", k, hp)], [("d1", k, hp)])
                                P.op("dve", (lambda dd: (lambda e: e.reciprocal(out=dd, in_=dd)))(d_[:, hs]), [("d1", k, hp)], [("d1", k, hp)])
                                P.tt(d_[:, hs], d_[:, hs], a_[:, hs], ALU.mult, [("d1", k, hp), a_], [("d1", k, hp)])
                                P.tt(h_[:, hs, :], av[:, :, 0:128], d_[:, hs].unsqueeze(2).to_broadcast([128, 2, 128]), ALU.mult, [a, ("d1", k, hp)], [(h_.key, hp)])
                                dc = dcp[hp]; dv = dc[:, 0:258].rearrange("p (h e) -> p h e", e=129)
                                P.tt(Cf[dr][:, hs, :], dv, Cf[dr][:, hs, :], ALU.add, [dc, Cf[dr]], [Cf[dr]], eng="pool" if False else "dve")
                                P.tt(Cf[dr][:, hs, :], Cf[dr][:, hs, :], t_[:, hs].unsqueeze(2).to_broadcast([128, 2, 129]), ALU.mult, [Cf[dr], t_], [Cf[dr]])
                                P.cp(Cb[dr][:, hs, 0:129], Cf[dr][:, hs, :], [Cf[dr]], [Cb[dr]])
                            P.dma("sp", Hd[r0:r0 + 128, :], h_[:].rearrange("p h e -> p (h e)"), R=[(h_.key, 0), (h_.key, 1)], W=[(hname, c)])
                            yield
                from itertools import zip_longest
                for _ in zip_longest(scan_gen(0), scan_gen(1)):
                    pass
                P.emit()

            P.barrier()
            with ExitStack() as st:
                ngb = load_bc(st, "ngb", mng[l, :], 512)
                hf = ring(st, "hf", 2, [128, 4, 128]); hb_ = ring(st, "hb", 2, [128, 4, 128]); ot = ring(st, "ot", 2, [128, 512])
                sq = ring(st, "sq", 2, [128, 4, 128]); mu = ring(st, "mu", 2, [128, 4]); vr = ring(st, "vr", 2, [128, 4])
                for i in range(NT):
                    k = i % 2; a = hf[k]; b = hb_[k]; o_ = ot[k]; s_ = sq[k]; m_ = mu[k]; v_ = vr[k]
                    P.dma("sp", a[:].rearrange("p h e -> p (h e)"), HF[i * 128:(i + 1) * 128, :], R=[("HF", i)], W=[a])
                    P.dma("pool", b[:].rearrange("p h e -> p (h e)"), HB[i * 128:(i + 1) * 128, :], R=[("HB", i)], W=[b])
                    P.dma("sp", o_[:], O[i * 128:(i + 1) * 128, :], R=[("O", i, 0)], W=[o_])
                    P.tt(a[:], a[:], b[:], ALU.add, [a, b], [a])
                    P.red(m_[:], a[:], ALU.add, [a], [m_])
                    P.ts(m_[:], m_[:], 1.0 / 128, None, ALU.mult, None, [m_], [m_])
                    P.tt(a[:], a[:], m_[:].unsqueeze(2).to_broadcast([128, 4, 128]), ALU.subtract, [a, m_], [a])
                    P.tt(s_[:], a[:], a[:], ALU.mult, [a], [s_])
                    P.red(v_[:], s_[:], ALU.add, [s_], [v_])
                    P.ts(v_[:], v_[:], 1.0 / 128, EPS, ALU.mult, ALU.add, [v_], [v_])
                    rsqrt_(v_[:], [v_])
                    P.tt(a[:], a[:], v_[:].unsqueeze(2).to_broadcast([128, 4, 128]), ALU.mult, [a, v_], [a])
                    P.act(o_[:], o_[:], AF.Sigmoid, [o_], [o_])
                    P.tt(o_[:], o_[:], ngb[:], ALU.mult, [o_, ngb], [o_])
                    P.tt(o_[:], o_[:], a[:].rearrange("p h e -> p (h e)"), ALU.mult, [o_, a], [o_])
                    P.dma("sp", B0[i * 128:(i + 1) * 128, :], o_[:], R=[o_], W=[("B0", i)])
                P.emit()

            P.barrier()
            with ExitStack() as st:
                wsn = sbt(st, "wsn", [128, 4, 128]); wsT = sbt(st, "wsT", [128, 4, 128], BF16)
                P.dma("sp", wsn[:], sgu_w[l].rearrange("g t s -> t g s"), W=[wsn])
                for g in range(4):
                    P.tr(PS[0][:, g * 128:(g + 1) * 128], wsn[:, g, :], ident[:], [wsn, ident], [PS[0]])
                P.cp(wsT[:].rearrange("p g t -> p (g t)"), PS[0][:, :], [PS[0]], [wsT])
                bsT = load_bc(st, "bsT", sgu_b[l, :], 512)
                sv = ring(st, "sv", 2, [128, 512]); su = ring(st, "su", 2, [128, 4, 128]); vn = ring(st, "vn", 2, [128, 512], BF16)
                sq = ring(st, "sq2", 2, [128, 512]); mu = ring(st, "mu2", 2, [128, 1]); vr = ring(st, "vr2", 2, [128, 1]); b1 = ring(st, "b1", 2, [128, 4, 128])
                for i in range(NT):
                    k = i % 2; v_ = sv[k]; u_ = su[k]; n_ = vn[k]; s_ = sq[k]; m_ = mu[k]; r_ = vr[k]; o_ = b1[k]
                    P.dma("sp", v_[:], SV[i * 128:(i + 1) * 128, :], R=[("SV", i, 0)], W=[v_])
                    P.dma("pool", u_[:], SUT.rearrange("(g c) t -> c g t", c=128)[:, :, i * 128:(i + 1) * 128], R=[("SUT", i)], W=[u_])
                    P.act(v_[:], v_[:], AF.Gelu, [v_], [v_])
                    P.act(u_[:], u_[:], AF.Gelu, [u_], [u_])
                    P.red(m_[:], v_[:], ALU.add, [v_], [m_])
                    P.ts(m_[:], m_[:], 1.0 / 512, None, ALU.mult, None, [m_], [m_])
                    P.ts(v_[:], v_[:], m_[:], None, ALU.subtract, None, [v_, m_], [v_])
                    P.tt(s_[:], v_[:], v_[:], ALU.mult, [v_], [s_])
                    P.red(r_[:], s_[:], ALU.add, [s_], [r_])
                    P.ts(r_[:], r_[:], 1.0 / 512, EPS, ALU.mult, ALU.add, [r_], [r_])
                    rsqrt_(r_[:], [r_])
                    P.ts(n_[:], v_[:], r_[:], None, ALU.mult, None, [v_, r_], [n_])
                    ps = PS[1 + k]
                    for g in range(4):
                        P.mm(ps[:, g * 128:(g + 1) * 128], n_[:, g * 128:(g + 1) * 128], wsT[:, g, :], True, True, [n_, wsT], [ps])
                    P.tt(s_[:], ps[:, :], bsT[:], ALU.add, [ps, bsT], [s_])
                    P.tt(o_[:].rearrange("p g t -> p (g t)"), s_[:], u_[:].rearrange("p g t -> p (g t)"), ALU.mult, [s_, u_], [o_])
                    P.dma("sp", B1T.rearrange("(g c) t -> c g t", c=128)[:, :, i * 128:(i + 1) * 128], o_[:], R=[o_], W=[("B1T", i)])
                P.emit()

            P.barrier()
            with ExitStack() as st:
                fz = ring(st, "fz", 2, [128, 4, 128]); fzb = ring(st, "fzb", 2, [128, 4, 128], BF16); zt = ring(st, "zt", 2, [128, 2, 4, 128])
                for i in range(NT):
                    k = i % 2; f_ = fz[k]; fb = fzb[k]; z_ = zt[k]
                    P.dma("sp", f_[:], FZT.rearrange("(g c) t -> c g t", c=128)[:, :, i * 128:(i + 1) * 128], R=[("FZT", i)], W=[f_])
                    P.cp(fb[:], f_[:], [f_], [fb])
                    for half in range(2):
                        ps = PS[k * 2 + half]
                        for gg in range(2):
                            g = half * 2 + gg
                            P.mm(ps[:, gg * 256:(gg + 1) * 256], fb[:, g, :], cdb[:], True, True, [fb, cdb], [ps])
                        P.act(z_[:, :, half * 2:half * 2 + 2, :], ps[:, :].rearrange("p (g r c) -> p r g c", r=2, c=128), AF.Copy, [ps], [z_])
                    for r in range(2):
                        P.dma("sp" if r == 0 else "pool", Z[r, i * 128:(i + 1) * 128, :], z_[:, r, :, :].rearrange("p g c -> p (g c)"), R=[z_], W=[("Z", r, i)])
                P.emit()
            for (s0, sl), gxd, fsd, APD in ((segs[0], gx_c, fs_c, APDC), (segs[1], gx_x, fs_x, APDX)):
                N1 = sl // 128
                P.barrier()
                with ExitStack() as st:
                    gf = ring(st, "gf", 2, [128, 3, 128]); gb = ring(st, "gb", 2, [128, 3, 128], BF16)
                    zr = ring(st, "zr", 2, [128, 2, 512]); zb = ring(st, "zb", 2, [128, 2, 512], BF16); ao = ring(st, "ao", 2, [128, 2, 512])
                    zkeys = [("Z", r, i) for r in range(2) for i in range(s0 // 128, (s0 + sl) // 128)]
                    for n1 in range(N1):
                        k = n1 % 2; g_ = gf[k]; gb_ = gb[k]; z_ = zr[k]; zb_ = zb[k]; a_ = ao[k]
                        P.dma("sp", g_[:], gxd[n1], W=[g_])
                        P.act(gb_[:], g_[:], AF.Copy, [g_], [gb_])
                        for r in range(2):
                            src = Z[r, s0:s0 + sl, :].rearrange("(n2 n1) c -> n1 n2 c", n1=N1)[n1]
                            P.dma("sp" if r == 0 else "pool", z_[:, r, :], src, R=zkeys, W=[z_])
                        P.cp(zb_[:], z_[:], [z_], [zb_])
                        pr = PS[k * 2]; pi = PS[k * 2 + 1]
                        P.mm(pr[:, :], gb_[:, 0, :], zb_[:, 0, :], True, False, [gb_, zb_], [pr])
                        P.mm(pr[:, :], gb_[:, 1, :], zb_[:, 1, :], False, True, [gb_, zb_], [pr])
                        P.mm(pi[:, :], gb_[:, 0, :], zb_[:, 1, :], True, False, [gb_, zb_], [pi])
                        P.mm(pi[:, :], gb_[:, 2, :], zb_[:, 0, :], False, True, [gb_, zb_], [pi])
                        P.act(a_[:, 0, :], pr[:, :], AF.Copy, [pr], [a_])
                        P.cp(a_[:, 1, :], pi[:, :], [pi], [a_])
                        for r in range(2):
                            P.dma("sp" if r == 0 else "pool", APD[r, n1, :, :], a_[:, r, :], R=[a_], W=[("APD", r, n1)])
                    P.emit()
                P.barrier()
                with ExitStack() as st:
                    KB = 8 if N1 > 8 else 32
                    nrg = 2 if N1 > 8 else 1
                    fsf = sbt(st, "fsf", [128, N1]); fsb = sbt(st, "fsb", [128, N1], BF16)
                    P.memset(fsf[:], 0.0, [fsf], eng="dve")
                    P.dma("sp", fsf[0:2 * N1, :], fsd[:, :], R=[fsf], W=[fsf])
                    P.cp(fsb[:], fsf[:], [fsf], [fsb])
                    rt = ring(st, "rt", nrg, [128, KB, 512]); rb = ring(st, "rb", nrg, [128, KB, 512], BF16); ob = ring(st, "ob", nrg, [N1, KB, 512])
                    for r_ in rt:
                        P.memset(r_[:], 0.0, [r_], eng="dve")
                    akeys = [("APD", r, n1) for r in range(2) for n1 in range(N1)]
                    for kb0 in range(0, 128, KB):
                        k = (kb0 // KB) % 2; r_ = rt[k]; rb_ = rb[k]; o_ = ob[k]
                        for r in range(2):
                            P.dma("sp" if r == 0 else "pool", r_[r * N1:(r + 1) * N1, :, :], APD[r, 0:N1, kb0:kb0 + KB, :], R=akeys + [r_], W=[r_])
                        P.act(rb_[:, 0:KB // 2, :], r_[:, 0:KB // 2, :], AF.Copy, [r_], [(rb_.key, 0)])
                        P.cp(rb_[:, KB // 2:KB, :], r_[:, KB // 2:KB, :], [r_], [(rb_.key, 1)])
                        for kk in range(KB):
                            ps = PS[kk % 4]
                            P.mm(ps[0:N1, :], fsb[:], rb_[:, kk, :], True, True, [fsb, (rb_.key, 0), (rb_.key, 1)], [ps])
                            if kk % 2 == 0:
                                P.act(o_[:, kk, :], ps[0:N1, :], AF.Copy, [ps], [o_])
                            else:
                                P.cp(o_[:, kk, :], ps[0:N1, :], [ps], [o_])
                        dst = B2[s0:s0 + sl, :].rearrange("(k1 k2) c -> k1 k2 c", k2=128)[:, kb0:kb0 + KB, :]
                        P.dma("sp", dst, o_[:], R=[o_], W=[("B2", i) for i in range(s0 // 128, (s0 + sl) // 128)])
                    P.emit()

            P.barrier()
            with ExitStack() as st:
                wbr = sbt(st, "wbr", [128, 12, D], BF16); wob = sbt(st, "wob", [128, 8, D], BF16)
                wst = ring(st, "wst2", 2, [128, 4, D])
                for r in range(3):
                    w = wst[r % 2]
                    P.dma("sp", w[:], w_br[l, r].rearrange("(j p) n -> p j n", p=128), W=[w])
                    P.cp(wbr[:, r * 4:(r + 1) * 4, :], w[:], [w], [("wbr", r)])
                for hh_ in range(2):
                    w = wst[(3 + hh_) % 2]
                    P.dma("pool", w[:], w_out[l].rearrange("(j p) n -> p j n", p=128)[:, hh_ * 4:(hh_ + 1) * 4, :], W=[w])
                    P.act(wob[:, hh_ * 4:(hh_ + 1) * 4, :], w[:], AF.Copy, [w], [("wob", hh_)])
                wkeys = [("wbr", r) for r in range(3)] + [("wob", h) for h in range(2)]
                g1bc = [modbc(st, f"g1bc{r}", 2, r) for r in range(2)]
                b0 = ring(st, "b0", 2, [128, 512]); b2 = ring(st, "b2", 2, [128, 512]); b1f = ring(st, "b1f", 2, [128, 4, 128])
                bT = ring(st, "bT", 2, [128, 12, 128], BF16); mg = ring(st, "mg", 2, [128, 3072]); y = ring(st, "y", 2, [128, D]); tm = ring(st, "tm", 2, [128, D])
                yT = ring(st, "yT", 2, [128, 8, 128], BF16); xt = ring(st, "xt2", 2, [128, D])
                for i in range(NT):
                    k = i % 2; r = 1 if i < NCT else 0
                    a = b0[k]; c_ = b2[k]; f1 = b1f[k]; bt = bT[k]; m_ = mg[k]; y_ = y[k]; t_ = tm[k]; yt = yT[k]; x_ = xt[k]
                    P.dma("sp", a[:], B0[i * 128:(i + 1) * 128, :], R=[("B0", i)], W=[a])
                    P.dma("pool", c_[:], B2[i * 128:(i + 1) * 128, :], R=[("B2", i)], W=[c_])
                    P.dma("sp", f1[:], B1T.rearrange("(g c) t -> c g t", c=128)[:, :, i * 128:(i + 1) * 128], R=[("B1T", i)], W=[f1])
                    P.dma("pool", m_[:], MG[i * 128:(i + 1) * 128, :], R=[("MG", i, 512 * m) for m in range(6)], W=[m_])
                    P.dma("sp", x_[:], XS[i * 128:(i + 1) * 128, :], R=[("XS", i)], W=[x_])
                    for bi, src in ((0, a), (2, c_)):
                        ps = PS[0 if bi == 0 else 1]
                        for j in range(4):
                            P.tr(ps[:, j * 128:(j + 1) * 128], src[:, j * 128:(j + 1) * 128], ident[:], [src, ident], [ps])
                        P.act(bt[:, bi * 4:(bi + 1) * 4, :], ps[:, :].rearrange("p (j t) -> p j t", t=128), AF.Copy, [ps], [(bt.key, bi)])
                    P.act(bt[:, 4:8, :], f1[:], AF.Copy, [f1], [(bt.key, 1)])
                    P.act(m_[:], m_[:], AF.Sigmoid, [m_], [m_])
                    for rr in range(3):
                        pa, pb = PS[2 + 2 * (rr % 2)], PS[3 + 2 * (rr % 2)]
                        for half, ps in enumerate((pa, pb)):
                            for j in range(4):
                                P.mm(ps[:, :], bt[:, rr * 4 + j, :], wbr[:, rr * 4 + j, half * 512:(half + 1) * 512], j == 0, j == 3, [(bt.key, rr)] + wkeys, [ps])
                        dstt = y_ if rr == 0 else t_
                        for half, ps in enumerate((pa, pb)):
                            P.tt(dstt[:, half * 512:(half + 1) * 512], ps[:, :], m_[:, rr * D + half * 512: rr * D + (half + 1) * 512], ALU.mult, [ps, m_], [(dstt.key, half)])
                        if rr > 0:
                            P.tt(y_[:], y_[:], t_[:], ALU.add, [(y_.key, 0), (y_.key, 1), (t_.key, 0), (t_.key, 1)], [(y_.key, 0), (y_.key, 1)])
                    for half in range(2):
                        ps = PS[6 + half]
                        for jj in range(4):
                            j = half * 4 + jj
                            P.tr(ps[:, jj * 128:(jj + 1) * 128], y_[:, j * 128:(j + 1) * 128], ident[:], [(y_.key, 0), (y_.key, 1), ident], [ps])
                        P.act(yt[:, half * 4:(half + 1) * 4, :], ps[:, :].rearrange("p (j t) -> p j t", t=128), AF.Copy, [ps], [(yt.key, half)])
                    for half in range(2):
                        ps = PS[0 + half]
                        for j in range(8):
                            P.mm(ps[:, :], yt[:, j, :], wob[:, j, half * 512:(half + 1) * 512], j == 0, j == 7, [(yt.key, 0), (yt.key, 1)] + wkeys, [ps])
                        P.tt(t_[:, half * 512:(half + 1) * 512], ps[:, :], g1bc[r][:, half * 512:(half + 1) * 512], ALU.mult, [ps, g1bc[r]], [(t_.key, half)])
                    P.tt(x_[:], x_[:], t_[:], ALU.add, [x_, (t_.key, 0), (t_.key, 1)], [x_])
                    P.dma("sp", XS[i * 128:(i + 1) * 128, :], x_[:], R=[x_], W=[("XS", i)])
                P.emit()

            P.barrier()
            with ExitStack() as st:
                KR = 4
                tf = ring(st, "tf", 3, [128, KR, D]); tb = ring(st, "tb", 3, [128, KR, D], BF16)
                it = 0
                for (src, dst, dn) in ((peer_u, UV[:, 0, :], "UB"), (peer_v, UV[:, 1, :], "VB")):
                    for ch in range(NEXP // (128 * KR)):
                        f_ = tf[it]; b_ = tb[it]
                        r0 = ch * 128 * KR
                        P.dma("sp", f_[:], src[l, r0:r0 + 128 * KR, :].rearrange("(p k) d -> p k d", k=KR), W=[f_])
                        if it % 3 == 0:
                            P.cp(b_[:], f_[:], [f_], [b_])
                        else:
                            P.act(b_[:], f_[:], AF.Copy, [f_], [b_])
                        P.dma("pool", dst[r0:r0 + 128 * KR, :].rearrange("(p k) d -> p k d", k=KR), b_[:], R=[b_], W=[(dn, ch)])
                        it += 1
                P.emit()

            P.barrier()
            with ExitStack() as st:
                wqb = sbt(st, "wqb", [128, 8, 2048], BF16)
                with ExitStack() as st2:
                    wst = ring(st2, "wst3", 2, [128, 8, 256])
                    for blk in range(8):
                        w = wst[blk % 2]
                        P.dma("sp" if blk % 2 == 0 else "pool", w[:], peer_wq[l].rearrange("(j p) n -> p j n", p=128)[:, :, blk * 256:(blk + 1) * 256], W=[w])
                        if blk % 2 == 0:
                            P.cp(wqb[:, :, blk * 256:(blk + 1) * 256], w[:], [w], [("wqb", blk)])
                        else:
                            P.act(wqb[:, :, blk * 256:(blk + 1) * 256], w[:], AF.Copy, [w], [("wqb", blk)])
                    P.emit()
                P.barrier()
                wqkeys = [("wqb", b) for b in range(8)]
                kn = sbt(st, "kn", [128, 2, 128]); kTb = sbt(st, "kTb", [128, 2, 128], BF16)
                P.dma("sp", kn[:], peer_keys[l].rearrange("p k c -> k p c"), W=[kn])
                for p in range(2):
                    P.tr(PS[0][:, p * 128:(p + 1) * 128], kn[:, p, :], ident[:], [kn, ident], [PS[0]])
                P.cp(kTb[:].rearrange("c p k -> c (p k)"), PS[0][:, 0:256], [PS[0]], [kTb])
                g2 = load_bc(st, "g2", norm2_g[l, :], D)
                A2t = sbt(st, "A2t", [128, D]); B2t = sbt(st, "B2t", [128, D]); g2bct = sbt(st, "g2bct", [128, D])

                def load_mods(r):
                    P.dma("sp", A2t[:], MOD[l, r, 4 * D:5 * D].partition_broadcast(128), W=[A2t])
                    P.dma("sp", B2t[:], MOD[l, r, 3 * D:4 * D].partition_broadcast(128), W=[B2t])
                    P.dma("sp", g2bct[:], MOD[l, r, 5 * D:6 * D].partition_broadcast(128), W=[g2bct])
                    P.stt(A2t[:], A2t[:], 1.0, g2[:], ALU.add, ALU.mult, [A2t, g2], [A2t])

                xt = ring(st, "xt3", 2, [128, D]); hn = ring(st, "hn", 2, [128, D]); ssr = ring(st, "ss3", 2, [128, 1]); rsr = ring(st, "rs3", 2, [128, 1])
                hT = ring(st, "hT3", 1, [128, 8, 128], BF16); qTb = ring(st, "qTb", 1, [128, 16, 128], BF16)
                sc_ = ring(st, "scr", 1, [128, 16, 128]); wk = ring(st, "wk", 1, [128, 16, 128])
                tv = ring(st, "tv", 2, [128, 16, 16]); ti = ring(st, "ti", 2, [128, 16, 16], U32); tif = ring(st, "tif", 1, [128, 16, 16])
                cs = ring(st, "cs", 1, [128, 8, 256])
                bv = ring(st, "bv", 2, [128, 8, 16]); bj = ring(st, "bj", 2, [128, 8, 16], U32); ba = ring(st, "ba", 2, [128, 8, 16], U32); bbq = ring(st, "bbq", 2, [128, 8, 16], U32)
                baf = ring(st, "baf", 2, [128, 8, 16]); bbf = ring(st, "bbf", 2, [128, 8, 16])
                oh = ring(st, "oh", 1, [128, 8, 16, 16]); i1s = ring(st, "i1s", 2, [128, 8, 16]); i2s = ring(st, "i2s", 2, [128, 8, 16])
                ei = ring(st, "ei", 2, [128, 128], I32); mx = ring(st, "mx", 2, [128, 8]); pw = ring(st, "pw", 2, [128, 8, 16]); sm = ring(st, "sm", 2, [128, 8])
                RC = 8
                ug = ring(st, "ug", 2, [128, RC, 2 * D], BF16)
                ux = ring(st, "ux", 4, [128, RC]); acc = ring(st, "acc", 1, [128, D]); junk = sbt(st, "junk", [128, D], BF16)
                dg = ring(st, "dg", 2, [128, RC, 128], BF16)
                UVf = UV.rearrange("e two d -> e (two d)")
                cnt = {"uc": 0, "jc": 0}

                def top16(vals, work, outv, outi, rk, wk_, ok):
                    P.op("dve", lambda e: e.max(out=outv[:, 0:8], in_=vals), rk, ok)
                    P.op("dve", lambda e: e.match_replace(out=work, in_to_replace=outv[:, 0:8], in_values=vals, imm_value=-1e30), rk + ok, wk_)
                    P.op("dve", lambda e: e.max(out=outv[:, 8:16], in_=work), wk_, ok)
                    P.op("dve", lambda e: e.max_index(out=outi[:, 0:8], in_max=outv[:, 0:8], in_values=vals), rk + ok, ok)
                    P.op("dve", lambda e: e.max_index(out=outi[:, 8:16], in_max=outv[:, 8:16], in_values=vals), rk + ok, ok)

                def p1_tile(i):
                    k = i % 2
                    x_ = xt[k]; n_ = hn[k]; ss = ssr[k]; rs = rsr[k]; h_ = hT[k]; q_ = qTb[k]; s_ = sc_[k]; w_ = wk[k]
                    tv_ = tv[k]; ti_ = ti[k]; tf_ = tif[k]; cs_ = cs[k]; bv_ = bv[k]; bj_ = bj[k]; ba_ = ba[k]; bb_ = bbq[k]
                    P.dma("sp", x_[:], XS[i * 128:(i + 1) * 128, :], R=[("XS", i)], W=[x_])
                    rms_rstd(x_, n_, ss, rs)
                    P.ts(n_[:], x_[:], rs[:], None, ALU.mult, None, [x_, rs], [n_])
                    P.tt(n_[:], n_[:], A2t[:], ALU.mult, [n_, A2t], [n_])
                    P.tt(n_[:], n_[:], B2t[:], ALU.add, [n_, B2t], [n_])
                    for half in range(2):
                        ps = PS[half]
                        for jj in range(4):
                            j = half * 4 + jj
                            P.tr(ps[:, jj * 128:(jj + 1) * 128], n_[:, j * 128:(j + 1) * 128], ident[:], [n_, ident], [ps])
                        P.act(h_[:, half * 4:(half + 1) * 4, :], ps[:, :].rearrange("p (j t) -> p j t", t=128), AF.Copy, [ps], [(h_.key, half)])
                    yield
                    for g4 in range(4):
                        ps = PS[2 + g4]
                        for gg in range(4):
                            grp = g4 * 4 + gg
                            for j in range(8):
                                P.mm(ps[:, gg * 128:(gg + 1) * 128], wqb[:, j, grp * 128:(grp + 1) * 128], h_[:, j, :], j == 0, j == 7, [(h_.key, 0), (h_.key, 1)] + wqkeys, [ps])
                        P.act(q_[:, g4 * 4:(g4 + 1) * 4, :], ps[:, :].rearrange("p (g t) -> p g t", t=128), AF.Copy, [ps], [(q_.key, g4)])
                        if g4 % 2 == 1:
                            yield
                    for g4 in range(4):
                        ps = PS[g4]
                        for gg in range(4):
                            grp = g4 * 4 + gg
                            P.mm(ps[:, gg * 128:(gg + 1) * 128], q_[:, grp, :], kTb[:, grp % 2, :], True, True, [(q_.key, g4), kTb], [ps])
                        P.act(s_[:, g4 * 4:(g4 + 1) * 4, :], ps[:, :].rearrange("p (g t) -> p g t", t=128), AF.Copy, [ps], [(s_.key, g4)])
                    yield
                    for grp in range(16):
                        top16(s_[:, grp, :], w_[:, grp, :], tv_[:, grp, :], ti_[:, grp, :], [(s_.key, grp // 4)], [(w_.key, grp)], [(tv_.key, grp)])
                        if grp % 2 == 1:
                            yield
                    tvk = [(tv_.key, g) for g in range(16)]
                    P.cp(tf_[:], ti_[:], tvk, [tf_])
                    tv4 = tv_[:].rearrange("p (h q) a -> p h q a", q=2)
                    P.tt(cs_[:].rearrange("p h (a b) -> p h a b", b=16), tv4[:, :, 0, :].unsqueeze(3).to_broadcast([128, 8, 16, 16]),
                         tv4[:, :, 1, :].unsqueeze(2).to_broadcast([128, 8, 16, 16]), ALU.add, tvk, [cs_])
                    cwv = w_[:].rearrange("p (h a) b -> p h (a b)", a=2)
                    wall = [(w_.key, g) for g in range(16)]
                    for h in range(8):
                        top16(cs_[:, h, :], cwv[:, h, :], bv_[:, h, :], bj_[:, h, :], [cs_], wall, [(bv_.key, h)])
                        if h % 2 == 1:
                            yield
                    bvk = [(bv_.key, h) for h in range(8)]
                    P.ts(ba_[:], bj_[:], 4, None, ALU.logical_shift_right, None, bvk, [ba_])
                    P.ts(bb_[:], bj_[:], 15, None, ALU.bitwise_and, None, bvk, [bb_])
                    af = baf[k]; bf = bbf[k]; oh_ = oh[k]; s1 = i1s[k]; s2 = i2s[k]
                    P.cp(af[:], ba_[:], [ba_], [af]); P.cp(bf[:], bb_[:], [bb_], [bf])
                    tf4 = tf_[:].rearrange("p (h q) a -> p h q a", q=2)
                    io4 = iota16[:].unsqueeze(1).unsqueeze(1).to_broadcast([128, 8, 16, 16])
                    for (sel, src_q, dsts) in ((af, 0, s1), (bf, 1, s2)):
                        P.tt(oh_[:], sel[:].unsqueeze(3).to_broadcast([128, 8, 16, 16]), io4, ALU.is_equal, [sel, iota16], [oh_])
                        P.tt(oh_[:], oh_[:], tf4[:, :, src_q, :].unsqueeze(2).to_broadcast([128, 8, 16, 16]), ALU.mult, [oh_, tf_], [oh_])
                        P.red(dsts[:], oh_[:], ALU.add, [oh_], [dsts])
                    P.stt(s1[:], s1[:], 128.0, s2[:], ALU.mult, ALU.add, [s1, s2], [s1])
                    e_ = ei[k]
                    P.cp(e_[:], s1[:].rearrange("p h k -> p (h k)"), [s1], [e_])
                    m_ = mx[k]; p_ = pw[k]; z_ = sm[k]
                    P.red(m_[:], bv_[:], ALU.max, bvk, [m_])
                    P.tt(p_[:], bv_[:], m_[:].unsqueeze(2).to_broadcast([128, 8, 16]), ALU.subtract, bvk + [m_], [p_])
                    P.act(p_[:], p_[:], AF.Exp, [p_], [p_])
                    P.red(z_[:], p_[:], ALU.add, [p_], [z_])
                    P.op("dve", (lambda zz: (lambda e: e.reciprocal(out=zz, in_=zz)))(z_[:]), [z_], [z_])
                    P.tt(p_[:], p_[:], z_[:].unsqueeze(2).to_broadcast([128, 8, 16]), ALU.mult, [p_, z_], [p_])
                    yield

                def p2_tile(i):
                    k = i % 2
                    ix = ei[k]; p_ = pw[k]; n_ = hn[k]; x_ = xt[k]; a_ = acc[0]
                    pwv = p_[:].rearrange("p h k -> p (h k)")
                    pacc = (PS[6], PS[7])
                    nch = 128 // RC
                    for c in range(nch):
                        uc = cnt["uc"]; cnt["uc"] += 1
                        g_ = ug[uc]; u_ = ux[uc]; d_ = dg[uc]
                        for rr in range(RC):
                            P.gather(g_[:, rr, :], UVf, ix[:, c * RC + rr: c * RC + rr + 1], R=[ix], W=[(g_.key, rr)])
                        P.memset(u_[:], 0.0, [u_], eng="dve")
                        for rr in range(RC):
                            cnt["jc"] += 1
                            P.op("dve", (lambda o, a0, b0, ac: (lambda e: e.scalar_tensor_tensor(out=o, in0=a0, scalar=1.0, in1=b0, op0=ALU.mult, op1=ALU.mult, accum_out=ac)))(junk[:], g_[:, rr, 0:D], n_[:], u_[:, rr:rr + 1]),
                                 [(g_.key, rr), n_, u_], [("junk", cnt["jc"]), (u_.key, rr)])
                        uk = [(u_.key, rr) for rr in range(RC)]
                        P.act(u_[:], u_[:], AF.Gelu, uk + [u_], [u_])
                        P.tt(u_[:], u_[:], pwv[:, c * RC:(c + 1) * RC], ALU.mult, [u_, p_], [u_])
                        for rr in range(RC):
                            P.act(d_[:, rr, :], identb[:], AF.Copy, [identb, u_], [(d_.key, rr)], scale=u_[:, rr:rr + 1])
                        for rr in range(RC):
                            first = (c == 0 and rr == 0); last = (c == nch - 1 and rr == RC - 1)
                            for half in range(2):
                                P.mm(pacc[half][:, :], d_[:, rr, :], g_[:, rr, D + half * 512: D + (half + 1) * 512], first, last, [(d_.key, rr), (g_.key, rr)], [pacc[half]])
                        yield
                    for half in range(2):
                        P.tt(a_[:, half * 512:(half + 1) * 512], pacc[half][:, :], g2bct[:, half * 512:(half + 1) * 512], ALU.mult, [pacc[half], g2bct], [(a_.key, half)])
                    P.tt(x_[:], x_[:], a_[:], ALU.add, [x_, (a_.key, 0), (a_.key, 1)], [x_])
                    P.dma("sp", XS[i * 128:(i + 1) * 128, :], x_[:], R=[x_], W=[("XS", i)])

                from itertools import zip_longest

                def run_pair(g1, g2_):
                    for _ in zip_longest(g1 if g1 is not None else (), g2_ if g2_ is not None else ()):
                        pass

                load_mods(1)
                for i in range(NCT):
                    run_pair(p1_tile(i), p2_tile(i - 1) if i >= 1 else None)
                run_pair(None, p2_tile(NCT - 1))
                load_mods(0)
                for i in range(NCT, NT):
                    run_pair(p1_tile(i), p2_tile(i - 1) if i > NCT else None)
                run_pair(None, p2_tile(NT - 1))
                P.emit()

        P.barrier()
        with ExitStack() as st:
            fg = load_bc(st, "fg", final_g[0, :], D)
            xt = ring(st, "xt5", 2, [128, D]); jn = ring(st, "jn", 2, [128, D]); ssr = ring(st, "ss5", 2, [128, 1]); rsr = ring(st, "rs5", 2, [128, 1])
            for i in range(NCT, NT):
                k = i % 2; x_ = xt[k]; j_ = jn[k]; ss = ssr[k]; rs = rsr[k]
                P.dma("sp", x_[:], XS[i * 128:(i + 1) * 128, :], R=[("XS", i)], W=[x_])
                rms_rstd(x_, j_, ss, rs)
                P.ts(j_[:], x_[:], rs[:], None, ALU.mult, None, [x_, rs], [j_])
                P.tt(j_[:], j_[:], fg[:], ALU.mult, [j_, fg], [j_])
                P.dma("sp", out[(i - NCT) * 128:(i - NCT + 1) * 128, :], j_[:], R=[j_], W=[("out", i)])
            P.emit(final=True)
    return nc


_CACHE = {}


def make_in_maps(inputs, T, CT, L, nb):
    pe = _pos_embed(T)
    gxx, fsx = _consts(T)
    gxc, fsc = _consts(CT)
    cc_ = np.arange(128, dtype=np.float64)
    ang = 2 * np.pi * ((cc_[:, None] * cc_[None, :]) % 128) / 128
    cdft = np.concatenate([np.cos(ang), -np.sin(ang)], axis=1).astype(np.float32)
    f = lambda a: np.ascontiguousarray(np.asarray(a, dtype=np.float32))
    shared = {
        "pe": pe, "w_mod": f(inputs["w_mod"])[:L], "b_mod": f(inputs["b_mod"])[:L],
        "norm1_g": f(inputs["norm1_g"])[:L], "norm2_g": f(inputs["norm2_g"])[:L],
        "w_in": f(inputs["w_in"])[:L], "b_in": f(inputs["b_in"])[:L], "conv_qk": f(inputs["conv_qk"])[:L],
        "mlstm_norm_g": f(inputs["mlstm_norm_g"])[:L], "sgu_w": f(inputs["sgu_w"])[:L],
        "sgu_b": f(inputs["sgu_b"])[:L].reshape(L, 512), "w_br": f(inputs["w_br"])[:L], "w_out": f(inputs["w_out"])[:L],
        "peer_wq": f(inputs["peer_wq"])[:L], "peer_keys": f(inputs["peer_keys"])[:L],
        "peer_u": f(inputs["peer_u"])[:L], "peer_v": f(inputs["peer_v"])[:L],
        "final_g": f(inputs["final_g"]).reshape(1, D), "cdft": cdft,
        "gx_x": gxx, "fs_x": fsx, "gx_c": gxc, "fs_c": fsc,
    }
    x = f(inputs["x"]); c = f(inputs["c"]); ctx = f(inputs["ctx"]); c_ctx = f(inputs["c_ctx"])
    maps = []
    for b in range(nb):
        m = dict(shared)
        m["x"] = x[b]; m["ctx"] = ctx[b]
        m["cc"] = np.ascontiguousarray(np.stack([c[b], c_ctx], axis=0))
        maps.append(m)
    return maps


def kernel(**inputs):
    x = np.asarray(inputs["x"])
    B, T, _ = x.shape
    CT = np.asarray(inputs["ctx"]).shape[1]
    L = np.asarray(inputs["w_mod"]).shape[0]
    key = (T, CT, L)
    if key not in _CACHE:
        _CACHE[key] = build(T, CT, L)
    nc = _CACHE[key]
    maps = make_in_maps(inputs, T, CT, L, B)
    res = run_bass_kernel_spmd(nc, maps, core_ids=list(range(B)))
    return np.stack([res.results[b]["out"] for b in range(B)], axis=0).astype(np.float32)
```

```python
import math
import numpy as np
from contextlib import ExitStack
import concourse.bass as bass
import concourse.mybir as mybir
from concourse.bass_utils import run_bass_kernel_spmd

F32 = mybir.dt.float32
BF16 = mybir.dt.bfloat16
I32 = mybir.dt.int32
U32 = mybir.dt.uint32
AF = mybir.ActivationFunctionType
ALU = mybir.AluOpType
AX = mybir.AxisListType
ENGS = ("sp", "act", "dve", "pool", "pe")

D = 1024
PW = 6672
OQ, OK_, OV, OO, OG, OSU, OSV, OFZ, OMG = 0, 512, 1024, 1536, 2048, 2064, 2576, 3088, 3600
EPS = 1e-6
NEXP = 16384
NSLOT = 24


class Tile:
    def __init__(self, t, key):
        self.t = t
        self.key = key

    def __getitem__(self, idx):
        return self.t[idx]


class Prog:
    def __init__(self, nc, csem, dsem):
        self.nc = nc
        self.csem, self.dsem = csem, dsem
        self.streams = {e: [] for e in ENGS}
        self.ccount = {e: 0 for e in ENGS}
        self.dcount = {e: 0 for e in ENGS}
        self.waited = {e: {} for e in ENGS}
        self.lastw = {}
        self.reads = {}

    def _issue(self, eng, fn, reads, writes, is_dma):
        reads = [getattr(r, "key", r) for r in reads]
        writes = [getattr(w, "key", w) for w in writes]
        deps = {}
        def add(k, n):
            if deps.get(k, 0) < n:
                deps[k] = n
        for r in reads:
            w = self.lastw.get(r)
            if w: add((w[0], w[1]), w[2])
        for wkey in writes:
            w = self.lastw.get(wkey)
            if w: add((w[0], w[1]), w[2])
            for k, n in self.reads.get(wkey, {}).items():
                add(k, n)
        waits = []
        wd = self.waited[eng]
        for k, n in deps.items():
            if k == ("c", "pe") and eng == "pe" and not is_dma:
                continue
            if wd.get(k, 0) >= n:
                continue
            wd[k] = n
            waits.append((k[0], k[1], n))
        slot = None
        if is_dma:
            self.dcount[eng] += 1
            n = self.dcount[eng]
            slot = (n - 1) % NSLOT
            me = ("d", (eng, slot), (n - 1) // NSLOT + 1)
        else:
            self.ccount[eng] += 1
            me = ("c", eng, self.ccount[eng])
        self.streams[eng].append((waits, fn, is_dma, slot))
        for r in reads:
            self.reads.setdefault(r, {})[(me[0], me[1])] = me[2]
        for w in writes:
            self.lastw[w] = me
            self.reads[w] = {}

    def op(self, eng, fn, R=(), W=()):
        self._issue(eng, fn, R, W, False)

    def dma(self, eng, out, in_, R=(), W=(), **kw):
        self._issue(eng, lambda e: e.dma_start(out=out, in_=in_, **kw), R, W, True)

    def gather(self, out, table, idx_ap, R=(), W=()):
        self._issue("pool", lambda e: e.indirect_dma_start(
            out=out, out_offset=None, in_=table,
            in_offset=bass.IndirectOffsetOnAxis(ap=idx_ap, axis=0)), R, W, True)

    def act(self, out, in_, func, R, W, eng="act", **kw):
        self.op(eng, lambda e: e.activation(out=out, in_=in_, func=func, **kw), R, W)

    def tt(self, out, in0, in1, op, R, W, eng="dve"):
        self.op(eng, lambda e: e.tensor_tensor(out=out, in0=in0, in1=in1, op=op), R, W)

    def ts(self, out, in0, s1, s2, op0, op1, R, W, eng="dve"):
        if s2 is None:
            self.op(eng, lambda e: e.tensor_scalar(out=out, in0=in0, scalar1=s1, scalar2=None, op0=op0), R, W)
        else:
            self.op(eng, lambda e: e.tensor_scalar(out=out, in0=in0, scalar1=s1, scalar2=s2, op0=op0, op1=op1), R, W)

    def stt(self, out, in0, scalar, in1, op0, op1, R, W, eng="dve"):
        self.op(eng, lambda e: e.scalar_tensor_tensor(out=out, in0=in0, scalar=scalar, in1=in1, op0=op0, op1=op1), R, W)

    def cp(self, out, in_, R, W, eng="dve"):
        self.op(eng, lambda e: e.tensor_copy(out=out, in_=in_), R, W)

    def red(self, out, in_, op, R, W, eng="dve", axis=AX.X):
        self.op(eng, lambda e: e.tensor_reduce(out=out, in_=in_, axis=axis, op=op), R, W)

    def mm(self, out, lhsT, rhs, start, stop, R, W):
        self.op("pe", lambda e: e.matmul(out, lhsT=lhsT, rhs=rhs, start=start, stop=stop), R, W)

    def tr(self, out, in_, ident, R, W):
        self.op("pe", lambda e: e.transpose(out, in_, ident), R, W)

    def memset(self, ap, val, W, eng="pool"):
        self.op(eng, lambda e: e.memset(ap, val), (), W)

    def barrier(self):
        for e in ENGS:
            waits = []
            wd = self.waited[e]
            for E in ENGS:
                if self.ccount[E] and not (E == e) and wd.get(("c", E), 0) < self.ccount[E]:
                    wd[("c", E)] = self.ccount[E]
                    waits.append(("c", E, self.ccount[E]))
                n = self.dcount[E]
                for slot in range(min(n, NSLOT)):
                    cnt = (n - 1 - slot) // NSLOT + 1
                    if wd.get(("d", (E, slot)), 0) < cnt:
                        wd[("d", (E, slot))] = cnt
                        waits.append(("d", (E, slot), cnt))
            if waits:
                self.streams[e].append((waits, None, False, None))

    def emit(self, final=False):
        nc = self.nc
        csem, dsem = self.csem, self.dsem
        if final:
            self.barrier()
        with nc.Block() as block:
            engobj = {"sp": block.sync, "act": block.scalar, "dve": block.vector,
                      "pool": block.gpsimd, "pe": block.tensor}

            def mk(ename):
                stream = self.streams[ename]
                def body(eng):
                    for waits, fn, is_dma, slot in stream:
                        for kind, E, n in waits:
                            if kind == "c":
                                eng.wait_ge(csem[E], n)
                            else:
                                eng.wait_ge(dsem[E[0]][E[1]], 16 * n)
                        if fn is None:
                            continue
                        inst = fn(eng)
                        if is_dma:
                            inst.then_inc(dsem[ename][slot], 16)
                        else:
                            inst.then_inc(csem[ename], 1)
                return body
            for e in ENGS:
                if self.streams[e]:
                    engobj[e](mk(e))
        self.streams = {e: [] for e in ENGS}


def _consts(T):
    N1 = T // 128
    n2 = np.arange(128)[:, None].astype(np.float64)
    k2 = np.arange(128)[None, :].astype(np.float64)
    gx = np.zeros((N1, 128, 3, 128), np.float32)
    for n1 in range(N1):
        ang = 2 * np.pi * (((N1 * n2 + n1) * k2) % T) / T
        gx[n1, :, 0, :] = np.cos(ang)
        gx[n1, :, 1, :] = np.sin(ang)
        gx[n1, :, 2, :] = -np.sin(ang)
    a = np.arange(N1)[:, None].astype(np.float64)
    b = np.arange(N1)[None, :].astype(np.float64)
    ang = 2 * np.pi * ((a * b) % N1) / N1
    fs = np.concatenate([np.cos(ang), np.sin(ang)], axis=0) / np.sqrt(T * 128.0)
    return gx, fs.astype(np.float32)


def _pos_embed(T):
    quarter = D // 4
    omega = (1.0 / (10000.0 ** (np.arange(quarter, dtype=np.float32) / np.float32(quarter)))).astype(np.float32)
    rows = T // 64
    r = np.repeat(np.arange(rows, dtype=np.float32), 64)[:, None] * omega
    cc = np.tile(np.arange(64, dtype=np.float32), rows)[:, None] * omega
    return np.concatenate([np.sin(r), np.cos(r), np.sin(cc), np.cos(cc)], axis=-1).astype(np.float32)


def build(T, CT, L, dbg=()):
    TT = T + CT
    NT = TT // 128
    NCT = CT // 128
    nc = bass.Bass("TRN2", target_bir_lowering=False)

    def din(name, shape, dt=F32):
        return nc.dram_tensor(name, list(shape), dt, kind="ExternalInput").ap()

    def dscr(name, shape, dt=F32):
        kind = "ExternalOutput" if name in dbg else "Internal"
        return nc.dram_tensor(name, list(shape), dt, kind=kind).ap()

    x_in = din("x", [T, D]); pe = din("pe", [T, D]); ctx_in = din("ctx", [CT, D]); cc = din("cc", [2, D])
    w_mod = din("w_mod", [L, D, 6 * D]); b_mod = din("b_mod", [L, 6 * D])
    norm1_g = din("norm1_g", [L, D]); norm2_g = din("norm2_g", [L, D])
    w_in = din("w_in", [L, D, PW]); b_in = din("b_in", [L, PW])
    conv_qk = din("conv_qk", [L, 3, 1024]); mng = din("mlstm_norm_g", [L, 512])
    sgu_w = din("sgu_w", [L, 4, 128, 128]); sgu_b = din("sgu_b", [L, 512])
    w_br = din("w_br", [L, 3, 512, D]); w_out = din("w_out", [L, D, D])
    peer_wq = din("peer_wq", [L, D, 2048]); peer_keys = din("peer_keys", [L, 2, 128, 128])
    peer_u = din("peer_u", [L, NEXP, D]); peer_v = din("peer_v", [L, NEXP, D])
    final_g = din("final_g", [1, D])
    cdft = din("cdft", [128, 256])
    gx_x = din("gx_x", [T // 128, 128, 3, 128]); fs_x = din("fs_x", [2 * (T // 128), T // 128])
    gx_c = din("gx_c", [CT // 128, 128, 3, 128]); fs_c = din("fs_c", [2 * (CT // 128), CT // 128])
    out = nc.dram_tensor("out", [T, D], F32, kind="ExternalOutput").ap()

    XS = dscr("XS", [TT, D])
    QT = dscr("QT", [512, TT]); KT = dscr("KT", [512, TT]); SUT = dscr("SUT", [512, TT]); FZT = dscr("FZT", [512, TT])
    QC = dscr("QC", [512, TT]); KC = dscr("KC", [512, TT]); KTOK = dscr("KTOK", [TT, 512])
    V = dscr("V", [TT, 512]); O = dscr("O", [TT, 512]); SV = dscr("SV", [TT, 512]); G = dscr("G", [TT, 16])
    MG = dscr("MG", [TT, 3072])
    HF = dscr("HF", [TT, 512]); HB = dscr("HB", [TT, 512])
    B0 = dscr("B0", [TT, 512]); B1T = dscr("B1T", [512, TT]); B2 = dscr("B2", [TT, 512])
    Z = dscr("Z", [2, TT, 512]); APDX = dscr("APD", [2, T // 128, 128, 512]); APDC = dscr("APDC", [2, CT // 128, 128, 512])
    MOD = dscr("MOD", [L, 2, 6 * D])
    UV = dscr("UV", [NEXP, 2, D], BF16)
    IDX = dscr("IDX", [TT, 128], I32); PWT = dscr("PWT", [TT, 128]); HN = dscr("HN", [TT, D])

    es = ExitStack()
    with es:
        es.enter_context(nc.allow_non_contiguous_dma(reason="small strided parameter loads"))
        es.enter_context(nc.allow_low_precision(reason="bf16 matmul operands, fp32 accumulate"))
        csem = {e: es.enter_context(nc.semaphore("c_" + e)) for e in ENGS}
        dsem = {e: [es.enter_context(nc.semaphore(f"d_{e}{i}")) for i in range(NSLOT)] for e in ("sp", "pool")}
        P = Prog(nc, csem, dsem)

        uniq = [0]

        def sbt(st, name, shape, dt=F32):
            uniq[0] += 1
            name = f"{name}_{uniq[0]}"
            return Tile(st.enter_context(nc.sbuf_tensor(name, list(shape), dt)), name)

        class Ring(list):
            def __getitem__(self, i):
                return list.__getitem__(self, i % len(self))

        def ring(st, name, n, shape, dt=F32):
            return Ring([sbt(st, f"{name}{i}", shape, dt) for i in range(n)])

        PS = [Tile(es.enter_context(nc.psum_tensor(f"ps{i}", [128, 512], F32)), f"ps{i}") for i in range(8)]
        ident = sbt(es, "ident", [128, 128]); maskU = sbt(es, "maskU", [128, 128]); maskL = sbt(es, "maskL", [128, 128])
        ones = sbt(es, "ones", [128, 128]); iota16 = sbt(es, "iota16", [128, 16])
        Cf = [sbt(es, f"Cf{d}", [128, 4, 129]) for d in range(2)]
        Cb = [sbt(es, f"Cb{d}", [128, 4, 130], BF16) for d in range(2)]
        cdb = sbt(es, "cdb", [128, 256], BF16)
        identb = sbt(es, "identb", [128, 128], BF16)

        def ktiles(name, t0, t1):
            return [(name, i) for i in range(t0 // 128, (t1 + 127) // 128)]

        with ExitStack() as st:
            P.memset(ident[:], 0.0, [ident]); P.memset(maskU[:], 1.0, [maskU]); P.memset(maskL[:], 1.0, [maskL])
            P.memset(ones[:], 1.0, [ones])
            P.op("pool", lambda e: e.affine_select(out=ident[:], in_=ident[:], pattern=[[-1, 128]], compare_op=ALU.not_equal, fill=1.0, base=0, channel_multiplier=1), [ident], [ident])
            P.op("pool", lambda e: e.affine_select(out=maskU[:], in_=maskU[:], pattern=[[1, 128]], compare_op=ALU.is_ge, fill=0.0, base=0, channel_multiplier=-1), [maskU], [maskU])
            P.op("pool", lambda e: e.affine_select(out=maskL[:], in_=maskL[:], pattern=[[-1, 128]], compare_op=ALU.is_ge, fill=0.0, base=0, channel_multiplier=1), [maskL], [maskL])
            P.op("pool", lambda e: e.iota(iota16[:], pattern=[[1, 16]], base=0, channel_multiplier=0, allow_small_or_imprecise_dtypes=True), (), [iota16])
            P.cp(identb[:], ident[:], [ident], [identb], eng="pool")
            cdf = sbt(st, "cdf", [128, 256])
            P.dma("sp", cdf[:], cdft[:, :], W=[cdf])
            P.cp(cdb[:], cdf[:], [cdf], [cdb])
            xa = ring(st, "xa", 2, [128, D]); xb = ring(st, "xb", 2, [128, D])
            for i in range(NT):
                a = xa[i % 2]; b = xb[i % 2]
                if i < NCT:
                    P.dma("sp", a[:], ctx_in[i * 128:(i + 1) * 128, :], W=[a])
                    P.dma("sp", XS[i * 128:(i + 1) * 128, :], a[:], R=[a], W=[("XS", i)])
                else:
                    j = i - NCT
                    P.dma("sp", a[:], x_in[j * 128:(j + 1) * 128, :], W=[a])
                    P.dma("pool", b[:], pe[j * 128:(j + 1) * 128, :], W=[b])
                    P.tt(a[:], a[:], b[:], ALU.add, [a, b], [a])
                    P.dma("sp", XS[i * 128:(i + 1) * 128, :], a[:], R=[a], W=[("XS", i)])
            P.emit()

        def load_cols(st, name, src_row, eng="sp"):
            t = sbt(st, name, [128, 8])
            P.dma(eng, t[:], src_row.rearrange("(j p) -> p j", p=128), W=[t])
            return t

        def load_bc(st, name, src_row, n, eng="sp"):
            t = sbt(st, name, [128, n])
            P.dma(eng, t[:], src_row.partition_broadcast(128), W=[t])
            return t

        def rsqrt_(ap, keys):
            P.act(ap, ap, AF.Sqrt, keys, keys)
            P.op("dve", lambda e: e.reciprocal(out=ap, in_=ap), keys, keys)

        def rms_rstd(xt, junk, ss, rstd):
            P.act(junk[:], xt[:], AF.Square, [xt], [junk, ss], accum_out=ss[:])
            P.ts(rstd[:], ss[:], 1.0 / D, EPS, ALU.mult, ALU.add, [ss], [rstd])
            rsqrt_(rstd[:], [rstd])

        for l in range(L):
            P.barrier()
            with ExitStack() as st:
                scT = sbt(st, "scT", [128, 2, 8])
                for r in range(2):
                    P.dma("sp", scT[:, r, :], cc[r, :].rearrange("(j p) -> p j", p=128), W=[scT])
                P.act(scT[:], scT[:], AF.Silu, [scT], [scT])
                wm = ring(st, "wm", 2, [128, 8, 512]); bm = ring(st, "bm", 2, [2, 512]); mo = ring(st, "mo", 2, [2, 512])
                for blk in range(12):
                    w = wm[blk % 2]; bb = bm[blk % 2]; m = mo[blk % 2]; ps = PS[blk % 2]
                    P.dma("sp" if blk % 2 == 0 else "pool", w[:], w_mod[l].rearrange("(j p) n -> p j n", p=128)[:, :, blk * 512:(blk + 1) * 512], W=[w])
                    P.dma("sp", bb[:], b_mod[l, blk * 512:(blk + 1) * 512].partition_broadcast(2), W=[bb])
                    for j in range(8):
                        P.mm(ps[0:2, :], scT[:, :, j], w[:, j, :], j == 0, j == 7, [scT, w], [ps])
                    P.tt(m[:], ps[0:2, :], bb[:], ALU.add, [ps, bb], [m])
                    P.dma("sp", MOD[l, :, blk * 512:(blk + 1) * 512], m[:], R=[m], W=["MOD"])
                P.emit()

            def modcols(st, name, m, r):
                return load_cols(st, name, MOD[l, r, m * D:(m + 1) * D])

            def modbc(st, name, m, r):
                return load_bc(st, name, MOD[l, r, m * D:(m + 1) * D], D)

            P.barrier()
            with ExitStack() as st:
                winb = sbt(st, "winb", [128, 8, PW], BF16)
                wst = ring(st, "wst", 2, [128, 8, 512])
                nblk = (PW + 511) // 512
                for blk in range(nblk):
                    c0 = blk * 512; wd = min(512, PW - c0); w = wst[blk % 2]
                    P.dma("sp" if blk % 2 == 0 else "pool", w[:, :, 0:wd], w_in[l].rearrange("(j p) n -> p j n", p=128)[:, :, c0:c0 + wd], W=[w])
                    if blk % 2 == 0:
                        P.cp(winb[:, :, c0:c0 + wd], w[:, :, 0:wd], [w], [("winb", blk)])
                    else:
                        P.act(winb[:, :, c0:c0 + wd], w[:, :, 0:wd], AF.Copy, [w], [("winb", blk)])
                winb_keys = [("winb", b) for b in range(nblk)]
                bbc = load_bc(st, "bbc", b_in[l, :], PW, eng="pool")
                bcol = sbt(st, "bcol", [128, 16])
                for fi, off in enumerate((OQ, OK_, OSU, OFZ)):
                    P.dma("sp", bcol[:, fi * 4:(fi + 1) * 4], b_in[l, off:off + 512].rearrange("(i p) -> p i", p=128), W=[("bcol", fi)])
                bcol_keys = [("bcol", fi) for fi in range(4)]
                g1 = load_cols(st, "g1", norm1_g[l, :])
                Acol = []; Bcol = []
                for r in range(2):
                    sc = modcols(st, f"sc{r}", 1, r); sh = modcols(st, f"sh{r}", 0, r)
                    P.stt(sc[:], sc[:], 1.0, g1[:], ALU.add, ALU.mult, [sc, g1], [sc])
                    Acol.append(sc); Bcol.append(sh)
                xt = ring(st, "xt", 2, [128, D]); xn = ring(st, "xn", 2, [128, D]); ssr = ring(st, "ss", 2, [128, 1]); rsr = ring(st, "rs", 2, [128, 1])
                hT = ring(st, "hT", 2, [128, 8, 128], BF16)
                fo = ring(st, "fo", 4, [128, 4, 128]); to = ring(st, "to", 4, [128, 512]); go = ring(st, "go", 2, [128, 16])
                pcnt = [0]
                def nextps():
                    pcnt[0] += 1
                    return PS[2 + pcnt[0] % 6]
                fcnt = [0]; tcnt = [0]
                for i in range(NT):
                    r = 1 if i < NCT else 0
                    x_ = xt[i % 2]; n_ = xn[i % 2]; ss = ssr[i % 2]; rs = rsr[i % 2]; h_ = hT[i % 2]
                    P.dma("sp", x_[:], XS[i * 128:(i + 1) * 128, :], R=[("XS", i)], W=[x_])
                    rms_rstd(x_, n_, ss, rs)
                    P.ts(n_[:], x_[:], rs[:], None, ALU.mult, None, [x_, rs], [n_])
                    for half in range(2):
                        ps = PS[half]
                        for jj in range(4):
                            j = half * 4 + jj
                            P.tr(ps[:, jj * 128:(jj + 1) * 128], n_[:, j * 128:(j + 1) * 128], ident[:], [n_, ident], [ps])
                        for jj in range(4):
                            j = half * 4 + jj
                            P.act(h_[:, j, :], ps[:, jj * 128:(jj + 1) * 128], AF.Identity, [ps, Acol[r], Bcol[r]], [h_],
                                  scale=Acol[r][:, j:j + 1], bias=Bcol[r][:, j:j + 1])
                    for fi, (off, dst, dname) in enumerate(((OQ, QT, "QT"), (OK_, KT, "KT"), (OSU, SUT, "SUT"), (OFZ, FZT, "FZT"))):
                        ps = nextps(); f_ = fo[fcnt[0] % 4]; fcnt[0] += 1
                        for cb in range(4):
                            for j in range(8):
                                P.mm(ps[:, cb * 128:(cb + 1) * 128], winb[:, j, off + cb * 128: off + (cb + 1) * 128], h_[:, j, :], j == 0, j == 7, [h_] + winb_keys, [ps])
                        for cb in range(4):
                            P.act(f_[:, cb, :], ps[:, cb * 128:(cb + 1) * 128], AF.Identity, [ps] + bcol_keys, [f_], bias=bcol[:, fi * 4 + cb: fi * 4 + cb + 1])
                        P.dma("sp", dst.rearrange("(j c) t -> c j t", c=128)[:, :, i * 128:(i + 1) * 128], f_[:], R=[f_], W=[(dname, i)])
                    tm = [(OV, V, "V", 0), (OO, O, "O", 0), (OSV, SV, "SV", 0)] + [(OMG + 512 * m, MG, "MG", 512 * m) for m in range(6)]
                    for (off, dst, dname, dcol) in tm:
                        ps = nextps(); t_ = to[tcnt[0] % 4]; tcnt[0] += 1
                        for j in range(8):
                            P.mm(ps[:, :], h_[:, j, :], winb[:, j, off:off + 512], j == 0, j == 7, [h_] + winb_keys, [ps])
                        P.tt(t_[:], ps[:, :], bbc[:, off:off + 512], ALU.add, [ps, bbc], [t_])
                        P.dma("pool", dst[i * 128:(i + 1) * 128, dcol:dcol + 512], t_[:], R=[t_], W=[(dname, i, dcol)])
                    ps = nextps(); g_ = go[i % 2]
                    for j in range(8):
                        P.mm(ps[:, 0:16], h_[:, j, :], winb[:, j, OG:OG + 16], j == 0, j == 7, [h_] + winb_keys, [ps])
                    P.tt(g_[:], ps[:, 0:16], bbc[:, OG:OG + 16], ALU.add, [ps, bbc], [g_])
                    P.dma("pool", G[i * 128:(i + 1) * 128, :], g_[:], R=[g_], W=[("G", i)])
                P.emit()

            segs = [(0, CT), (CT, T)]

            P.barrier()
            with ExitStack() as st:
                WP = 1024
                xin = ring(st, "cin", 2, [128, WP + 2]); t1 = ring(st, "ct1", 2, [128, WP]); kt = ring(st, "ckt", 2, [128, WP // 128, 128])
                cw = ring(st, "cw", 2, [128, 3])
                it = 0
                for (src, dst, sname, dname, coff, isk) in ((QT, QC, "QT", "QC", 0, False), (KT, KC, "KT", "KC", 512, True)):
                    for cb in range(4):
                        w_ = cw[(it // 1) % 2]
                        for (s0, sl) in segs:
                            for p0 in range(0, sl, WP):
                                wlen = min(WP, sl - p0)
                                xi = xin[it % 2]; t_ = t1[it % 2]; k_ = kt[it % 2]
                                if p0 == 0 and s0 == 0:
                                    pass
                                a0 = s0 + p0
                                lo = 1 if p0 == 0 else 0
                                hi = 1 if p0 + wlen >= sl else 0
                                if lo:
                                    P.memset(xi[:, 0:1], 0.0, [xi], eng="dve")
                                if hi:
                                    P.memset(xi[:, wlen + 1:wlen + 2], 0.0, [xi], eng="dve")
                                P.dma("sp", xi[:, lo:wlen + 2 - hi], src[cb * 128:(cb + 1) * 128, a0 - 1 + lo:a0 + wlen + 1 - hi],
                                      R=ktiles(sname, a0 - 1 + lo, a0 + wlen + 1 - hi), W=[xi])
                                if True:
                                    P.dma("pool", w_[:], conv_qk[l, :, coff + cb * 128: coff + (cb + 1) * 128].rearrange("k c -> c k"), W=[w_])
                                P.ts(t_[:, 0:wlen], xi[:, 1:wlen + 1], w_[:, 1:2], None, ALU.mult, None, [xi, w_], [t_])
                                P.stt(t_[:, 0:wlen], xi[:, 0:wlen], w_[:, 0:1], t_[:, 0:wlen], ALU.mult, ALU.add, [xi, w_, t_], [t_])
                                P.stt(t_[:, 0:wlen], xi[:, 2:wlen + 2], w_[:, 2:3], t_[:, 0:wlen], ALU.mult, ALU.add, [xi, w_, t_], [t_])
                                P.act(t_[:, 0:wlen], t_[:, 0:wlen], AF.Silu, [t_], [t_])
                                P.dma("sp", dst[cb * 128:(cb + 1) * 128, a0:a0 + wlen], t_[:, 0:wlen], R=[t_], W=ktiles(dname + str(cb), a0, a0 + wlen))
                                if isk:
                                    nch = wlen // 128
                                    for c4 in range(0, nch, 4):
                                        ps = PS[(c4 // 4) % 4]
                                        for q in range(min(4, nch - c4)):
                                            P.tr(ps[:, q * 128:(q + 1) * 128], t_[:, (c4 + q) * 128:(c4 + q + 1) * 128], ident[:], [t_, ident], [ps])
                                        nq = min(4, nch - c4)
                                        P.act(k_[:, c4:c4 + nq, :], ps[:, 0:nq * 128].rearrange("p (q c) -> p q c", c=128), AF.Copy, [ps], [k_])
                                    P.dma("pool", KTOK[a0:a0 + wlen, cb * 128:(cb + 1) * 128].rearrange("(n t) c -> t n c", t=128), k_[:, 0:nch, :], R=[k_], W=ktiles("KTOK" + str(cb), a0, a0 + wlen))
                                it += 1
                P.emit()

            P.barrier()
            with ExitStack() as st:
                gt = ring(st, "gt", 2, [128, 16]); e1 = ring(st, "e1", 2, [128, 4]); sp_ = ring(st, "spl", 2, [128, 4])
                al = ring(st, "al", 2, [128, 4]); be = ring(st, "be", 2, [128, 4]); et = ring(st, "et", 2, [128, 4]); tmp4 = ring(st, "tmp4", 2, [128, 4])
                qf = ring(st, "qf", 2, [128, 4, 128]); kf = ring(st, "kf", 2, [128, 4, 128]); ktk = ring(st, "ktk", 2, [128, 512]); vf = ring(st, "vf", 2, [128, 512])
                qb = ring(st, "qb", 2, [128, 4, 128], BF16); kb = ring(st, "kb", 2, [128, 4, 128], BF16); ktb = ring(st, "ktb", 2, [128, 512], BF16)
                vb = ring(st, "vb", 2, [128, 4, 130], BF16); pt = ring(st, "pt", 2, [128, 4, 128], BF16)
                hh = ring(st, "hh", 2, [128, 4, 128]); d1 = ring(st, "d1", 2, [128, 4])
                lnb = sbt(st, "lnb", [128, 1])
                P.memset(lnb[:], -0.5 * math.log(128.0), [lnb])
                def scan_gen(dr):
                    mask = maskU if dr == 0 else maskL
                    Hd, hname = (HF, "HF") if dr == 0 else (HB, "HB")
                    fcol = 4 if dr == 0 else 12
                    icol = 0 if dr == 0 else 8
                    order = []
                    for (s0, sl) in segs:
                        ch = list(range(s0 // 128, (s0 + sl) // 128))
                        order.append(ch if dr == 0 else ch[::-1])
                    for si, chs in enumerate(order):
                        if si == 0:
                            P.memset(Cf[dr][:], 0.0, [Cf[dr]], eng="dve")
                            P.memset(Cb[dr][:], 0.0, [Cb[dr]], eng="dve")
                        for c in chs:
                            k = dr
                            g_ = gt[k]; e_ = e1[k]; s_ = sp_[k]; a_ = al[k]; b_ = be[k]; t_ = et[k]; m4 = tmp4[k]
                            r0 = c * 128
                            P.dma("sp", g_[:], G[r0:r0 + 128, :], R=[("G", c)], W=[g_])
                            P.act(e_[:], g_[:, fcol:fcol + 4], AF.Exp, [g_], [e_], scale=-1.0)
                            P.act(s_[:], e_[:], AF.Ln, [e_], [s_], bias=1.0)
                            psg = PS[7]
                            P.mm(psg[:, 0:4], mask[:], s_[:], True, True, [mask, s_], [psg])
                            P.mm(psg[:, 4:8], ones[:], s_[:], True, True, [ones, s_], [psg])
                            P.act(a_[:], psg[:, 0:4], AF.Exp, [psg, lnb], [a_], scale=-1.0, bias=lnb[:])
                            P.tt(m4[:], psg[:, 0:4], g_[:, icol:icol + 4], ALU.add, [psg, g_], [m4])
                            P.act(b_[:], m4[:], AF.Exp, [m4], [b_])
                            P.act(t_[:], psg[:, 4:8], AF.Exp, [psg], [t_], scale=-1.0)
                            q_ = qf[k]; k_ = kf[k]; kt_ = ktk[k]; v_ = vf[k]
                            P.dma("sp", q_[:], QC.rearrange("(h d) t -> d h t", d=128)[:, :, r0:r0 + 128], R=[("QC%d" % h, c) for h in range(4)], W=[q_])
                            P.dma("pool", k_[:], KC.rearrange("(h d) t -> d h t", d=128)[:, :, r0:r0 + 128], R=[("KC%d" % h, c) for h in range(4)], W=[k_])
                            P.dma("sp", kt_[:], KTOK[r0:r0 + 128, :], R=[("KTOK%d" % h, c) for h in range(4)], W=[kt_])
                            P.dma("pool", v_[:], V[r0:r0 + 128, :], R=[("V", c, 0)], W=[v_])
                            qb_ = qb[k]; kb_ = kb[k]; ktb_ = ktb[k]; vb_ = vb[k]; pt_ = pt[k]
                            P.act(qb_[:], q_[:], AF.Copy, [q_], [qb_])
                            P.act(kb_[:], k_[:], AF.Copy, [k_], [kb_])
                            P.act(ktb_[:], kt_[:], AF.Copy, [kt_], [ktb_])
                            P.tt(vb_[:, :, 0:128], v_[:].rearrange("p (h e) -> p h e", e=128), b_[:].unsqueeze(2).to_broadcast([128, 4, 128]), ALU.mult, [v_, b_], [vb_])
                            P.cp(vb_[:, :, 128:129], b_[:].unsqueeze(2), [b_], [vb_])
                            pss = PS[0]
                            for h in range(4):
                                P.mm(pss[:, h * 128:(h + 1) * 128], kb_[:, h, :], qb_[:, h, :], True, True, [kb_, qb_], [pss])
                            P.tt(pt_[:], pss[:].rearrange("p (h t) -> p h t", t=128), mask[:].unsqueeze(1).to_broadcast([128, 4, 128]), ALU.mult, [pss, mask], [pt_])
                            acc = [PS[1], PS[2]]; dcp = [PS[3], PS[4]]
                            for h in range(4):
                                a = acc[h // 2]; o_ = (h % 2) * 129
                                P.mm(a[:, o_:o_ + 129], pt_[:, h, :], vb_[:, h, 0:129], True, False, [pt_, vb_], [a])
                                P.mm(a[:, o_:o_ + 129], qb_[:, h, :], Cb[dr][:, h, 0:129], False, True, [qb_, Cb[dr]], [a])
                            for h in range(4):
                                dc = dcp[h // 2]; o_ = (h % 2) * 129
                                P.mm(dc[:, o_:o_ + 129], ktb_[:, h * 128:(h + 1) * 128], vb_[:, h, 0:129], True, True, [ktb_, vb_], [dc])
                            h_ = hh[k]; d_ = d1[k]
                            for hp in range(2):
                                a = acc[hp]; av = a[:, 0:258].rearrange("p (h e) -> p h e", e=129)
                                hs = slice(hp * 2, hp * 2 + 2)
                                P.tt(d_[:, hs], av[:, :, 128], a_[:, hs], ALU.mult, [a, a_], [("d1", k, hp)])
                                P.act(d_[:, hs], d_[:, hs], AF.Abs, [("d1", k, hp)], [("d1", k, hp)])
                                P.ts(d_[:, hs], d_[:, hs], 1.0, None, ALU.max, None, [("d1", k, hp)], [("d1", k, hp)])
                                P.op("dve", (lambda dd: (lambda e: e.reciprocal(out=dd, in_=dd)))(d_[:, hs]), [("d1", k, hp)], [("d1", k, hp)])
                                P.tt(d_[:, hs], d_[:, hs], a_[:, hs], ALU.mult, [("d1", k, hp), a_], [("d1", k, hp)])
                                P.tt(h_[:, hs, :], av[:, :, 0:128], d_[:, hs].unsqueeze(2).to_broadcast([128, 2, 128]), ALU.mult, [a, ("d1", k, hp)], [(h_.key, hp)])
                                dc = dcp[hp]; dv = dc[:, 0:258].rearrange("p (h e) -> p h e", e=129)
                                P.tt(Cf[dr][:, hs, :], dv, Cf[dr][:, hs, :], ALU.add, [dc, Cf[dr]], [Cf[dr]], eng="pool" if False else "dve")
                                P.tt(Cf[dr][:, hs, :], Cf[dr][:, hs, :], t_[:, hs].unsqueeze(2).to_broadcast([128, 2, 129]), ALU.mult, [Cf[dr], t_], [Cf[dr]])
                                P.cp(Cb[dr][:, hs, 0:129], Cf[dr][:, hs, :], [Cf[dr]], [Cb[dr]])
                            P.dma("sp", Hd[r0:r0 + 128, :], h_[:].rearrange("p h e -> p (h e)"), R=[(h_.key, 0), (h_.key, 1)], W=[(hname, c)])
                            yield
                from itertools import zip_longest
                for _ in zip_longest(scan_gen(0), scan_gen(1)):
                    pass
                P.emit()

            P.barrier()
            with ExitStack() as st:
                ngb = load_bc(st, "ngb", mng[l, :], 512)
                hf = ring(st, "hf", 2, [128, 4, 128]); hb_ = ring(st, "hb", 2, [128, 4, 128]); ot = ring(st, "ot", 2, [128, 512])
                sq = ring(st, "sq", 2, [128, 4, 128]); mu = ring(st, "mu", 2, [128, 4]); vr = ring(st, "vr", 2, [128, 4])
                for i in range(NT):
                    k = i % 2; a = hf[k]; b = hb_[k]; o_ = ot[k]; s_ = sq[k]; m_ = mu[k]; v_ = vr[k]
                    P.dma("sp", a[:].rearrange("p h e -> p (h e)"), HF[i * 128:(i + 1) * 128, :], R=[("HF", i)], W=[a])
                    P.dma("pool", b[:].rearrange("p h e -> p (h e)"), HB[i * 128:(i + 1) * 128, :], R=[("HB", i)], W=[b])
                    P.dma("sp", o_[:], O[i * 128:(i + 1) * 128, :], R=[("O", i, 0)], W=[o_])
                    P.tt(a[:], a[:], b[:], ALU.add, [a, b], [a])
                    P.red(m_[:], a[:], ALU.add, [a], [m_])
                    P.ts(m_[:], m_[:], 1.0 / 128, None, ALU.mult, None, [m_], [m_])
                    P.tt(a[:], a[:], m_[:].unsqueeze(2).to_broadcast([128, 4, 128]), ALU.subtract, [a, m_], [a])
                    P.tt(s_[:], a[:], a[:], ALU.mult, [a], [s_])
                    P.red(v_[:], s_[:], ALU.add, [s_], [v_])
                    P.ts(v_[:], v_[:], 1.0 / 128, EPS, ALU.mult, ALU.add, [v_], [v_])
                    rsqrt_(v_[:], [v_])
                    P.tt(a[:], a[:], v_[:].unsqueeze(2).to_broadcast([128, 4, 128]), ALU.mult, [a, v_], [a])
                    P.act(o_[:], o_[:], AF.Sigmoid, [o_], [o_])
                    P.tt(o_[:], o_[:], ngb[:], ALU.mult, [o_, ngb], [o_])
                    P.tt(o_[:], o_[:], a[:].rearrange("p h e -> p (h e)"), ALU.mult, [o_, a], [o_])
                    P.dma("sp", B0[i * 128:(i + 1) * 128, :], o_[:], R=[o_], W=[("B0", i)])
                P.emit()

            P.barrier()
            with ExitStack() as st:
                wsn = sbt(st, "wsn", [128, 4, 128]); wsT = sbt(st, "wsT", [128, 4, 128], BF16)
                P.dma("sp", wsn[:], sgu_w[l].rearrange("g t s -> t g s"), W=[wsn])
                for g in range(4):
                    P.tr(PS[0][:, g * 128:(g + 1) * 128], wsn[:, g, :], ident[:], [wsn, ident], [PS[0]])
                P.cp(wsT[:].rearrange("p g t -> p (g t)"), PS[0][:, :], [PS[0]], [wsT])
                bsT = load_bc(st, "bsT", sgu_b[l, :], 512)
                sv = ring(st, "sv", 2, [128, 512]); su = ring(st, "su", 2, [128, 4, 128]); vn = ring(st, "vn", 2, [128, 512], BF16)
                sq = ring(st, "sq2", 2, [128, 512]); mu = ring(st, "mu2", 2, [128, 1]); vr = ring(st, "vr2", 2, [128, 1]); b1 = ring(st, "b1", 2, [128, 4, 128])
                for i in range(NT):
                    k = i % 2; v_ = sv[k]; u_ = su[k]; n_ = vn[k]; s_ = sq[k]; m_ = mu[k]; r_ = vr[k]; o_ = b1[k]
                    P.dma("sp", v_[:], SV[i * 128:(i + 1) * 128, :], R=[("SV", i, 0)], W=[v_])
                    P.dma("pool", u_[:], SUT.rearrange("(g c) t -> c g t", c=128)[:, :, i * 128:(i + 1) * 128], R=[("SUT", i)], W=[u_])
                    P.act(v_[:], v_[:], AF.Gelu, [v_], [v_])
                    P.act(u_[:], u_[:], AF.Gelu, [u_], [u_])
                    P.red(m_[:], v_[:], ALU.add, [v_], [m_])
                    P.ts(m_[:], m_[:], 1.0 / 512, None, ALU.mult, None, [m_], [m_])
                    P.ts(v_[:], v_[:], m_[:], None, ALU.subtract, None, [v_, m_], [v_])
                    P.tt(s_[:], v_[:], v_[:], ALU.mult, [v_], [s_])
                    P.red(r_[:], s_[:], ALU.add, [s_], [r_])
                    P.ts(r_[:], r_[:], 1.0 / 512, EPS, ALU.mult, ALU.add, [r_], [r_])
                    rsqrt_(r_[:], [r_])
                    P.ts(n_[:], v_[:], r_[:], None, ALU.mult, None, [v_, r_], [n_])
                    ps = PS[1 + k]
                    for g in range(4):
                        P.mm(ps[:, g * 128:(g + 1) * 128], n_[:, g * 128:(g + 1) * 128], wsT[:, g, :], True, True, [n_, wsT], [ps])
                    P.tt(s_[:], ps[:, :], bsT[:], ALU.add, [ps, bsT], [s_])
                    P.tt(o_[:].rearrange("p g t -> p (g t)"), s_[:], u_[:].rearrange("p g t -> p (g t)"), ALU.mult, [s_, u_], [o_])
                    P.dma("sp", B1T.rearrange("(g c) t -> c g t", c=128)[:, :, i * 128:(i + 1) * 128], o_[:], R=[o_], W=[("B1T", i)])
                P.emit()

            P.barrier()
            with ExitStack() as st:
                fz = ring(st, "fz", 2, [128, 4, 128]); fzb = ring(st, "fzb", 2, [128, 4, 128], BF16); zt = ring(st, "zt", 2, [128, 2, 4, 128])
                for i in range(NT):
                    k = i % 2; f_ = fz[k]; fb = fzb[k]; z_ = zt[k]
                    P.dma("sp", f_[:], FZT.rearrange("(g c) t -> c g t", c=128)[:, :, i * 128:(i + 1) * 128], R=[("FZT", i)], W=[f_])
                    P.cp(fb[:], f_[:], [f_], [fb])
                    for half in range(2):
                        ps = PS[k * 2 + half]
                        for gg in range(2):
                            g = half * 2 + gg
                            P.mm(ps[:, gg * 256:(gg + 1) * 256], fb[:, g, :], cdb[:], True, True, [fb, cdb], [ps])
                        P.act(z_[:, :, half * 2:half * 2 + 2, :], ps[:, :].rearrange("p (g r c) -> p r g c", r=2, c=128), AF.Copy, [ps], [z_])
                    for r in range(2):
                        P.dma("sp" if r == 0 else "pool", Z[r, i * 128:(i + 1) * 128, :], z_[:, r, :, :].rearrange("p g c -> p (g c)"), R=[z_], W=[("Z", r, i)])
                P.emit()
            for (s0, sl), gxd, fsd, APD in ((segs[0], gx_c, fs_c, APDC), (segs[1], gx_x, fs_x, APDX)):
                N1 = sl // 128
                P.barrier()
                with ExitStack() as st:
                    gf = ring(st, "gf", 2, [128, 3, 128]); gb = ring(st, "gb", 2, [128, 3, 128], BF16)
                    zr = ring(st, "zr", 2, [128, 2, 512]); zb = ring(st, "zb", 2, [128, 2, 512], BF16); ao = ring(st, "ao", 2, [128, 2, 512])
                    zkeys = [("Z", r, i) for r in range(2) for i in range(s0 // 128, (s0 + sl) // 128)]
                    for n1 in range(N1):
                        k = n1 % 2; g_ = gf[k]; gb_ = gb[k]; z_ = zr[k]; zb_ = zb[k]; a_ = ao[k]
                        P.dma("sp", g_[:], gxd[n1], W=[g_])
                        P.act(gb_[:], g_[:], AF.Copy, [g_], [gb_])
                        for r in range(2):
                            src = Z[r, s0:s0 + sl, :].rearrange("(n2 n1) c -> n1 n2 c", n1=N1)[n1]
                            P.dma("sp" if r == 0 else "pool", z_[:, r, :], src, R=zkeys, W=[z_])
                        P.cp(zb_[:], z_[:], [z_], [zb_])
                        pr = PS[k * 2]; pi = PS[k * 2 + 1]
                        P.mm(pr[:, :], gb_[:, 0, :], zb_[:, 0, :], True, False, [gb_, zb_], [pr])
                        P.mm(pr[:, :], gb_[:, 1, :], zb_[:, 1, :], False, True, [gb_, zb_], [pr])
                        P.mm(pi[:, :], gb_[:, 0, :], zb_[:, 1, :], True, False, [gb_, zb_], [pi])
                        P.mm(pi[:, :], gb_[:, 2, :], zb_[:, 0, :], False, True, [gb_, zb_], [pi])
                        P.act(a_[:, 0, :], pr[:, :], AF.Copy, [pr], [a_])
                        P.cp(a_[:, 1, :], pi[:, :], [pi], [a_])
                        for r in range(2):
                            P.dma("sp" if r == 0 else "pool", APD[r, n1, :, :], a_[:, r, :], R=[a_], W=[("APD", r, n1)])
                    P.emit()
                P.barrier()
                with ExitStack() as st:
                    KB = 8 if N1 > 8 else 32
                    nrg = 2 if N1 > 8 else 1
                    fsf = sbt(st, "fsf", [128, N1]); fsb = sbt(st, "fsb", [128, N1], BF16)
                    P.memset(fsf[:], 0.0, [fsf], eng="dve")
                    P.dma("sp", fsf[0:2 * N1, :], fsd[:, :], R=[fsf], W=[fsf])
                    P.cp(fsb[:], fsf[:], [fsf], [fsb])
                    rt = ring(st, "rt", nrg, [128, KB, 512]); rb = ring(st, "rb", nrg, [128, KB, 512], BF16); ob = ring(st, "ob", nrg, [N1, KB, 512])
                    for r_ in rt:
                        P.memset(r_[:], 0.0, [r_], eng="dve")
                    akeys = [("APD", r, n1) for r in range(2) for n1 in range(N1)]
                    for kb0 in range(0, 128, KB):
                        k = (kb0 // KB) % 2; r_ = rt[k]; rb_ = rb[k]; o_ = ob[k]
                        for r in range(2):
                            P.dma("sp" if r == 0 else "pool", r_[r * N1:(r + 1) * N1, :, :], APD[r, 0:N1, kb0:kb0 + KB, :], R=akeys + [r_], W=[r_])
                        P.act(rb_[:, 0:KB // 2, :], r_[:, 0:KB // 2, :], AF.Copy, [r_], [(rb_.key, 0)])
                        P.cp(rb_[:, KB // 2:KB, :], r_[:, KB // 2:KB, :], [r_], [(rb_.key, 1)])
                        for kk in range(KB):
                            ps = PS[kk % 4]
                            P.mm(ps[0:N1, :], fsb[:], rb_[:, kk, :], True, True, [fsb, (rb_.key, 0), (rb_.key, 1)], [ps])
                            if kk % 2 == 0:
                                P.act(o_[:, kk, :], ps[0:N1, :], AF.Copy, [ps], [o_])
                            else:
                                P.cp(o_[:, kk, :], ps[0:N1, :], [ps], [o_])
                        dst = B2[s0:s0 + sl, :].rearrange("(k1 k2) c -> k1 k2 c", k2=128)[:, kb0:kb0 + KB, :]
                        P.dma("sp", dst, o_[:], R=[o_], W=[("B2", i) for i in range(s0 // 128, (s0 + sl) // 128)])
                    P.emit()

            P.barrier()
            with ExitStack() as st:
                wbr = sbt(st, "wbr", [128, 12, D], BF16); wob = sbt(st, "wob", [128, 8, D], BF16)
                wst = ring(st, "wst2", 2, [128, 4, D])
                for r in range(3):
                    w = wst[r % 2]
                    P.dma("sp", w[:], w_br[l, r].rearrange("(j p) n -> p j n", p=128), W=[w])
                    P.cp(wbr[:, r * 4:(r + 1) * 4, :], w[:], [w], [("wbr", r)])
                for hh_ in range(2):
                    w = wst[(3 + hh_) % 2]
                    P.dma("pool", w[:], w_out[l].rearrange("(j p) n -> p j n", p=128)[:, hh_ * 4:(hh_ + 1) * 4, :], W=[w])
                    P.act(wob[:, hh_ * 4:(hh_ + 1) * 4, :], w[:], AF.Copy, [w], [("wob", hh_)])
                wkeys = [("wbr", r) for r in range(3)] + [("wob", h) for h in range(2)]
                g1bc = [modbc(st, f"g1bc{r}", 2, r) for r in range(2)]
                b0 = ring(st, "b0", 2, [128, 512]); b2 = ring(st, "b2", 2, [128, 512]); b1f = ring(st, "b1f", 2, [128, 4, 128])
                bT = ring(st, "bT", 2, [128, 12, 128], BF16); mg = ring(st, "mg", 2, [128, 3072]); y = ring(st, "y", 2, [128, D]); tm = ring(st, "tm", 2, [128, D])
                yT = ring(st, "yT", 2, [128, 8, 128], BF16); xt = ring(st, "xt2", 2, [128, D])
                for i in range(NT):
                    k = i % 2; r = 1 if i < NCT else 0
                    a = b0[k]; c_ = b2[k]; f1 = b1f[k]; bt = bT[k]; m_ = mg[k]; y_ = y[k]; t_ = tm[k]; yt = yT[k]; x_ = xt[k]
                    P.dma("sp", a[:], B0[i * 128:(i + 1) * 128, :], R=[("B0", i)], W=[a])
                    P.dma("pool", c_[:], B2[i * 128:(i + 1) * 128, :], R=[("B2", i)], W=[c_])
                    P.dma("sp", f1[:], B1T.rearrange("(g c) t -> c g t", c=128)[:, :, i * 128:(i + 1) * 128], R=[("B1T", i)], W=[f1])
                    P.dma("pool", m_[:], MG[i * 128:(i + 1) * 128, :], R=[("MG", i, 512 * m) for m in range(6)], W=[m_])
                    P.dma("sp", x_[:], XS[i * 128:(i + 1) * 128, :], R=[("XS", i)], W=[x_])
                    for bi, src in ((0, a), (2, c_)):
                        ps = PS[0 if bi == 0 else 1]
                        for j in range(4):
                            P.tr(ps[:, j * 128:(j + 1) * 128], src[:, j * 128:(j + 1) * 128], ident[:], [src, ident], [ps])
                        P.act(bt[:, bi * 4:(bi + 1) * 4, :], ps[:, :].rearrange("p (j t) -> p j t", t=128), AF.Copy, [ps], [(bt.key, bi)])
                    P.act(bt[:, 4:8, :], f1[:], AF.Copy, [f1], [(bt.key, 1)])
                    P.act(m_[:], m_[:], AF.Sigmoid, [m_], [m_])
                    for rr in range(3):
                        pa, pb = PS[2 + 2 * (rr % 2)], PS[3 + 2 * (rr % 2)]
                        for half, ps in enumerate((pa, pb)):
                            for j in range(4):
                                P.mm(ps[:, :], bt[:, rr * 4 + j, :], wbr[:, rr * 4 + j, half * 512:(half + 1) * 512], j == 0, j == 3, [(bt.key, rr)] + wkeys, [ps])
                        dstt = y_ if rr == 0 else t_
                        for half, ps in enumerate((pa, pb)):
                            P.tt(dstt[:, half * 512:(half + 1) * 512], ps[:, :], m_[:, rr * D + half * 512: rr * D + (half + 1) * 512], ALU.mult, [ps, m_], [(dstt.key, half)])
                        if rr > 0:
                            P.tt(y_[:], y_[:], t_[:], ALU.add, [(y_.key, 0), (y_.key, 1), (t_.key, 0), (t_.key, 1)], [(y_.key, 0), (y_.key, 1)])
                    for half in range(2):
                        ps = PS[6 + half]
                        for jj in range(4):
                            j = half * 4 + jj
                            P.tr(ps[:, jj * 128:(jj + 1) * 128], y_[:, j * 128:(j + 1) * 128], ident[:], [(y_.key, 0), (y_.key, 1), ident], [ps])
                        P.act(yt[:, half * 4:(half + 1) * 4, :], ps[:, :].rearrange("p (j t) -> p j t", t=128), AF.Copy, [ps], [(yt.key, half)])
                    for half in range(2):
                        ps = PS[0 + half]
                        for j in range(8):
                            P.mm(ps[:, :], yt[:, j, :], wob[:, j, half * 512:(half + 1) * 512], j == 0, j == 7, [(yt.key, 0), (yt.key, 1)] + wkeys, [ps])
                        P.tt(t_[:, half * 512:(half + 1) * 512], ps[:, :], g1bc[r][:, half * 512:(half + 1) * 512], ALU.mult, [ps, g1bc[r]], [(t_.key, half)])
                    P.tt(x_[:], x_[:], t_[:], ALU.add, [x_, (t_.key, 0), (t_.key, 1)], [x_])
                    P.dma("sp", XS[i * 128:(i + 1) * 128, :], x_[:], R=[x_], W=[("XS", i)])
                P.emit()

            P.barrier()
            with ExitStack() as st:
                KR = 4
                tf = ring(st, "tf", 3, [128, KR, D]); tb = ring(st, "tb", 3, [128, KR, D], BF16)
                it = 0
                for (src, dst, dn) in ((peer_u, UV[:, 0, :], "UB"), (peer_v, UV[:, 1, :], "VB")):
                    for ch in range(NEXP // (128 * KR)):
                        f_ = tf[it]; b_ = tb[it]
                        r0 = ch * 128 * KR
                        P.dma("sp", f_[:], src[l, r0:r0 + 128 * KR, :].rearrange("(p k) d -> p k d", k=KR), W=[f_])
                        if it % 3 == 0:
                            P.cp(b_[:], f_[:], [f_], [b_])
                        else:
                            P.act(b_[:], f_[:], AF.Copy, [f_], [b_])
                        P.dma("pool", dst[r0:r0 + 128 * KR, :].rearrange("(p k) d -> p k d", k=KR), b_[:], R=[b_], W=[(dn, ch)])
                        it += 1
                P.emit()

            P.barrier()
            with ExitStack() as st:
                wqb = sbt(st, "wqb", [128, 8, 2048], BF16)
                with ExitStack() as st2:
                    wst = ring(st2, "wst3", 2, [128, 8, 256])
                    for blk in range(8):
                        w = wst[blk % 2]
                        P.dma("sp" if blk % 2 == 0 else "pool", w[:], peer_wq[l].rearrange("(j p) n -> p j n", p=128)[:, :, blk * 256:(blk + 1) * 256], W=[w])
                        if blk % 2 == 0:
                            P.cp(wqb[:, :, blk * 256:(blk + 1) * 256], w[:], [w], [("wqb", blk)])
                        else:
                            P.act(wqb[:, :, blk * 256:(blk + 1) * 256], w[:], AF.Copy, [w], [("wqb", blk)])
                    P.emit()
                P.barrier()
                wqkeys = [("wqb", b) for b in range(8)]
                kn = sbt(st, "kn", [128, 2, 128]); kTb = sbt(st, "kTb", [128, 2, 128], BF16)
                P.dma("sp", kn[:], peer_keys[l].rearrange("p k c -> k p c"), W=[kn])
                for p in range(2):
                    P.tr(PS[0][:, p * 128:(p + 1) * 128], kn[:, p, :], ident[:], [kn, ident], [PS[0]])
                P.cp(kTb[:].rearrange("c p k -> c (p k)"), PS[0][:, 0:256], [PS[0]], [kTb])
                g2 = load_bc(st, "g2", norm2_g[l, :], D)
                A2t = sbt(st, "A2t", [128, D]); B2t = sbt(st, "B2t", [128, D]); g2bct = sbt(st, "g2bct", [128, D])

                def load_mods(r):
                    P.dma("sp", A2t[:], MOD[l, r, 4 * D:5 * D].partition_broadcast(128), W=[A2t])
                    P.dma("sp", B2t[:], MOD[l, r, 3 * D:4 * D].partition_broadcast(128), W=[B2t])
                    P.dma("sp", g2bct[:], MOD[l, r, 5 * D:6 * D].partition_broadcast(128), W=[g2bct])
                    P.stt(A2t[:], A2t[:], 1.0, g2[:], ALU.add, ALU.mult, [A2t, g2], [A2t])

                xt = ring(st, "xt3", 2, [128, D]); hn = ring(st, "hn", 2, [128, D]); ssr = ring(st, "ss3", 2, [128, 1]); rsr = ring(st, "rs3", 2, [128, 1])
                hT = ring(st, "hT3", 1, [128, 8, 128], BF16); qTb = ring(st, "qTb", 1, [128, 16, 128], BF16)
                sc_ = ring(st, "scr", 1, [128, 16, 128]); wk = ring(st, "wk", 1, [128, 16, 128])
                tv = ring(st, "tv", 2, [128, 16, 16]); ti = ring(st, "ti", 2, [128, 16, 16], U32); tif = ring(st, "tif", 1, [128, 16, 16])
                cs = ring(st, "cs", 1, [128, 8, 256])
                bv = ring(st, "bv", 2, [128, 8, 16]); bj = ring(st, "bj", 2, [128, 8, 16], U32); ba = ring(st, "ba", 2, [128, 8, 16], U32); bbq = ring(st, "bbq", 2, [128, 8, 16], U32)
                baf = ring(st, "baf", 2, [128, 8, 16]); bbf = ring(st, "bbf", 2, [128, 8, 16])
                oh = ring(st, "oh", 1, [128, 8, 16, 16]); i1s = ring(st, "i1s", 2, [128, 8, 16]); i2s = ring(st, "i2s", 2, [128, 8, 16])
                ei = ring(st, "ei", 2, [128, 128], I32); mx = ring(st, "mx", 2, [128, 8]); pw = ring(st, "pw", 2, [128, 8, 16]); sm = ring(st, "sm", 2, [128, 8])
                RC = 8
                ug = ring(st, "ug", 2, [128, RC, 2 * D], BF16)
                ux = ring(st, "ux", 4, [128, RC]); acc = ring(st, "acc", 1, [128, D]); junk = ring(st, "junk", 2, [128, D], BF16)
                dg = ring(st, "dg", 2, [128, RC, 128], BF16)
                UVf = UV.rearrange("e two d -> e (two d)")
                cnt = {"uc": 0, "jc": 0}

                def top16(vals, work, outv, outi, rk, wk_, ok):
                    P.op("dve", lambda e: e.max(out=outv[:, 0:8], in_=vals), rk, ok)
                    P.op("dve", lambda e: e.match_replace(out=work, in_to_replace=outv[:, 0:8], in_values=vals, imm_value=-1e30), rk + ok, wk_)
                    P.op("dve", lambda e: e.max(out=outv[:, 8:16], in_=work), wk_, ok)
                    P.op("dve", lambda e: e.max_index(out=outi[:, 0:8], in_max=outv[:, 0:8], in_values=vals), rk + ok, ok)
                    P.op("dve", lambda e: e.max_index(out=outi[:, 8:16], in_max=outv[:, 8:16], in_values=vals), rk + ok, ok)

                def p1_tile(i):
                    k = i % 2
                    x_ = xt[k]; n_ = hn[k]; ss = ssr[k]; rs = rsr[k]; h_ = hT[k]; q_ = qTb[k]; s_ = sc_[k]; w_ = wk[k]
                    tv_ = tv[k]; ti_ = ti[k]; tf_ = tif[k]; cs_ = cs[k]; bv_ = bv[k]; bj_ = bj[k]; ba_ = ba[k]; bb_ = bbq[k]
                    P.dma("sp", x_[:], XS[i * 128:(i + 1) * 128, :], R=[("XS", i)], W=[x_])
                    rms_rstd(x_, n_, ss, rs)
                    P.ts(n_[:], x_[:], rs[:], None, ALU.mult, None, [x_, rs], [n_])
                    P.tt(n_[:], n_[:], A2t[:], ALU.mult, [n_, A2t], [n_])
                    P.tt(n_[:], n_[:], B2t[:], ALU.add, [n_, B2t], [n_])
                    for half in range(2):
                        ps = PS[half]
                        for jj in range(4):
                            j = half * 4 + jj
                            P.tr(ps[:, jj * 128:(jj + 1) * 128], n_[:, j * 128:(j + 1) * 128], ident[:], [n_, ident], [ps])
                        P.act(h_[:, half * 4:(half + 1) * 4, :], ps[:, :].rearrange("p (j t) -> p j t", t=128), AF.Copy, [ps], [(h_.key, half)])
                    yield
                    for g4 in range(4):
                        ps = PS[2 + g4]
                        for gg in range(4):
                            grp = g4 * 4 + gg
                            for j in range(8):
                                P.mm(ps[:, gg * 128:(gg + 1) * 128], wqb[:, j, grp * 128:(grp + 1) * 128], h_[:, j, :], j == 0, j == 7, [(h_.key, 0), (h_.key, 1)] + wqkeys, [ps])
                        P.act(q_[:, g4 * 4:(g4 + 1) * 4, :], ps[:, :].rearrange("p (g t) -> p g t", t=128), AF.Copy, [ps], [(q_.key, g4)])
                        if g4 % 2 == 1:
                            yield
                    for g4 in range(4):
                        ps = PS[g4]
                        for gg in range(4):
                            grp = g4 * 4 + gg
                            P.mm(ps[:, gg * 128:(gg + 1) * 128], q_[:, grp, :], kTb[:, grp % 2, :], True, True, [(q_.key, g4), kTb], [ps])
                        P.act(s_[:, g4 * 4:(g4 + 1) * 4, :], ps[:, :].rearrange("p (g t) -> p g t", t=128), AF.Copy, [ps], [(s_.key, g4)])
                    yield
                    for grp in range(16):
                        top16(s_[:, grp, :], w_[:, grp, :], tv_[:, grp, :], ti_[:, grp, :], [(s_.key, grp // 4)], [(w_.key, grp)], [(tv_.key, grp)])
                        if grp % 2 == 1:
                            yield
                    tvk = [(tv_.key, g) for g in range(16)]
                    P.cp(tf_[:], ti_[:], tvk, [tf_])
                    tv4 = tv_[:].rearrange("p (h q) a -> p h q a", q=2)
                    P.tt(cs_[:].rearrange("p h (a b) -> p h a b", b=16), tv4[:, :, 0, :].unsqueeze(3).to_broadcast([128, 8, 16, 16]),
                         tv4[:, :, 1, :].unsqueeze(2).to_broadcast([128, 8, 16, 16]), ALU.add, tvk, [cs_])
                    cwv = w_[:].rearrange("p (h a) b -> p h (a b)", a=2)
                    wall = [(w_.key, g) for g in range(16)]
                    for h in range(8):
                        top16(cs_[:, h, :], cwv[:, h, :], bv_[:, h, :], bj_[:, h, :], [cs_], wall, [(bv_.key, h)])
                        if h % 2 == 1:
                            yield
                    bvk = [(bv_.key, h) for h in range(8)]
                    P.ts(ba_[:], bj_[:], 4, None, ALU.logical_shift_right, None, bvk, [ba_])
                    P.ts(bb_[:], bj_[:], 15, None, ALU.bitwise_and, None, bvk, [bb_])
                    af = baf[k]; bf = bbf[k]; oh_ = oh[k]; s1 = i1s[k]; s2 = i2s[k]
                    P.cp(af[:], ba_[:], [ba_], [af]); P.cp(bf[:], bb_[:], [bb_], [bf])
                    tf4 = tf_[:].rearrange("p (h q) a -> p h q a", q=2)
                    io4 = iota16[:].unsqueeze(1).unsqueeze(1).to_broadcast([128, 8, 16, 16])
                    for (sel, src_q, dsts) in ((af, 0, s1), (bf, 1, s2)):
                        P.tt(oh_[:], sel[:].unsqueeze(3).to_broadcast([128, 8, 16, 16]), io4, ALU.is_equal, [sel, iota16], [oh_])
                        P.tt(oh_[:], oh_[:], tf4[:, :, src_q, :].unsqueeze(2).to_broadcast([128, 8, 16, 16]), ALU.mult, [oh_, tf_], [oh_])
                        P.red(dsts[:], oh_[:], ALU.add, [oh_], [dsts])
                    P.stt(s1[:], s1[:], 128.0, s2[:], ALU.mult, ALU.add, [s1, s2], [s1])
                    e_ = ei[k]
                    P.cp(e_[:], s1[:].rearrange("p h k -> p (h k)"), [s1], [e_])
                    m_ = mx[k]; p_ = pw[k]; z_ = sm[k]
                    P.red(m_[:], bv_[:], ALU.max, bvk, [m_])
                    P.tt(p_[:], bv_[:], m_[:].unsqueeze(2).to_broadcast([128, 8, 16]), ALU.subtract, bvk + [m_], [p_])
                    P.act(p_[:], p_[:], AF.Exp, [p_], [p_])
                    P.red(z_[:], p_[:], ALU.add, [p_], [z_])
                    P.op("dve", (lambda zz: (lambda e: e.reciprocal(out=zz, in_=zz)))(z_[:]), [z_], [z_])
                    P.tt(p_[:], p_[:], z_[:].unsqueeze(2).to_broadcast([128, 8, 16]), ALU.mult, [p_, z_], [p_])
                    yield

                def p2_tile(i):
                    k = i % 2
                    ix = ei[k]; p_ = pw[k]; n_ = hn[k]; x_ = xt[k]; a_ = acc[0]
                    pwv = p_[:].rearrange("p h k -> p (h k)")
                    pacc = (PS[6], PS[7])
                    nch = 128 // RC
                    for c in range(nch):
                        uc = cnt["uc"]; cnt["uc"] += 1
                        g_ = ug[uc]; u_ = ux[uc]; d_ = dg[uc]
                        for rr in range(RC):
                            P.gather(g_[:, rr, :], UVf, ix[:, c * RC + rr: c * RC + rr + 1], R=[ix], W=[(g_.key, rr)])
                        P.memset(u_[:], 0.0, [u_], eng="dve")
                        for rr in range(RC):
                            cnt["jc"] += 1
                            P.op("dve", (lambda o, a0, b0, ac: (lambda e: e.scalar_tensor_tensor(out=o, in0=a0, scalar=1.0, in1=b0, op0=ALU.mult, op1=ALU.mult, accum_out=ac)))(junk[cnt["jc"]][:], g_[:, rr, 0:D], n_[:], u_[:, rr:rr + 1]),
                                 [(g_.key, rr), n_, u_], [junk[cnt["jc"]], (u_.key, rr)])
                        uk = [(u_.key, rr) for rr in range(RC)]
                        P.act(u_[:], u_[:], AF.Gelu, uk + [u_], [u_])
                        P.tt(u_[:], u_[:], pwv[:, c * RC:(c + 1) * RC], ALU.mult, [u_, p_], [u_])
                        for rr in range(RC):
                            P.act(d_[:, rr, :], identb[:], AF.Copy, [identb, u_], [(d_.key, rr)], scale=u_[:, rr:rr + 1])
                        for rr in range(RC):
                            first = (c == 0 and rr == 0); last = (c == nch - 1 and rr == RC - 1)
                            for half in range(2):
                                P.mm(pacc[half][:, :], d_[:, rr, :], g_[:, rr, D + half * 512: D + (half + 1) * 512], first, last, [(d_.key, rr), (g_.key, rr)], [pacc[half]])
                        yield
                    for half in range(2):
                        P.tt(a_[:, half * 512:(half + 1) * 512], pacc[half][:, :], g2bct[:, half * 512:(half + 1) * 512], ALU.mult, [pacc[half], g2bct], [(a_.key, half)])
                    P.tt(x_[:], x_[:], a_[:], ALU.add, [x_, (a_.key, 0), (a_.key, 1)], [x_])
                    P.dma("sp", XS[i * 128:(i + 1) * 128, :], x_[:], R=[x_], W=[("XS", i)])

                from itertools import zip_longest

                def run_pair(g1, g2_):
                    for _ in zip_longest(g1 if g1 is not None else (), g2_ if g2_ is not None else ()):
                        pass

                load_mods(1)
                for i in range(NCT):
                    run_pair(p1_tile(i), p2_tile(i - 1) if i >= 1 else None)
                run_pair(None, p2_tile(NCT - 1))
                load_mods(0)
                for i in range(NCT, NT):
                    run_pair(p1_tile(i), p2_tile(i - 1) if i > NCT else None)
                run_pair(None, p2_tile(NT - 1))
                P.emit()

        P.barrier()
        with ExitStack() as st:
            fg = load_bc(st, "fg", final_g[0, :], D)
            xt = ring(st, "xt5", 2, [128, D]); jn = ring(st, "jn", 2, [128, D]); ssr = ring(st, "ss5", 2, [128, 1]); rsr = ring(st, "rs5", 2, [128, 1])
            for i in range(NCT, NT):
                k = i % 2; x_ = xt[k]; j_ = jn[k]; ss = ssr[k]; rs = rsr[k]
                P.dma("sp", x_[:], XS[i * 128:(i + 1) * 128, :], R=[("XS", i)], W=[x_])
                rms_rstd(x_, j_, ss, rs)
                P.ts(j_[:], x_[:], rs[:], None, ALU.mult, None, [x_, rs], [j_])
                P.tt(j_[:], j_[:], fg[:], ALU.mult, [j_, fg], [j_])
                P.dma("sp", out[(i - NCT) * 128:(i - NCT + 1) * 128, :], j_[:], R=[j_], W=[("out", i)])
            P.emit(final=True)
    return nc


_CACHE = {}


def make_in_maps(inputs, T, CT, L, nb):
    pe = _pos_embed(T)
    gxx, fsx = _consts(T)
    gxc, fsc = _consts(CT)
    cc_ = np.arange(128, dtype=np.float64)
    ang = 2 * np.pi * ((cc_[:, None] * cc_[None, :]) % 128) / 128
    cdft = np.concatenate([np.cos(ang), -np.sin(ang)], axis=1).astype(np.float32)
    f = lambda a: np.ascontiguousarray(np.asarray(a, dtype=np.float32))
    shared = {
        "pe": pe, "w_mod": f(inputs["w_mod"])[:L], "b_mod": f(inputs["b_mod"])[:L],
        "norm1_g": f(inputs["norm1_g"])[:L], "norm2_g": f(inputs["norm2_g"])[:L],
        "w_in": f(inputs["w_in"])[:L], "b_in": f(inputs["b_in"])[:L], "conv_qk": f(inputs["conv_qk"])[:L],
        "mlstm_norm_g": f(inputs["mlstm_norm_g"])[:L], "sgu_w": f(inputs["sgu_w"])[:L],
        "sgu_b": f(inputs["sgu_b"])[:L].reshape(L, 512), "w_br": f(inputs["w_br"])[:L], "w_out": f(inputs["w_out"])[:L],
        "peer_wq": f(inputs["peer_wq"])[:L], "peer_keys": f(inputs["peer_keys"])[:L],
        "peer_u": f(inputs["peer_u"])[:L], "peer_v": f(inputs["peer_v"])[:L],
        "final_g": f(inputs["final_g"]).reshape(1, D), "cdft": cdft,
        "gx_x": gxx, "fs_x": fsx, "gx_c": gxc, "fs_c": fsc,
    }
    x = f(inputs["x"]); c = f(inputs["c"]); ctx = f(inputs["ctx"]); c_ctx = f(inputs["c_ctx"])
    maps = []
    for b in range(nb):
        m = dict(shared)
        m["x"] = x[b]; m["ctx"] = ctx[b]
        m["cc"] = np.ascontiguousarray(np.stack([c[b], c_ctx], axis=0))
        maps.append(m)
    return maps


def kernel(**inputs):
    x = np.asarray(inputs["x"])
    B, T, _ = x.shape
    CT = np.asarray(inputs["ctx"]).shape[1]
    L = np.asarray(inputs["w_mod"]).shape[0]
    key = (T, CT, L)
    if key not in _CACHE:
        _CACHE[key] = build(T, CT, L)
    nc = _CACHE[key]
    maps = make_in_maps(inputs, T, CT, L, B)
    res = run_bass_kernel_spmd(nc, maps, core_ids=list(range(B)))
    return np.stack([res.results[b]["out"] for b in range(B)], axis=0).astype(np.float32)
```
